# Optimizing a Trainium2 kernel written in Bass

```python
import jax, jax.numpy as jnp
from jax import lax
import numpy as np

D_MODEL = 1024
BATCH = 4
SEQ = 8192
DEPTH = 2

N_HEADS = 8
QK_NOPE_DIM = 64
QK_ROPE_DIM = 32
V_HEAD_DIM = 64
Q_LORA_RANK = 384
KV_LORA_RANK = 256
ROPE_BASE = 10000.0
Q_BLOCK = 128
QK_DIM = QK_NOPE_DIM + QK_ROPE_DIM
ATTN_WIDTH = N_HEADS * V_HEAD_DIM
CONV_CHANNELS = 512
CONV_KERNEL = 31
N_GROUPS = 4
EXPERTS_PER_GROUP = 8
TOP_K_IN_GROUP = 2
D_EXPERT = 128
EPS = 1e-6
SPLIT_POINTS = tuple(int(v) for v in np.cumsum([Q_LORA_RANK, KV_LORA_RANK, QK_ROPE_DIM, 2 * CONV_CHANNELS]))
IN_COLS = Q_LORA_RANK + KV_LORA_RANK + QK_ROPE_DIM + 2 * CONV_CHANNELS + 2 * D_MODEL

kernel_name = "hybrid_mla_conformer_hmoe_adaln"


def rmsnorm(x, g):
    xf = x.astype(jnp.float32)
    y = xf * lax.rsqrt(jnp.mean(xf * xf, axis=-1, keepdims=True) + EPS)
    return (y * g.astype(jnp.float32)).astype(x.dtype)


def layernorm(x, g, b):
    xf = x.astype(jnp.float32)
    mu = jnp.mean(xf, axis=-1, keepdims=True)
    var = jnp.mean(jnp.square(xf - mu), axis=-1, keepdims=True)
    y = (xf - mu) * lax.rsqrt(var + EPS)
    return (y * g.astype(jnp.float32) + b.astype(jnp.float32)).astype(x.dtype)


def rope_tables(positions):
    inv_freq = ROPE_BASE ** (-jnp.arange(0, QK_ROPE_DIM, 2, dtype=jnp.float32) / QK_ROPE_DIM)
    ang = positions.astype(jnp.float32)[..., None] * inv_freq
    return jnp.cos(ang), jnp.sin(ang)


def apply_rope(x, cos, sin):
    xf = x.astype(jnp.float32)
    x1, x2 = jnp.split(xf, 2, axis=-1)
    return jnp.concatenate([x1 * cos - x2 * sin, x2 * cos + x1 * sin], axis=-1).astype(x.dtype)


def causal_block_attention(q, k, v, positions):
    B, H, S, _ = q.shape
    nb = S // Q_BLOCK
    scale = QK_DIM ** -0.5
    qb = q.reshape(B, H, nb, Q_BLOCK, QK_DIM).transpose(2, 0, 1, 3, 4)
    pb = positions.reshape(B, nb, Q_BLOCK).transpose(1, 0, 2)
    neg = jnp.finfo(jnp.float32).min

    def one_block(args):
        q_blk, p_blk = args
        s = jnp.einsum('bhqd,bhkd->bhqk', q_blk, k, preferred_element_type=jnp.float32) * scale
        mask = positions[:, None, None, :] <= p_blk[:, None, :, None]
        p = jax.nn.softmax(jnp.where(mask, s, neg), axis=-1)
        return jnp.einsum('bhqk,bhkd->bhqd', p.astype(v.dtype), v)

    out = lax.map(one_block, (qb, pb))
    return out.transpose(1, 0, 3, 2, 4).reshape(B, S, H * V_HEAD_DIM)


def causal_depthwise_conv(u, w, b):
    y = lax.conv_general_dilated(
        u, w[:, None, :].astype(u.dtype), window_strides=(1,),
        padding=[(CONV_KERNEL - 1, 0)], dimension_numbers=('NWC', 'WIO', 'NWC'),
        feature_group_count=u.shape[-1])
    return y + b


def hierarchical_moe(h, rg_w, rg_b, re_w, re_b, e_gate, e_up, e_down):
    B, S, D = h.shape
    t = h.reshape(-1, D)
    g_prob = jax.nn.softmax((t @ rg_w + rg_b).astype(jnp.float32), axis=-1)
    g_val, g_idx = lax.top_k(g_prob, 1)
    e_logits = (t @ re_w + re_b).astype(jnp.float32).reshape(-1, N_GROUPS, EXPERTS_PER_GROUP)
    e_sel = jnp.take_along_axis(e_logits, g_idx[:, :, None], axis=1)[:, 0]
    e_val, e_idx = lax.top_k(jax.nn.softmax(e_sel, axis=-1), TOP_K_IN_GROUP)
    e_val = e_val / jnp.sum(e_val, axis=-1, keepdims=True)
    w_group = jnp.sum(jax.nn.one_hot(e_idx, EXPERTS_PER_GROUP, dtype=jnp.float32) * e_val[..., None], axis=1)
    combine = (jax.nn.one_hot(g_idx[:, 0], N_GROUPS, dtype=jnp.float32)[:, :, None]
               * (g_val * w_group)[:, None, :]).astype(h.dtype)
    out = jnp.zeros_like(t)
    for gi in range(N_GROUPS):
        a = jnp.einsum('td,edf->tef', t, e_gate[gi])
        u = jnp.einsum('td,edf->tef', t, e_up[gi])
        hid = jax.nn.silu(a) * u * combine[:, gi, :, None]
        out = out + jnp.einsum('tef,efd->td', hid, e_down[gi])
    return out.reshape(B, S, D)


def setup_inputs(seed: int = 0) -> dict:
    key = jax.random.key(seed)
    ks = iter(jax.random.split(key, 32))
    L, D = DEPTH, D_MODEL

    def nrm(shape, scale):
        return jax.random.normal(next(ks), shape, jnp.float32) * scale

    x = nrm((BATCH, SEQ, D), 1.0)
    c = nrm((BATCH, D), 1.0)
    offsets = jax.random.randint(next(ks), (BATCH, 1), 0, 1024, dtype=jnp.int32)
    positions = offsets + jnp.arange(SEQ, dtype=jnp.int32)[None, :]
    G, E, F = N_GROUPS, EXPERTS_PER_GROUP, D_EXPERT
    return {
        "x": x, "c": c, "positions": positions,
        "ada_w": nrm((L, D, 6 * D), 0.5 * D ** -0.5),
        "ada_b": nrm((L, 6 * D), 0.02),
        "norm1_g": 1.0 + nrm((L, D), 0.02),
        "norm2_g": 1.0 + nrm((L, D), 0.02),
        "w_in": nrm((L, D, IN_COLS), D ** -0.5),
        "q_norm_g": 1.0 + nrm((L, Q_LORA_RANK), 0.02),
        "w_uq": nrm((L, Q_LORA_RANK, N_HEADS * QK_DIM), Q_LORA_RANK ** -0.5),
        "kv_norm_g": 1.0 + nrm((L, KV_LORA_RANK), 0.02),
        "w_ukv": nrm((L, KV_LORA_RANK, N_HEADS * (QK_NOPE_DIM + V_HEAD_DIM)), KV_LORA_RANK ** -0.5),
        "w_o_attn": nrm((L, ATTN_WIDTH, D), ATTN_WIDTH ** -0.5),
        "conv_w": nrm((L, CONV_KERNEL, CONV_CHANNELS), CONV_KERNEL ** -0.5),
        "conv_b": nrm((L, CONV_CHANNELS), 0.02),
        "conv_ln_g": 1.0 + nrm((L, CONV_CHANNELS), 0.02),
        "conv_ln_b": nrm((L, CONV_CHANNELS), 0.02),
        "w_conv_out": nrm((L, CONV_CHANNELS, D), CONV_CHANNELS ** -0.5),
        "w_out": nrm((L, D, D), D ** -0.5),
        "router_group_w": nrm((L, D, G), D ** -0.5),
        "router_group_b": nrm((L, G), 0.01),
        "router_expert_w": nrm((L, D, G * E), D ** -0.5),
        "router_expert_b": nrm((L, G * E), 0.01),
        "expert_w_gate": nrm((L, G, E, D, F), D ** -0.5),
        "expert_w_up": nrm((L, G, E, D, F), D ** -0.5),
        "expert_w_down": nrm((L, G, E, F, D), F ** -0.5),
        "final_norm_g": 1.0 + nrm((D,), 0.02),
    }


def reference(x, c, positions, ada_w, ada_b, norm1_g, norm2_g, w_in, q_norm_g, w_uq,
              kv_norm_g, w_ukv, w_o_attn, conv_w, conv_b, conv_ln_g, conv_ln_b,
              w_conv_out, w_out, router_group_w, router_group_b, router_expert_w,
              router_expert_b, expert_w_gate, expert_w_up, expert_w_down, final_norm_g):
    B, S, D = x.shape
    cos, sin = rope_tables(positions)
    c_act = jax.nn.silu(c)
    for l in range(DEPTH):
        ada = c_act @ ada_w[l] + ada_b[l]
        sh1, sc1, gt1, sh2, sc2, gt2 = [a[:, None, :] for a in jnp.split(ada, 6, axis=-1)]

        h = rmsnorm(x, norm1_g[l]) * (1.0 + sc1) + sh1
        z = h @ w_in[l]
        q_lat, kv_lat, k_rope, conv_in, gate_logits = jnp.split(z, SPLIT_POINTS, axis=-1)

        q = (rmsnorm(q_lat, q_norm_g[l]) @ w_uq[l]).reshape(B, S, N_HEADS, QK_DIM)
        q_nope, q_pe = jnp.split(q, [QK_NOPE_DIM], axis=-1)
        q_pe = apply_rope(q_pe, cos[:, :, None, :], sin[:, :, None, :])
        kv = (rmsnorm(kv_lat, kv_norm_g[l]) @ w_ukv[l]).reshape(B, S, N_HEADS, QK_NOPE_DIM + V_HEAD_DIM)
        k_nope, v = jnp.split(kv, [QK_NOPE_DIM], axis=-1)
        k_pe = jnp.broadcast_to(apply_rope(k_rope, cos, sin)[:, :, None, :], (B, S, N_HEADS, QK_ROPE_DIM))
        qh = jnp.concatenate([q_nope, q_pe], axis=-1).transpose(0, 2, 1, 3)
        kh = jnp.concatenate([k_nope, k_pe], axis=-1).transpose(0, 2, 1, 3)
        attn = causal_block_attention(qh, kh, v.transpose(0, 2, 1, 3), positions)
        y_attn = attn @ w_o_attn[l]

        glu_a, glu_b = jnp.split(conv_in, 2, axis=-1)
        u = glu_a * jax.nn.sigmoid(glu_b)
        u = causal_depthwise_conv(u, conv_w[l], conv_b[l])
        u = jax.nn.silu(layernorm(u, conv_ln_g[l], conv_ln_b[l]))
        y_conv = u @ w_conv_out[l]

        g_attn, g_conv = jnp.split(jax.nn.sigmoid(gate_logits), 2, axis=-1)
        y = (g_attn * y_attn + g_conv * y_conv) @ w_out[l]
        x = x + gt1 * y

        h = rmsnorm(x, norm2_g[l]) * (1.0 + sc2) + sh2
        x = x + gt2 * hierarchical_moe(h, router_group_w[l], router_group_b[l], router_expert_w[l],
                                       router_expert_b[l], expert_w_gate[l], expert_w_up[l],
                                       expert_w_down[l])
    return rmsnorm(x, final_norm_g)
```

```python
import math
from contextlib import ExitStack

import numpy as np
import ml_dtypes

import concourse.bass as bass
import concourse.mybir as mybir
from concourse.bass_utils import run_bass_kernel_spmd

F32 = mybir.dt.float32
BF16 = mybir.dt.bfloat16
I32 = mybir.dt.int32
ALU = mybir.AluOpType
AF = mybir.ActivationFunctionType
AX = mybir.AxisListType

D = 1024
NH = 8
QK = 96
NOPE = 64
ROPE = 32
VD = 64
QL = 384
KVL = 256
CC = 512
CK = 31
NG = 4
NE = 8
FE = 128
INC = 3744
S = 8192
B = 4
CH = 512
EPS = 1e-6
DEPTH = 2
TWO_PI = 2.0 * math.pi
CW1 = 6.28125
CW2 = TWO_PI - CW1

EPOCH = 12000


class Res:
    __slots__ = ("name", "w", "r", "dsem", "dcnt", "excl")

    def __init__(self, name):
        self.excl = False
        self.name = name
        self.w = {}
        self.r = {}
        self.dsem = None
        self.dcnt = 0


def _res(x):
    return x.r if isinstance(x, TL) else x


class FW:
    def __init__(self, nc, same_engine_sync=True):
        self.nc = nc
        self.eng = {"pe": nc.tensor, "act": nc.scalar, "dve": nc.vector,
                    "pool": nc.gpsimd, "sp": nc.sync}
        self.esem = {}
        self.ecnt = {}
        self.waited = {k: {} for k in self.eng}
        self.same = same_engine_sync
        self.nsem = 0
        self.all_sems = {}
        self.free_dsems = []
        self.ninst = {k: 0 for k in self.eng}
        for k in self.eng:
            self._new_epoch(k)

    def _alloc_sem(self, name):
        s = self.nc.alloc_semaphore(name)
        self.nsem += 1
        return s

    def _new_epoch(self, k):
        self.esem[k] = self._alloc_sem(f"e_{k}_{self.nsem}")
        self.ecnt[k] = 0

    def _collect(self, reads, writes, skip_waw_sem=None):
        deps = {}

        def add(tok):
            key = id(tok[0])
            if key not in deps or deps[key][1] < tok[1]:
                deps[key] = tok
        for r in reads:
            for tok in r.w.values():
                add(tok)
        for w in writes:
            for tok in w.w.values():
                if skip_waw_sem is not None and tok[0] is skip_waw_sem:
                    continue
                add(tok)
            for tok in w.r.values():
                add(tok)
        return deps

    def _wait(self, k, deps, skip_own=False):
        e = self.eng[k]
        wd = self.waited[k]
        for key, (sem, val) in deps.items():
            if skip_own and sem is self.esem[k]:
                continue
            if wd.get(key, 0) >= val:
                continue
            e.wait_ge(sem, val)
            self.ninst[k] += 1
            wd[key] = val

    def _record(self, tok, reads, writes, partial=False):
        key = id(tok[0])
        self.all_sems[key] = (tok[0], tok[1])
        for r in reads:
            r.r[key] = tok
        for w in writes:
            if partial:
                w.w[key] = tok
            else:
                w.w = {key: tok}
            w.r = {}

    def op(self, k, ins_fn, reads=(), writes=()):
        reads = [_res(x) for x in reads]
        writes = [_res(x) for x in writes]
        writes = writes + [x for x in reads if x.excl]
        reads = [x for x in reads if not x.excl]
        deps = self._collect(reads, writes)
        skip_own = (k == "pe") or (not self.same)
        self._wait(k, deps, skip_own=skip_own)
        ins = ins_fn()
        if self.ecnt[k] >= EPOCH:
            self._new_epoch(k)
        self.ecnt[k] += 1
        ins.then_inc(self.esem[k], 1)
        self.ninst[k] += 1
        tok = (self.esem[k], self.ecnt[k])
        self._record(tok, reads, writes)
        return ins

    def dma(self, q, out, in_, sb, reads=(), writes=(), **kw):
        sb = _res(sb)
        reads = [_res(x) for x in reads]
        writes = [_res(x) for x in writes]
        deps = self._collect(reads, writes, skip_waw_sem=sb.dsem)
        self._wait(q, deps)
        if sb.dsem is None or sb.dcnt >= 30000:
            if self.free_dsems:
                sb.dsem, sb.dcnt = self.free_dsems.pop()
            else:
                sb.dsem = self._alloc_sem(f"d_{sb.name}_{self.nsem}")
                sb.dcnt = 0
        sb.dcnt += 16
        ins = self.eng[q].dma_start(out=out, in_=in_, **kw)
        ins.then_inc(sb.dsem, 16)
        self.ninst[q] += 1
        tok = (sb.dsem, sb.dcnt)
        self._record(tok, reads, writes, partial=True)
        return ins

    def barrier(self):
        deps = dict(self.all_sems)
        for k in self.eng:
            self._wait(k, {kk: v for kk, v in deps.items()})


class TL:
    __slots__ = ("t", "r")

    def __init__(self, t, name, excl=False):
        self.t = t
        self.r = Res(name)
        self.r.excl = excl

    def __getitem__(self, key):
        return self.t[key]


class Phase:
    def __init__(self, nc, fw):
        self.nc = nc
        self.fw = fw
        self.es = ExitStack()
        self.n = 0
        self.tiles = []

    def sb(self, name, shape, dtype):
        self.n += 1
        t = self.es.enter_context(self.nc.sbuf_tensor(f"{name}_{id(self) % 100000}_{self.n}", list(shape), dtype))
        tl = TL(t, name)
        self.tiles.append(tl)
        return tl

    def ps(self, name, shape, dtype=F32):
        self.n += 1
        t = self.es.enter_context(self.nc.psum_tensor(f"{name}_{id(self) % 100000}_{self.n}", list(shape), dtype))
        return TL(t, name, excl=True)

    def close(self):
        self.fw.barrier()
        for tl in self.tiles:
            if tl.r.dsem is not None and tl.r.dcnt < 30000:
                self.fw.free_dsems.append((tl.r.dsem, tl.r.dcnt))
                tl.r.dsem = None
        self.es.close()


class Rot:
    def __init__(self, tiles):
        self.tiles = tiles
        self.i = 0

    def next(self):
        t = self.tiles[self.i % len(self.tiles)]
        self.i += 1
        return t


def own_chunks(npar, p):
    if npar == 1:
        return list(range(16))
    return [2 * j + ((j + p) % 2) for j in range(8)]


class Prog:
    def __init__(self, npar=2, depth=DEPTH, debug=None, stop_after=None, a_chunks=None, a_stop=99):
        self.a_chunks = a_chunks
        self.a_stop = a_stop
        self.npar = npar
        self.depth = depth
        self.debug = debug or set()
        self.stop_after = stop_after
        self.nch = 16 // npar
        self.ntok = self.nch * CH
        self.nc = bass.Bass("TRN2", target_bir_lowering=False)
        self.fw = FW(self.nc)
        self.dq = Rot(["sp"])
        self.build()

    def PE(self, fn, r=(), w=()):
        return self.fw.op("pe", fn, r, w)

    def ACT(self, fn, r=(), w=()):
        return self.fw.op("act", fn, r, w)

    def DVE(self, fn, r=(), w=()):
        return self.fw.op("dve", fn, r, w)

    def POOL(self, fn, r=(), w=()):
        return self.fw.op("pool", fn, r, w)

    def load(self, q, out_ap, in_ap, tile, dram=(), **kw):
        return self.fw.dma(q, out_ap, in_ap, tile, reads=list(dram), writes=[tile], **kw)

    def store(self, q, out_ap, in_ap, tile, dram=(), **kw):
        return self.fw.dma(q, out_ap, in_ap, tile, reads=[tile], writes=list(dram), **kw)

    def din(self, name, shape, dtype=F32):
        return self.nc.dram_tensor(name, list(shape), dtype, kind="ExternalInput").ap()

    def dscr(self, name, shape, dtype):
        kind = "ExternalOutput" if name in self.debug else "Internal"
        return self.nc.dram_tensor(name, list(shape), dtype, kind=kind).ap()

    def __getattr__(self, name):
        specs = self.__dict__.get("_specs", {})
        if name in specs:
            ap = self.din(*specs[name])
            self.__dict__[name] = ap
            self.used_inputs.append(specs[name][0])
            return ap
        raise AttributeError(name)

    def declare(self):
        self._specs = {}
        self.used_inputs = []
        L = self.depth
        nt = self.ntok
        self._specs["x_in"] = ("x_own", [nt, D])
        self._specs["pos_in"] = ("pos_own", [1, nt], I32)
        self._specs["c_col"] = ("c_col", [128, 8])
        self._specs["consts"] = ("consts", [128, 4])
        self._specs["ident_in"] = ("ident", [128, 128])
        self._specs["masks_in"] = ("masks", [2, 8, 128, CH], BF16)
        self._specs["halo_sel"] = ("halo_sel", [1, 2 * self.nch])
        self._specs["ada_w"] = ("ada_w", [L, D, 6 * D])
        self._specs["ada_b"] = ("ada_b", [L, 1, 6 * D])
        self._specs["n1g"] = ("n1g", [L, 128, 8])
        self._specs["n2g"] = ("n2g", [L, 128, 8])
        self._specs["w_in"] = ("w_in", [L, D, INC])
        self._specs["qng"] = ("qng", [L, 128, 3])
        self._specs["w_uq"] = ("w_uq", [L, QL, NH * QK])
        self._specs["kvng"] = ("kvng", [L, 128, 2])
        self._specs["w_ukv"] = ("w_ukv", [L, KVL, NH * 128])
        self._specs["w_oa"] = ("w_oa", [L, CC, D])
        self._specs["conv_w"] = ("conv_w", [L, 128, 4, CK])
        self._specs["conv_v"] = ("conv_v", [L, 128, 3, 4])
        self._specs["w_co"] = ("w_co", [L, CC, D])
        self._specs["w_out"] = ("w_out", [L, D, D])
        self._specs["w_r"] = ("w_r", [L, D, 36])
        self._specs["b_r"] = ("b_r", [L, 1, 36])
        self._specs["e_g"] = ("e_g", [L, NG, 128, 8 * NE * FE])
        self._specs["e_u"] = ("e_u", [L, NG, 128, 8 * NE * FE])
        self._specs["e_d"] = ("e_d", [L, NG, 128, NE * D])
        self._specs["fng"] = ("fng", [1, D])
        self.out = self.nc.dram_tensor("out", [nt, D], F32, kind="ExternalOutput").ap()
        self.xa = self.dscr("xa", [nt, D], F32)
        self.cs_d = self.dscr("cs_d", [2, ROPE, nt], F32)
        self.qT_d = self.dscr("qT_d", [NH * QK, nt], BF16)
        self.nparts = nt // 2048
        self.ex_d = [self.dscr(f"ex_d{i}", [KVL + ROPE, 2048], BF16) for i in range(self.nparts)]
        self.exh_d = self.dscr("exh_d", [CC, self.nch * 32], BF16)
        self.uT_d = self.dscr("uT_d", [CC, nt], BF16)
        self.gt_d = self.dscr("gt_d", [2 * D, nt], BF16)
        self.at_d = self.dscr("at_d", [NH * VD, nt], BF16)
        self.h2_d = self.dscr("h2_d", [D, nt], BF16)
        self.uc_d = self.dscr("uc_d", [CC, nt], BF16)
        if self.npar == 2:
            self.exg_d = [self.dscr(f"exg_d{i}", [2 * (KVL + ROPE), 2048], BF16) for i in range(self.nparts)]
            self.exhg_d = self.dscr("exhg_d", [2 * CC, self.nch * 32], BF16)
        n = self.nch
        self.R_x = [Res(f"Rx{j}") for j in range(n)]
        self.R_xa = [Res(f"Rxa{j}") for j in range(n)]
        self.R_cs = [Res(f"Rcs{j}") for j in range(n)]
        self.R_q = [Res(f"Rq{j}") for j in range(n)]
        self.R_ex = [Res(f"Rex{j}") for j in range(n)]
        self.R_exh = [Res(f"Rexh{j}") for j in range(n)]
        self.R_u = [Res(f"Ru{j}") for j in range(n)]
        self.R_gt = [Res(f"Rgt{j}") for j in range(n)]
        self.R_at = [Res(f"Rat{j}") for j in range(n)]
        self.R_h2 = [Res(f"Rh2{j}") for j in range(n)]
        self.R_out = [Res(f"Rout{j}") for j in range(n)]
        self.R_uc = [Res(f"Ruc{j}") for j in range(n)]
        self.R_exg = Res("Rexg")
        self.dbg = {}

    def dbg_out(self, name, shape, dtype=F32):
        ap = self.nc.dram_tensor("dbg_" + name, list(shape), dtype, kind="ExternalOutput").ap()
        self.dbg[name] = ap
        return ap

    def build(self):
        nc, fw = self.nc, self.fw
        self.declare()
        G = Phase(nc, fw)
        self.G = G
        self.ident_f = G.sb("ident_f", [128, 128], F32)
        self.ident = G.sb("ident", [128, 128], BF16)
        self.ones_f = G.sb("ones_f", [128, 128], F32)
        self.ones_b = G.sb("ones_b", [128, 128], BF16)
        self.cst = G.sb("cst", [128, 4], F32)
        self.load("sp", self.ident_f[:], self.ident_in[:, :], self.ident_f)
        self.load("sp", self.cst[:], self.consts[:, :], self.cst)
        self.DVE(lambda: nc.vector.tensor_copy(out=self.ident[:], in_=self.ident_f[:]), [self.ident_f], [self.ident])
        self.POOL(lambda: nc.gpsimd.memset(self.ones_f[:], 1.0), [], [self.ones_f])
        self.POOL(lambda: nc.gpsimd.memset(self.ones_b[:], 1.0), [], [self.ones_b])
        self.modc = G.sb("modc", [128, 4, 8], F32)
        self.gtb = G.sb("gtb", [128, 2, D], F32)
        self.comb = G.sb("comb", [128, self.ntok // 128, NG * NE], F32)
        self.cact = G.sb("cact", [128, 8], F32)
        self.load("sp", self.cact[:], self.c_col[:, :], self.cact)
        self.ACT(lambda: nc.scalar.activation(out=self.cact[:], in_=self.cact[:], func=AF.Silu), [self.cact], [self.cact])

        PR = self.rope_tables()
        if self.stop_after == "rope":
            PR.close()
            return self.finish()
        for l in range(self.depth):
            self.layer_mod(l)
            if l == 0:
                PR.close()
            if self.stop_after == "mod":
                return self.finish()
            self.phase_a(l)
            if self.stop_after == "a":
                return self.finish()
            self.exchange(l)
            self.phase_b(l)
            if self.stop_after == "b":
                return self.finish()
            self.phase_c(l)
            if self.stop_after == "c":
                return self.finish()
            self.phase_d(l)
        return self.finish()

    def finish(self):
        self.fw.barrier()
        print("ninst", self.fw.ninst, "nsem", self.fw.nsem)

    def rope_tables(self):
        nc = self.nc
        P = Phase(nc, self.fw)
        lo, hi = 64, 96
        pis = Rot([P.sb(f"pi{i}", [128, CH], I32) for i in range(2)])
        as_ = Rot([P.sb(f"a{i}", [128, CH], F32) for i in range(2)])
        t = P.sb("t", [128, CH], F32)
        ki = P.sb("ki", [128, CH], I32)
        kf = P.sb("kf", [128, CH], F32)
        r = P.sb("r", [128, CH], F32)
        m = P.sb("m", [128, CH], F32)
        os_ = Rot([P.sb(f"o{i}", [128, CH], F32) for i in range(2)])
        V = nc.vector
        for j in range(self.nch):
            sl = slice(j * CH, (j + 1) * CH)
            pi = pis.next()
            a = as_.next()
            self.load("sp", pi[lo:hi, :], self.pos_in[0:1, sl].partition_broadcast(32), pi)
            self.DVE(lambda: V.tensor_copy(out=a[lo:hi, :], in_=pi[lo:hi, :]), [pi], [a])
            self.DVE(lambda: V.tensor_scalar(out=a[lo:hi, :], in0=a[lo:hi, :], scalar1=self.cst[lo:hi, 0:1],
                                             scalar2=None, op0=ALU.mult), [a, self.cst], [a])
            for ti, ph in ((1, 0.0), (0, 0.5 * math.pi)):
                o = os_.next()
                self.DVE(lambda: V.tensor_scalar(out=t[lo:hi, :], in0=a[lo:hi, :], scalar1=ph, scalar2=1.0 / TWO_PI,
                                                 op0=ALU.add, op1=ALU.mult), [a], [t])
                self.DVE(lambda: V.tensor_copy(out=ki[lo:hi, :], in_=t[lo:hi, :]), [t], [ki])
                self.DVE(lambda: V.tensor_copy(out=kf[lo:hi, :], in_=ki[lo:hi, :]), [ki], [kf])
                self.DVE(lambda: V.scalar_tensor_tensor(out=r[lo:hi, :], in0=kf[lo:hi, :], scalar=-CW1, in1=a[lo:hi, :],
                                                        op0=ALU.mult, op1=ALU.add), [kf, a], [r])
                self.DVE(lambda: V.tensor_scalar(out=r[lo:hi, :], in0=r[lo:hi, :], scalar1=ph, scalar2=None,
                                                 op0=ALU.add), [r], [r])
                self.DVE(lambda: V.scalar_tensor_tensor(out=r[lo:hi, :], in0=kf[lo:hi, :], scalar=-CW2, in1=r[lo:hi, :],
                                                        op0=ALU.mult, op1=ALU.add), [kf, r], [r])
                self.DVE(lambda: V.tensor_scalar(out=m[lo:hi, :], in0=r[lo:hi, :], scalar1=math.pi, scalar2=None,
                                                 op0=ALU.is_gt), [r], [m])
                self.DVE(lambda: V.scalar_tensor_tensor(out=r[lo:hi, :], in0=m[lo:hi, :], scalar=-TWO_PI, in1=r[lo:hi, :],
                                                        op0=ALU.mult, op1=ALU.add), [m, r], [r])
                self.DVE(lambda: V.tensor_scalar(out=m[lo:hi, :], in0=r[lo:hi, :], scalar1=-math.pi, scalar2=None,
                                                 op0=ALU.is_lt), [r], [m])
                self.DVE(lambda: V.scalar_tensor_tensor(out=r[lo:hi, :], in0=m[lo:hi, :], scalar=TWO_PI, in1=r[lo:hi, :],
                                                        op0=ALU.mult, op1=ALU.add), [m, r], [r])
                self.DVE(lambda: V.tensor_scalar(out=r[lo:hi, :], in0=r[lo:hi, :], scalar1=-math.pi, scalar2=math.pi,
                                                 op0=ALU.max, op1=ALU.min), [r], [r])
                if ti == 1:
                    self.ACT(lambda: nc.scalar.activation(out=o[lo:hi, :], in_=r[lo:hi, :], func=AF.Sin,
                                                          scale=self.cst[lo:hi, 1:2]), [r, self.cst], [o])
                else:
                    self.ACT(lambda: nc.scalar.activation(out=o[lo:hi, :], in_=r[lo:hi, :], func=AF.Sin), [r], [o])
                self.store("act", self.cs_d[ti, :, sl], o[lo:hi, :], o, dram=[self.R_cs[j]])
        return P

    def layer_mod(self, l):
        nc = self.nc
        P = Phase(nc, self.fw)
        row = P.sb("adarow", [1, 6 * D], F32)
        brow = P.sb("adab", [1, 6 * D], F32)
        self.load("sp", brow[:], self.ada_b[l, :, :], brow)
        wv = self.ada_w[l].rearrange("(c p) n -> p c n", p=128)
        wts = Rot([P.sb(f"adaw{i}", [128, 8, 512], F32) for i in range(2)])
        pss = Rot([P.ps(f"adaps{i}", [128, 512]) for i in range(2)])
        for n in range(12):
            wt = wts.next()
            ps = pss.next()
            self.load(self.dq.next(), wt[:], wv[:, :, n * 512:(n + 1) * 512], wt)
            for c in range(8):
                self.PE(lambda c=c: nc.tensor.matmul(ps[0:1, :], lhsT=self.cact[:, c:c + 1], rhs=wt[:, c, :],
                                                     start=(c == 0), stop=(c == 7)), [self.cact, wt], [ps])
            self.DVE(lambda: nc.vector.tensor_tensor(out=row[:, n * 512:(n + 1) * 512], in0=ps[0:1, :],
                                                      in1=brow[:, n * 512:(n + 1) * 512], op=ALU.add), [ps, brow], [row])
        g1 = P.sb("g1", [128, 8], F32)
        g2 = P.sb("g2", [128, 8], F32)
        self.load("sp", g1[:], self.n1g[l], g1)
        self.load("sp", g2[:], self.n2g[l], g2)
        pc = P.ps("pc", [128, 128, 4])
        for vi, off in enumerate((0, 1, 3, 4)):
            for c in range(8):
                col = off * D + c * 128
                self.PE(lambda vi=vi, c=c, col=col: nc.tensor.matmul(pc[:, vi * 8 + c, 0:1], lhsT=row[0:1, col:col + 128],
                                                                     rhs=self.ones_f[0:1, 0:1], start=True, stop=True),
                        [row, self.ones_f], [pc])
        cols = P.sb("cols", [128, 4, 8], F32)
        self.DVE(lambda: nc.vector.tensor_copy(out=cols[:].rearrange("p a b -> p (a b)"), in_=pc[:, 0:32, 0]), [pc], [cols])
        for k, (g, sci, shi) in enumerate(((g1, 1, 0), (g2, 3, 2))):
            self.DVE(lambda g=g, sci=sci, k=k: nc.vector.scalar_tensor_tensor(
                out=self.modc[:, 2 * k, :], in0=cols[:, sci, :], scalar=1.0, in1=g[:], op0=ALU.add, op1=ALU.mult),
                [cols, g], [self.modc])
            self.DVE(lambda shi=shi, k=k: nc.vector.tensor_copy(out=self.modc[:, 2 * k + 1, :], in_=cols[:, shi, :]),
                     [cols], [self.modc])
        pbs = Rot([P.ps(f"pbb{i}", [128, 512]) for i in range(2)])
        for k, off in enumerate((2, 5)):
            for hf in range(2):
                pbb = pbs.next()
                col = off * D + hf * 512
                self.PE(lambda: nc.tensor.matmul(pbb[:], lhsT=self.ones_f[0:1, :], rhs=row[0:1, col:col + 512],
                                                 start=True, stop=True), [row, self.ones_f], [pbb])
                self.ACT(lambda: nc.scalar.copy(out=self.gtb[:, k, hf * 512:(hf + 1) * 512], in_=pbb[:]),
                         [pbb], [self.gtb])
        if "mod" in self.debug and l == 0:
            d1 = self.dbg_out("modc", [128, 32])
            d2 = self.dbg_out("gtb", [128, 2 * D])
            self.store("sp", d1[:, :], self.modc[:].rearrange("p a b -> p (a b)"), self.modc)
            self.store("sp", d2[:, :], self.gtb[:].rearrange("p a b -> p (a b)"), self.gtb)
        P.close()

    def norm_hT(self, W, xs, k, hT):
        nc = self.nc
        ss = W["ss"].next()
        for s_ in range(4):
            junk = W["junk"].next()
            self.ACT(lambda: nc.scalar.activation(out=junk[:], in_=xs[s_][:], func=AF.Square,
                                                  accum_out=ss[:, s_:s_ + 1]), [xs[s_]], [junk, ss])
        rstd = W["rstd"].next()
        self.DVE(lambda: nc.vector.tensor_scalar(out=rstd[:], in0=ss[:], scalar1=1.0 / D, scalar2=EPS,
                                                  op0=ALU.mult, op1=ALU.add), [ss], [rstd])
        self.ACT(lambda: nc.scalar.activation(out=rstd[:], in_=rstd[:], func=AF.Sqrt), [rstd], [rstd])
        self.DVE(lambda: nc.vector.reciprocal(out=rstd[:], in_=rstd[:]), [rstd], [rstd])
        xn = W["xn"].next()
        for s_ in range(4):
            self.DVE(lambda: nc.vector.tensor_scalar(out=xn[:, s_, :], in0=xs[s_][:], scalar1=rstd[:, s_:s_ + 1],
                                                      scalar2=None, op0=ALU.mult), [xs[s_], rstd], [xn])
        for c in range(8):
            pT = W["pT"].next()
            for s_ in range(4):
                self.PE(lambda: nc.tensor.transpose(out=pT[:, s_ * 128:(s_ + 1) * 128],
                                                    in_=xn[:, s_, c * 128:(c + 1) * 128], identity=self.ident[:]),
                        [xn, self.ident], [pT])
            if c % 2 == 0:
                self.DVE(lambda: nc.vector.tensor_scalar(out=hT[:, c, :], in0=pT[:, 0:CH], scalar1=self.modc[:, 2 * k, c:c + 1],
                                                          scalar2=self.modc[:, 2 * k + 1, c:c + 1], op0=ALU.mult, op1=ALU.add),
                         [pT, self.modc], [hT])
            else:
                self.ACT(lambda: nc.scalar.activation(out=hT[:, c, :], in_=pT[:, 0:CH], func=AF.Identity,
                                                      scale=self.modc[:, 2 * k, c:c + 1], bias=self.modc[:, 2 * k + 1, c:c + 1]),
                         [pT, self.modc], [hT])

    def rstd_bcast(self, ps, n, out, tmp):
        nc = self.nc
        self.DVE(lambda: nc.vector.tensor_scalar(out=out[:], in0=ps[:], scalar1=1.0 / n, scalar2=EPS,
                                                  op0=ALU.mult, op1=ALU.add), [ps], [out])
        self.ACT(lambda: nc.scalar.activation(out=out[:], in_=out[:], func=AF.Sqrt), [out], [out])
        self.DVE(lambda: nc.vector.reciprocal(out=out[:], in_=out[:]), [out], [out])

    def phase_a(self, l):
        nc = self.nc
        P = Phase(nc, self.fw)
        x_src = self.x_in if l == 0 else self.xa
        R_src = self.R_x if l == 0 else self.R_xa
        scale = QK ** -0.5
        wv = self.w_in[l].rearrange("(c p) n -> p c n", p=128)
        WSPL = [0, QL + KVL + ROPE, QL + KVL + ROPE + 2 * CC, INC]
        wparts = []
        for i in range(3):
            wt_ = P.sb(f"w_in{i}", [128, 8, WSPL[i + 1] - WSPL[i]], BF16)
            self.load("pool", wt_[:], wv[:, :, WSPL[i]:WSPL[i + 1]], wt_)
            wparts.append(wt_)
        w_in = wparts[0]
        wkr = P.sb("wkr", [128, 8, 2, QK], BF16)
        self.POOL(lambda: nc.gpsimd.memset(wkr[:], 0.0), [], [wkr])
        KR0 = QL + KVL
        self.POOL(lambda: nc.gpsimd.tensor_copy(out=wkr[:, :, 0, 64:96], in_=w_in[:, :, KR0:KR0 + 32]), [w_in], [wkr])
        self.POOL(lambda: nc.gpsimd.tensor_copy(out=wkr[:, :, 1, 64:80], in_=w_in[:, :, KR0 + 16:KR0 + 32]), [w_in], [wkr])
        self.POOL(lambda: nc.gpsimd.tensor_copy(out=wkr[:, :, 1, 80:96], in_=w_in[:, :, KR0:KR0 + 16]), [w_in], [wkr])
        stg = P.sb("stg", [128, 3, NH * QK], F32)
        qg = P.sb("qg", [128, 3], F32)
        self.load("sp", stg[:], self.w_uq[l].rearrange("(c p) n -> p c n", p=128), stg)
        self.load("sp", qg[:], self.qng[l], qg)
        w_uq = P.sb("w_uq", [128, 3, NH * QK], BF16)
        w_uqr = P.sb("w_uqr", [128, 3, NH, QK], BF16)
        self.POOL(lambda: nc.gpsimd.memset(w_uqr[:], 0.0), [], [w_uqr])
        for c in range(3):
            self.DVE(lambda: nc.vector.tensor_scalar(out=w_uq[:, c, :], in0=stg[:, c, :], scalar1=qg[:, c:c + 1],
                                                      scalar2=None, op0=ALU.mult), [stg, qg], [w_uq])
            v = w_uq[:, c, :].rearrange("p (h r) -> p h r", r=QK)
            self.POOL(lambda: nc.gpsimd.tensor_copy(out=w_uqr[:, c, :, 64:80], in_=v[:, :, 80:96]), [w_uq], [w_uqr])
            self.POOL(lambda: nc.gpsimd.tensor_copy(out=w_uqr[:, c, :, 80:96], in_=v[:, :, 64:80]), [w_uq], [w_uqr])
        W = {
            "ss": Rot([P.sb(f"ss{i}", [128, 4], F32) for i in range(2)]),
            "rstd": Rot([P.sb(f"rstd{i}", [128, 4], F32) for i in range(2)]),
            "junk": Rot([P.sb("junk", [128, D], BF16)]),
            "xn": Rot([P.sb("xn", [128, 4, D], BF16)]),
            "pT": Rot([P.ps(f"pT{i}", [128, 2 * CH], BF16) for i in range(2)]),
        }
        xts = Rot([P.sb(f"xt{i}", [128, D], F32) for i in range(5)])
        hTs = [P.sb(f"hT{i}", [128, 8, CH], BF16) for i in range(2)]
        mps = Rot([P.ps(f"mps{i}", [128, CH]) for i in range(4)])
        ps_sq = P.ps("ps_sq", [128, CH])
        ps_skv = P.ps("ps_skv", [128, CH])
        sqt = P.sb("sqt", [128, 5, CH], BF16)
        qlT = P.sb("qlT", [128, 3, CH], BF16)
        kvraw = P.sb("kvraw", [128, 2, CH], F32)
        csts = Rot([P.sb(f"cst{i}", [128, 2, CH], F32) for i in range(2)])
        for t in csts.tiles:
            self.POOL(lambda: nc.gpsimd.memset(t[0:64, 0, :], 1.0), [], [t])
            self.POOL(lambda: nc.gpsimd.memset(t[0:64, 1, :], 0.0), [], [t])
        t1s = Rot([P.sb(f"t1{i}", [128, CH], F32) for i in range(2)])
        t2s = Rot([P.sb(f"t2{i}", [128, CH], F32) for i in range(2)])
        kro = P.sb("kro", [128, CH], BF16)
        sg = P.sb("sg", [128, 4, CH], F32)
        uT = P.sb("uT", [128, 4, CH], BF16)
        rq = P.sb("rq", [128, CH], F32)
        rkv = P.sb("rkv", [128, CH], F32)
        kvn = P.sb("kvn", [128, 2, CH], BF16)
        Cp = P.sb("Cp", [128, CH], F32)
        Sp = P.sb("Sp", [128, CH], F32)
        gts = Rot([P.sb(f"gts{i}", [128, 4, CH], BF16) for i in range(2)])
        qTc = P.sb("qTc", [128, NH, CH], BF16)

        cur = {}

        def proj(ps, lhs_fn, M, wt):
            hT = cur["hT"]
            for c in range(8):
                self.PE(lambda: nc.tensor.matmul(ps[0:M, :], lhsT=lhs_fn(c), rhs=hT[:, c, :],
                                                 start=(c == 0), stop=(c == 7)), [wt, hT], [ps])

        def wpart(off):
            i = 0 if off < WSPL[1] else (1 if off < WSPL[2] else 2)
            return wparts[i], off - WSPL[i]

        def colblk(off):
            wt_, o_ = wpart(off)
            return lambda c: wt_[:, c, o_:o_ + 128]

        nchunks = self.a_chunks or self.nch
        prepped = {}

        def prep(j):
            sl = slice(j * CH, (j + 1) * CH)
            xs = []
            for s_ in range(4):
                xt = xts.next()
                self.load(self.dq.next(), xt[:], x_src[j * CH + s_ * 128: j * CH + (s_ + 1) * 128, :], xt, dram=[R_src[j]])
                xs.append(xt)
            cst_c = csts.next()
            self.load("sp", cst_c[64:96, 0, :], self.cs_d[0, :, sl], cst_c, dram=[self.R_cs[j]])
            self.load("sp", cst_c[64:96, 1, :], self.cs_d[1, :, sl], cst_c, dram=[self.R_cs[j]])
            self.norm_hT(W, xs, 0, hTs[j % 2])
            prepped[j] = cst_c

        prep(0)
        for j in range(nchunks):
            sl = slice(j * CH, (j + 1) * CH)
            cur["hT"] = hTs[j % 2]
            cst_c = prepped.pop(j)
            for i in range(3):
                ps = mps.next()
                proj(ps, colblk(i * 128), 128, wpart(i * 128)[0])
                self.ACT(lambda: nc.scalar.activation(out=sqt[:, i, :], in_=ps[:], func=AF.Square), [ps], [sqt])
                self.DVE(lambda: nc.vector.tensor_copy(out=qlT[:, i, :], in_=ps[:]), [ps], [qlT])
            for i in range(2):
                ps = mps.next()
                proj(ps, colblk(QL + i * 128), 128, wpart(QL + i * 128)[0])
                self.ACT(lambda: nc.scalar.activation(out=sqt[:, 3 + i, :], in_=ps[:], func=AF.Square), [ps], [sqt])
                self.DVE(lambda: nc.vector.tensor_copy(out=kvraw[:, i, :], in_=ps[:]), [ps], [kvraw])
            ps_k = mps.next()
            proj(ps_k, lambda c: wkr[:, c, 0, :], QK, wkr)
            ps_kr = mps.next()
            proj(ps_kr, lambda c: wkr[:, c, 1, :], QK, wkr)
            t1 = t1s.next()
            t2 = t2s.next()
            self.DVE(lambda: nc.vector.tensor_tensor(out=t1[64:96, :], in0=ps_k[64:96, :], in1=cst_c[64:96, 0, :], op=ALU.mult),
                     [ps_k, cst_c], [t1])
            self.DVE(lambda: nc.vector.tensor_tensor(out=t2[64:96, :], in0=ps_kr[64:96, :], in1=cst_c[64:96, 1, :], op=ALU.mult),
                     [ps_kr, cst_c], [t2])
            self.POOL(lambda: nc.gpsimd.tensor_tensor(out=kro[64:96, :], in0=t1[64:96, :], in1=t2[64:96, :], op=ALU.add),
                      [t1, t2], [kro])
            exp_, eo = self.ex_d[j // 4], (j % 4) * CH
            self.store("pool", exp_[KVL:KVL + ROPE, eo:eo + CH], kro[64:96, :], kro, dram=[self.R_ex[j]])
            C0 = QL + KVL + ROPE
            for i in range(4):
                ps = mps.next()
                proj(ps, colblk(C0 + CC + i * 128), 128, wpart(C0 + CC + i * 128)[0])
                self.ACT(lambda: nc.scalar.activation(out=sg[:, i, :], in_=ps[:], func=AF.Sigmoid), [ps], [sg])
            for i in range(4):
                ps = mps.next()
                proj(ps, colblk(C0 + i * 128), 128, wpart(C0 + i * 128)[0])
                self.DVE(lambda: nc.vector.tensor_tensor(out=uT[:, i, :], in0=ps[:], in1=sg[:, i, :], op=ALU.mult),
                         [ps, sg], [uT])
            self.store("pool", self.uT_d[:, sl].rearrange("(i p) t -> p i t", p=128), uT[:], uT, dram=[self.R_u[j]])
            self.store("pool", self.exh_d[:, j * 32:(j + 1) * 32].rearrange("(i p) t -> p i t", p=128), uT[:, :, CH - 32:CH],
                       uT, dram=[self.R_exh[j]])
            if j + 1 < nchunks:
                prep(j + 1)
            for i in range(3):
                self.PE(lambda: nc.tensor.matmul(ps_sq[:], lhsT=self.ones_b[:], rhs=sqt[:, i, :], start=(i == 0), stop=(i == 2)),
                        [self.ones_b, sqt], [ps_sq])
            for i in range(2):
                self.PE(lambda: nc.tensor.matmul(ps_skv[:], lhsT=self.ones_b[:], rhs=sqt[:, 3 + i, :], start=(i == 0), stop=(i == 1)),
                        [self.ones_b, sqt], [ps_skv])
            self.rstd_bcast(ps_sq, QL, rq, None)
            self.rstd_bcast(ps_skv, KVL, rkv, None)
            for i in range(2):
                self.DVE(lambda: nc.vector.tensor_tensor(out=kvn[:, i, :], in0=kvraw[:, i, :], in1=rkv[:], op=ALU.mult),
                         [kvraw, rkv], [kvn])
            self.store("pool", exp_[0:KVL, eo:eo + CH].rearrange("(i p) t -> p i t", p=128), kvn[:], kvn, dram=[self.R_ex[j]])
            self.DVE(lambda: nc.vector.scalar_tensor_tensor(out=Cp[0:QK, :], in0=cst_c[0:QK, 0, :], scalar=scale, in1=rq[0:QK, :],
                                                             op0=ALU.mult, op1=ALU.mult), [cst_c, rq], [Cp])
            self.DVE(lambda: nc.vector.scalar_tensor_tensor(out=Sp[0:QK, :], in0=cst_c[0:QK, 1, :], scalar=scale, in1=rq[0:QK, :],
                                                             op0=ALU.mult, op1=ALU.mult), [cst_c, rq], [Sp])
            G0 = C0 + 2 * CC
            for i in range(16):
                if i % 4 == 0:
                    gt = gts.next()
                ps = mps.next()
                proj(ps, colblk(G0 + i * 128), 128, wpart(G0 + i * 128)[0])
                self.ACT(lambda: nc.scalar.activation(out=gt[:, i % 4, :], in_=ps[:], func=AF.Sigmoid), [ps], [gt])
                if i % 4 == 3:
                    r0 = (i - 3) * 128
                    self.store("act", self.gt_d[r0:r0 + 512, sl].rearrange("(i p) t -> p i t", p=128), gt[:], gt,
                               dram=[self.R_gt[j]])
            for h in range(NH):
                ps_q = mps.next()
                ps_r = mps.next()
                for c in range(3):
                    self.PE(lambda: nc.tensor.matmul(ps_q[0:QK, :], lhsT=w_uq[:, c, h * QK:(h + 1) * QK], rhs=qlT[:, c, :],
                                                     start=(c == 0), stop=(c == 2)), [w_uq, qlT], [ps_q])
                for c in range(3):
                    self.PE(lambda: nc.tensor.matmul(ps_r[0:QK, :], lhsT=w_uqr[:, c, h, :], rhs=qlT[:, c, :],
                                                     start=(c == 0), stop=(c == 2)), [w_uqr, qlT], [ps_r])
                t1 = t1s.next()
                t2 = t2s.next()
                self.DVE(lambda: nc.vector.tensor_tensor(out=t1[0:QK, :], in0=ps_q[0:QK, :], in1=Cp[0:QK, :], op=ALU.mult),
                         [ps_q, Cp], [t1])
                self.DVE(lambda: nc.vector.tensor_tensor(out=t2[0:QK, :], in0=ps_r[0:QK, :], in1=Sp[0:QK, :], op=ALU.mult),
                         [ps_r, Sp], [t2])
                self.POOL(lambda: nc.gpsimd.tensor_tensor(out=qTc[0:QK, h, :], in0=t1[0:QK, :], in1=t2[0:QK, :], op=ALU.add),
                          [t1, t2], [qTc])
            self.store("pool", self.qT_d[:, sl].rearrange("(h r) t -> r h t", r=QK), qTc[0:QK, :, :], qTc, dram=[self.R_q[j]])
        P.close()


    def gmap(self):
        if self.npar == 1:
            return {g: (0, g) for g in range(16)}
        m = {}
        for p in range(2):
            for j, g in enumerate(own_chunks(2, p)):
                m[g] = (p, j)
        return m

    def exchange(self, l):
        nc = self.nc
        if self.npar == 1:
            self.exsrc, self.exhsrc = self.ex_d, self.exh_d
            self.R_exsrc = lambda g: [self.R_ex[g]]
            self.R_exhsrc = lambda g: [self.R_exh[g]]
            return
        fw = self.fw
        fw.barrier()
        groups = [[0, 1], [2, 3], [4, 5], [6, 7]]
        cc_sem = fw._alloc_sem(f"cc{l}")
        n = 0
        for i in range(self.nparts):
            nc.gpsimd.collective_compute("AllGather", ALU.bypass, replica_groups=groups,
                                         ins=[self.ex_d[i][:, :]], outs=[self.exg_d[i][:, :]]).then_inc(cc_sem, 1)
            n += 1
        nc.gpsimd.collective_compute("AllGather", ALU.bypass, replica_groups=groups,
                                     ins=[self.exh_d[:, :]], outs=[self.exhg_d[:, :]]).then_inc(cc_sem, 1)
        n += 1
        fw.all_sems[id(cc_sem)] = (cc_sem, n)
        fw.barrier()
        self.exsrc, self.exhsrc = self.exg_d, self.exhg_d
        self.R_exsrc = lambda g: []
        self.R_exhsrc = lambda g: []

    def phase_b(self, l):
        nc = self.nc
        P = Phase(nc, self.fw)
        gm = self.gmap()
        EXR = KVL + ROPE
        stg = P.sb("stgkv", [128, 2, NH * 128], F32)
        kg = P.sb("kg", [128, 2], F32)
        self.load("sp", stg[:], self.w_ukv[l].rearrange("(c p) n -> p c n", p=128), stg)
        self.load("sp", kg[:], self.kvng[l], kg)
        w_ukv = P.sb("w_ukv", [128, 2, NH * 128], BF16)
        for c in range(2):
            self.DVE(lambda: nc.vector.tensor_scalar(out=w_ukv[:, c, :], in0=stg[:, c, :], scalar1=kg[:, c:c + 1],
                                                      scalar2=None, op0=ALU.mult), [stg, kg], [w_ukv])
        mk = P.sb("mk", [128, 16, CH], BF16)
        self.load("sp", mk[:], self.masks_in.rearrange("a r p f -> p (a r) f"), mk)
        kvT = P.sb("kvT", [128, 2, S], BF16)
        kTs = [P.sb(f"kT{i}", [128, S], BF16) for i in range(2)]
        nloc = self.nch

        def kcol(g):
            p_, m_ = gm[g]
            return (p_ * nloc + m_) * CH
        for p_ in range(self.npar):
            for i in range(self.nparts):
                ext = self.exsrc[i]
                dst = slice((p_ * nloc + 4 * i) * CH, (p_ * nloc + 4 * i + 4) * CH)
                rr = [r_ for m_ in range(4 * i, 4 * i + 4) for r_ in self.R_exsrc(m_)]
                for c in range(2):
                    self.load("sp", kvT[:, c, dst], ext[p_ * EXR + c * 128: p_ * EXR + (c + 1) * 128, :], kvT, dram=rr)
                for kT in kTs:
                    self.load("sp", kT[64:96, dst], ext[p_ * EXR + KVL: p_ * EXR + EXR, :], kT, dram=rr)
        vaugs = [P.sb(f"vaug{i}", [128, S // 128, 2 * VD], BF16) for i in range(2)]
        for v in vaugs:
            self.POOL(lambda: nc.gpsimd.memset(v[:, :, VD:2 * VD], 1.0), [], [v])
        qTs = [P.sb(f"qT{i}", [128, self.ntok], BF16) for i in range(2)]
        pts = Rot([P.sb(f"pt{i}", [128, CH], BF16) for i in range(6)])
        scs = Rot([P.ps(f"sc{i}", [128, CH]) for i in range(3)])
        pos_ = Rot([P.ps(f"po{i}", [128, CH]) for i in range(2)])
        blds = Rot([P.ps(f"bld{i}", [128, CH]) for i in range(2)])
        obs = Rot([P.sb(f"ob{i}", [128, CH], F32) for i in range(2)])
        lrows = Rot([P.sb(f"lrow{i}", [128, CH], F32) for i in range(2)])
        rlss = Rot([P.sb(f"rls{i}", [128, CH], F32) for i in range(2)])
        atts = Rot([P.sb(f"att{i}", [128, CH], BF16) for i in range(2)])
        nkt_per = 4 * self.npar
        ev = 0
        def build_groups(h):
            kT, vaug = kTs[h % 2], vaugs[h % 2]
            gl = []

            def kgrp(s16):
                ps = blds.next()
                sl = slice(s16 * CH, (s16 + 1) * CH)
                for c in range(2):
                    self.PE(lambda: nc.tensor.matmul(ps[0:64, :], lhsT=w_ukv[:, c, h * 128:h * 128 + 64], rhs=kvT[:, c, sl],
                                                     start=(c == 0), stop=(c == 1)), [w_ukv, kvT], [ps])
                self.DVE(lambda: nc.vector.tensor_copy(out=kT[0:64, sl], in_=ps[0:64, :]), [ps], [kT])

            def vgrp(g8):
                ps = blds.next()
                for i in range(8):
                    kt = g8 * 8 + i
                    for c in range(2):
                        self.PE(lambda: nc.tensor.matmul(ps[:, i * 64:(i + 1) * 64], lhsT=kvT[:, c, kt * 128:(kt + 1) * 128],
                                                         rhs=w_ukv[:, c, h * 128 + 64:h * 128 + 128],
                                                         start=(c == 0), stop=(c == 1)), [w_ukv, kvT], [ps])
                src = ps[:, :].rearrange("p (i d) -> p i d", d=64)
                self.DVE(lambda: nc.vector.tensor_copy(out=vaug[:, g8 * 8:(g8 + 1) * 8, 0:VD], in_=src), [ps], [vaug])
            for s16 in range(16):
                gl.append(lambda s16=s16: kgrp(s16))
            for g8 in range(8):
                gl.append(lambda g8=g8: vgrp(g8))
            return gl

        for g_ in build_groups(0):
            g_()
        self.load("sp", qTs[0][0:QK, :], self.qT_d[0:QK, :], qTs[0], dram=self.R_q)
        for h in range(NH):
            kT, vaug, qT = kTs[h % 2], vaugs[h % 2], qTs[h % 2]
            if h + 1 < NH:
                self.load("sp", qTs[(h + 1) % 2][0:QK, :], self.qT_d[(h + 1) * QK:(h + 2) * QK, :], qTs[(h + 1) % 2], dram=self.R_q)
                pending = build_groups(h + 1)
            else:
                pending = []
            blocks = [(j, kt) for j in range(self.nch) for kt in range(nkt_per * (j + 1))]
            every = max(1, len(blocks) // 26)
            sc_of = {}
            po_of = {}
            nq = [0]

            def qk_ahead(upto):
                while nq[0] < min(upto, len(blocks)):
                    j, kt = blocks[nq[0]]
                    sc = scs.next()
                    kc = kcol(kt // 4) + (kt % 4) * 128
                    self.PE(lambda: nc.tensor.matmul(sc[:], lhsT=kT[0:QK, kc:kc + 128], rhs=qT[0:QK, j * CH:(j + 1) * CH],
                                                     start=True, stop=True), [kT, qT], [sc])
                    sc_of[nq[0]] = sc
                    nq[0] += 1

            tails = []

            def tail2(j, po, ob, rls):
                att = atts.next()
                self.DVE(lambda: nc.vector.tensor_tensor(out=att[0:VD, :], in0=ob[0:VD, :], in1=rls[0:VD, :], op=ALU.mult),
                         [ob, rls], [att])
                self.store("sp", self.at_d[h * VD:(h + 1) * VD, j * CH:(j + 1) * CH], att[0:VD, :], att, dram=[self.R_at[j]])

            for idx, (j, kt) in enumerate(blocks):
                nkt = nkt_per * (j + 1)
                qk_ahead(idx + 3)
                if kt == 0:
                    po_of[j] = pos_.next()
                po = po_of[j]
                sc = sc_of.pop(idx)
                pt = pts.next()
                self.ACT(lambda: nc.scalar.activation(out=pt[:], in_=sc[:], func=AF.Exp), [sc], [pt])
                r = kt - nkt_per * j
                if r >= 0:
                    mi = (j % 2) * 8 + r if self.npar == 2 else r
                    if r % 3 == 2:
                        self.POOL(lambda: nc.gpsimd.tensor_tensor(out=pt[:], in0=pt[:], in1=mk[:, mi, :], op=ALU.mult),
                                  [pt, mk], [pt])
                    else:
                        self.DVE(lambda: nc.vector.tensor_tensor(out=pt[:], in0=pt[:], in1=mk[:, mi, :], op=ALU.mult),
                                 [pt, mk], [pt])
                kvi = (kcol(kt // 4) + (kt % 4) * 128) // 128
                self.PE(lambda: nc.tensor.matmul(po[:], lhsT=vaug[:, kvi, :], rhs=pt[:],
                                                 start=(kt == 0), stop=(kt == nkt - 1)), [vaug, pt], [po])
                if pending and idx % every == every - 1:
                    pending.pop(0)()
                if tails and (idx >= tails[0][0] + 4):
                    _, a_ = tails.pop(0)
                    tail2(*a_)
                if kt == nkt - 1:
                    ob, lrow, rls = obs.next(), lrows.next(), rlss.next()
                    self.DVE(lambda: nc.vector.tensor_copy(out=ob[0:VD, :], in_=po[0:VD, :]), [po], [ob])
                    self.DVE(lambda: nc.vector.reciprocal(out=lrow[VD:2 * VD, :], in_=po[VD:2 * VD, :]), [po], [lrow])
                    self.fw.dma("sp", rls[0:VD, :], lrow[VD:2 * VD, :], rls, reads=[lrow], writes=[rls])
                    tails.append((idx, (j, po, ob, rls)))
            for _, a_ in tails:
                tail2(*a_)
            for g_ in pending:
                g_()
        P.close()


    def phase_c1(self, l, PW):
        nc = self.nc
        P = Phase(nc, self.fw)
        gm = self.gmap()
        cw = P.sb("cw", [128, 4, CK], F32)
        cv = P.sb("cv", [128, 3, 4], F32)
        self.load("sp", cw[:], self.conv_w[l], cw)
        self.load("sp", cv[:], self.conv_v[l], cv)
        diagw = P.sb("diagw", [128, 4, CK, 128], BF16)
        for blk in range(4):
            self.DVE(lambda: nc.vector.tensor_tensor(out=diagw[:, blk, :, :],
                                                      in0=self.ident_f[:].unsqueeze(1).to_broadcast([128, CK, 128]),
                                                      in1=cw[:, blk, :].unsqueeze(2).to_broadcast([128, CK, 128]), op=ALU.mult),
                     [self.ident_f, cw], [diagw])
        w_co, w_oa, w_out, w_r, brow = self.c2w
        self.load("pool", w_co[:], self.w_co[l].rearrange("(i p) n -> p i n", p=128), w_co)
        self.load("pool", w_oa[:], self.w_oa[l].rearrange("(i p) n -> p i n", p=128), w_oa)
        self.load("pool", w_out[:], self.w_out[l].rearrange("(i p) n -> p i n", p=128), w_out)
        self.load("pool", w_r[:], self.w_r[l].rearrange("(i p) n -> p i n", p=128), w_r)
        self.load("sp", brow[:], self.b_r[l], brow)
        if self.npar == 2:
            hs = P.sb("hs", [128, 2 * self.nch], F32)
            self.load("sp", hs[:], self.halo_sel[0:1, :].partition_broadcast(128), hs)
            has = Rot([P.sb(f"ha{i}", [128, 4, 32], BF16) for i in range(3)])
            hbs = Rot([P.sb(f"hb{i}", [128, 4, 32], BF16) for i in range(3)])
            htmp = P.sb("htmp", [128, 4, 32], F32)
        uhs = Rot([P.sb(f"uh{i}", [128, 4, 32 + CH], BF16) for i in range(3)])
        cps = Rot([P.ps(f"cps{i}", [128, CH]) for i in range(4)])
        ps_mu = P.ps("ps_mu", [128, CH])
        ps_m2 = P.ps("ps_m2", [128, CH])
        ycs = Rot([P.sb(f"yc{i}", [128, 4, CH], F32) for i in range(2)])
        ycbs = Rot([P.sb(f"ycb{i}", [128, 4, CH], BF16) for i in range(2)])
        sqbs = Rot([P.sb(f"sqb{i}", [128, 4, CH], BF16) for i in range(2)])
        mu = P.sb("mu", [128, CH], F32)
        msq = P.sb("msq", [128, CH], F32)
        rs = P.sb("rs", [128, CH], F32)
        ds_ = Rot([P.sb(f"d{i}", [128, CH], F32) for i in range(4)])
        ucs = Rot([P.sb(f"uc{i}", [128, 4, CH], BF16) for i in range(2)])

        def tail_src(g):
            p_, m_ = gm[g]
            return self.exhsrc[p_ * CC:(p_ + 1) * CC, m_ * 32:(m_ + 1) * 32].rearrange("(i p) t -> p i t", p=128), self.R_exhsrc(m_)

        ybuf = {}

        uhb = {}

        def prep(j):
            sl = slice(j * CH, (j + 1) * CH)
            uh = uhs.next()
            uhb[j] = uh
            self.load(self.dq.next(), uh[:, :, 32:32 + CH], self.uT_d[:, sl].rearrange("(i p) t -> p i t", p=128), uh,
                      dram=[self.R_u[j]])
            if self.npar == 1:
                if j == 0:
                    self.POOL(lambda: nc.gpsimd.memset(uh[:, :, 0:32], 0.0), [], [uh])
                else:
                    src, rr = tail_src(j - 1)
                    self.load(self.dq.next(), uh[:, :, 0:32], src, uh, dram=rr)
            else:
                ha, hb = has.next(), hbs.next()
                if j == 0:
                    self.POOL(lambda: nc.gpsimd.memset(ha[:], 0.0), [], [ha])
                else:
                    src, rr = tail_src(2 * j - 1)
                    self.load(self.dq.next(), ha[:], src, ha, dram=rr)
                src, rr = tail_src(2 * j)
                self.load(self.dq.next(), hb[:], src, hb, dram=rr)
                self.DVE(lambda: nc.vector.tensor_scalar(out=htmp[:], in0=ha[:], scalar1=hs[:, 2 * j:2 * j + 1], scalar2=None,
                                                          op0=ALU.mult), [ha, hs], [htmp])
                self.DVE(lambda: nc.vector.scalar_tensor_tensor(out=uh[:, :, 0:32], in0=hb[:], scalar=hs[:, 2 * j + 1:2 * j + 2],
                                                                 in1=htmp[:], op0=ALU.mult, op1=ALU.add), [hb, hs, htmp], [uh])

        def conv(j):
            yc, ycb, sqb = ycs.next(), ycbs.next(), sqbs.next()
            ybuf[j] = (yc, ycb, sqb)
            uh = uhb.pop(j)
            for blk in range(4):
                ps = cps.next()
                for k in range(CK):
                    self.PE(lambda: nc.tensor.matmul(ps[:], lhsT=diagw[:, blk, k, :], rhs=uh[:, blk, 2 + k:2 + k + CH],
                                                     start=(k == 0), stop=(k == CK - 1)), [diagw, uh], [ps])
                self.ACT(lambda: nc.scalar.activation(out=yc[:, blk, :], in_=ps[:], func=AF.Identity, bias=cv[:, 0, blk:blk + 1]),
                         [ps, cv], [yc])
                self.ACT(lambda: nc.scalar.activation(out=sqb[:, blk, :], in_=ps[:], func=AF.Square, bias=cv[:, 0, blk:blk + 1]),
                         [ps, cv], [sqb])
                self.POOL(lambda: nc.gpsimd.tensor_copy(out=ycb[:, blk, :], in_=yc[:, blk, :]), [yc], [ycb])

        def stats(j):
            sl = slice(j * CH, (j + 1) * CH)
            yc, ycb, sqb = ybuf.pop(j)
            for blk in range(4):
                self.PE(lambda: nc.tensor.matmul(ps_mu[:], lhsT=self.ones_b[:], rhs=ycb[:, blk, :], start=(blk == 0), stop=(blk == 3)),
                        [self.ones_b, ycb], [ps_mu])
            for blk in range(4):
                self.PE(lambda: nc.tensor.matmul(ps_m2[:], lhsT=self.ones_b[:], rhs=sqb[:, blk, :], start=(blk == 0), stop=(blk == 3)),
                        [self.ones_b, sqb], [ps_m2])
            self.DVE(lambda: nc.vector.tensor_scalar(out=mu[:], in0=ps_mu[:], scalar1=1.0 / CC, scalar2=None, op0=ALU.mult), [ps_mu], [mu])
            self.DVE(lambda: nc.vector.tensor_tensor(out=msq[:], in0=mu[:], in1=mu[:], op=ALU.mult), [mu], [msq])
            self.DVE(lambda: nc.vector.scalar_tensor_tensor(out=rs[:], in0=ps_m2[:], scalar=1.0 / CC, in1=msq[:],
                                                             op0=ALU.mult, op1=ALU.subtract), [ps_m2, msq], [rs])
            self.DVE(lambda: nc.vector.tensor_scalar(out=rs[:], in0=rs[:], scalar1=EPS, scalar2=None, op0=ALU.add), [rs], [rs])
            self.ACT(lambda: nc.scalar.activation(out=rs[:], in_=rs[:], func=AF.Sqrt), [rs], [rs])
            self.DVE(lambda: nc.vector.reciprocal(out=rs[:], in_=rs[:]), [rs], [rs])
            uc = ucs.next()
            for blk in range(4):
                d = ds_.next()
                self.DVE(lambda: nc.vector.tensor_tensor(out=d[:], in0=yc[:, blk, :], in1=mu[:], op=ALU.subtract), [yc, mu], [d])
                self.POOL(lambda: nc.gpsimd.tensor_tensor(out=d[:], in0=d[:], in1=rs[:], op=ALU.mult), [d, rs], [d])
                self.ACT(lambda: nc.scalar.activation(out=uc[:, blk, :], in_=d[:], func=AF.Silu, scale=cv[:, 1, blk:blk + 1],
                                                      bias=cv[:, 2, blk:blk + 1]), [d, cv], [uc])
            self.store("act", self.uc_d[:, sl].rearrange("(i p) t -> p i t", p=128), uc[:], uc, dram=[self.R_uc[j]])

        prep(0)
        if self.nch > 1:
            prep(1)
        conv(0)
        for j in range(self.nch):
            if j + 1 < self.nch:
                conv(j + 1)
            if j + 2 < self.nch:
                prep(j + 2)
            stats(j)
        P.close()

    def phase_c2(self, l):
        nc = self.nc
        P = Phase(nc, self.fw)
        x_src = self.x_in if l == 0 else self.xa
        R_src = self.R_x if l == 0 else self.R_xa
        w_co, w_oa, w_out, w_r, brow = self.c2w
        rb_b = P.sb("rb_b", [128, 36], F32)
        mps = Rot([P.ps(f"mps{i}", [128, CH]) for i in range(4)])
        pr = P.ps("pr", [128, 4, 128])
        ps0 = mps.next()
        self.PE(lambda: nc.tensor.matmul(ps0[:, 0:36], lhsT=self.ones_f[0:1, :], rhs=brow[0:1, :], start=True, stop=True),
                [self.ones_f, brow], [ps0])
        self.DVE(lambda: nc.vector.tensor_copy(out=rb_b[:], in_=ps0[:, 0:36]), [ps0], [rb_b])
        W = {
            "ss": Rot([P.sb(f"ss{i}", [128, 4], F32) for i in range(2)]),
            "rstd": Rot([P.sb(f"rstd{i}", [128, 4], F32) for i in range(2)]),
            "junk": Rot([P.sb("junk", [128, D], BF16)]),
            "xn": Rot([P.sb("xn", [128, 4, D], BF16)]),
            "pT": Rot([P.ps(f"pT{i}", [128, 2 * CH], BF16) for i in range(2)]),
        }
        xts = Rot([P.sb(f"xt{i}", [128, D], F32) for i in range(9)])
        ucs = Rot([P.sb(f"ucl{i}", [128, 4, CH], BF16) for i in range(2)])
        ats = Rot([P.sb(f"atl{i}", [128, 4, CH], BF16) for i in range(2)])
        gcs = [P.sb(f"gc{i}", [128, 16, CH], BF16) for i in range(2)]
        t1s = Rot([P.sb(f"t1{i}", [128, CH], F32) for i in range(2)])
        t2s = Rot([P.sb(f"t2{i}", [128, CH], F32) for i in range(2)])
        tys = Rot([P.sb(f"ty{i}", [128, CH], F32) for i in range(2)])
        mTs = [P.sb(f"mT{i}", [128, 8, CH], BF16) for i in range(2)]
        h2T = P.sb("h2T", [128, 8, CH], BF16)
        R = {k: P.sb(k, shp, F32) for k, shp in dict(
            lg=[128, 4, 36], gmax=[128, 4], geq=[128, 4, 4], gsh=[128, 4, 4], gsum=[128, 4], gval=[128, 4],
            tmp=[128, 4, 4, 8], esel=[128, 4, 8], m1=[128, 4], mask1=[128, 4, 8], esel2=[128, 4, 8], m2=[128, 4],
            mask2=[128, 4, 8], w1=[128, 4], w2=[128, 4], ws=[128, 4, 8], ws2=[128, 4, 8]).items()}
        V = nc.vector
        cst = {}

        def S1(j):
            mT, gc = mTs[j % 2], gcs[j % 2]
            sl = slice(j * CH, (j + 1) * CH)
            ucl, atl = ucs.next(), ats.next()
            self.load(self.dq.next(), ucl[:], self.uc_d[:, sl].rearrange("(i p) t -> p i t", p=128), ucl, dram=[self.R_uc[j]])
            self.load(self.dq.next(), atl[:], self.at_d[:, sl].rearrange("(i p) t -> p i t", p=128), atl, dram=[self.R_at[j]])
            self.load(self.dq.next(), gc[:], self.gt_d[:, sl].rearrange("(i p) t -> p i t", p=128), gc, dram=[self.R_gt[j]])
            xs = []
            for s_ in range(4):
                xt = xts.next()
                self.load(self.dq.next(), xt[:], x_src[j * CH + s_ * 128: j * CH + (s_ + 1) * 128, :], xt, dram=[R_src[j]])
                xs.append(xt)
            for dm in range(8):
                dsl = slice(dm * 128, (dm + 1) * 128)
                ps_c, ps_a = mps.next(), mps.next()
                for i in range(4):
                    self.PE(lambda: nc.tensor.matmul(ps_c[:], lhsT=w_co[:, i, dsl], rhs=ucl[:, i, :], start=(i == 0), stop=(i == 3)),
                            [w_co, ucl], [ps_c])
                for i in range(4):
                    self.PE(lambda: nc.tensor.matmul(ps_a[:], lhsT=w_oa[:, i, dsl], rhs=atl[:, i, :], start=(i == 0), stop=(i == 3)),
                            [w_oa, atl], [ps_a])
                t1, t2 = t1s.next(), t2s.next()
                self.DVE(lambda: V.tensor_tensor(out=t1[:], in0=ps_a[:], in1=gc[:, dm, :], op=ALU.mult), [ps_a, gc], [t1])
                self.DVE(lambda: V.tensor_tensor(out=t2[:], in0=ps_c[:], in1=gc[:, 8 + dm, :], op=ALU.mult), [ps_c, gc], [t2])
                self.POOL(lambda: nc.gpsimd.tensor_tensor(out=mT[:, dm, :], in0=t1[:], in1=t2[:], op=ALU.add), [t1, t2], [mT])
            cst[j] = (xs, mT)

        def S2(j):
            xs, mT = cst[j]
            for s_ in range(4):
                for hf in range(2):
                    hsl = slice(hf * 512, (hf + 1) * 512)
                    ps_y = mps.next()
                    for dm in range(8):
                        self.PE(lambda: nc.tensor.matmul(ps_y[:], lhsT=mT[:, dm, s_ * 128:(s_ + 1) * 128], rhs=w_out[:, dm, hsl],
                                                         start=(dm == 0), stop=(dm == 7)), [mT, w_out], [ps_y])
                    ty = tys.next()
                    self.DVE(lambda: V.tensor_tensor(out=ty[:], in0=ps_y[:], in1=self.gtb[:, 0, hsl], op=ALU.mult),
                             [ps_y, self.gtb], [ty])
                    self.POOL(lambda: nc.gpsimd.tensor_tensor(out=xs[s_][:, hsl], in0=xs[s_][:, hsl], in1=ty[:], op=ALU.add),
                              [xs[s_], ty], [xs[s_]])
                self.store("pool", self.xa[j * CH + s_ * 128: j * CH + (s_ + 1) * 128, :], xs[s_][:], xs[s_],
                           dram=[self.R_xa[j]])

        def S34(j):
            xs, mT = cst.pop(j)
            sl = slice(j * CH, (j + 1) * CH)
            self.norm_hT(W, xs, 1, h2T)
            self.store("act", self.h2_d[:, sl].rearrange("(c p) t -> p c t", p=128), h2T[:], h2T, dram=[self.R_h2[j]])
            for s_ in range(4):
                for c in range(8):
                    self.PE(lambda: nc.tensor.matmul(pr[:, s_, 0:36], lhsT=h2T[:, c, s_ * 128:(s_ + 1) * 128], rhs=w_r[:, c, :],
                                                     start=(c == 0), stop=(c == 7)), [h2T, w_r], [pr])
            lg = R["lg"]
            self.DVE(lambda: V.tensor_tensor(out=lg[:], in0=pr[:, :, 0:36], in1=rb_b[:].unsqueeze(1).to_broadcast([128, 4, 36]),
                                             op=ALU.add), [pr, rb_b], [lg])
            gl = lg[:, :, 0:4]
            el = lg[:, :, 4:36].rearrange("p s (g e) -> p s g e", g=4)

            def b3(t, n):
                return t[:].unsqueeze(2).to_broadcast([128, 4, n])
            self.DVE(lambda: V.tensor_reduce(out=R["gmax"][:], in_=gl, axis=AX.X, op=ALU.max), [lg], [R["gmax"]])
            self.DVE(lambda: V.tensor_tensor(out=R["geq"][:], in0=gl, in1=b3(R["gmax"], 4), op=ALU.is_equal), [lg, R["gmax"]], [R["geq"]])
            self.DVE(lambda: V.tensor_tensor(out=R["gsh"][:], in0=gl, in1=b3(R["gmax"], 4), op=ALU.subtract), [lg, R["gmax"]], [R["gsh"]])
            self.ACT(lambda: nc.scalar.activation(out=R["gsh"][:], in_=R["gsh"][:], func=AF.Exp), [R["gsh"]], [R["gsh"]])
            self.DVE(lambda: V.tensor_reduce(out=R["gsum"][:], in_=R["gsh"][:], axis=AX.X, op=ALU.add), [R["gsh"]], [R["gsum"]])
            self.DVE(lambda: V.reciprocal(out=R["gval"][:], in_=R["gsum"][:]), [R["gsum"]], [R["gval"]])
            self.DVE(lambda: V.tensor_tensor(out=R["tmp"][:], in0=el, in1=R["geq"][:].unsqueeze(3).to_broadcast([128, 4, 4, 8]),
                                             op=ALU.mult), [lg, R["geq"]], [R["tmp"]])
            self.DVE(lambda: V.tensor_reduce(out=R["esel"][:], in_=R["tmp"][:].rearrange("p s g e -> p s e g"), axis=AX.X, op=ALU.add),
                     [R["tmp"]], [R["esel"]])
            self.DVE(lambda: V.tensor_reduce(out=R["m1"][:], in_=R["esel"][:], axis=AX.X, op=ALU.max), [R["esel"]], [R["m1"]])
            self.DVE(lambda: V.tensor_tensor(out=R["mask1"][:], in0=R["esel"][:], in1=b3(R["m1"], 8), op=ALU.is_equal),
                     [R["esel"], R["m1"]], [R["mask1"]])
            self.DVE(lambda: V.scalar_tensor_tensor(out=R["esel2"][:], in0=R["mask1"][:], scalar=-1e30, in1=R["esel"][:],
                                                    op0=ALU.mult, op1=ALU.add), [R["mask1"], R["esel"]], [R["esel2"]])
            self.DVE(lambda: V.tensor_reduce(out=R["m2"][:], in_=R["esel2"][:], axis=AX.X, op=ALU.max), [R["esel2"]], [R["m2"]])
            self.DVE(lambda: V.tensor_tensor(out=R["mask2"][:], in0=R["esel2"][:], in1=b3(R["m2"], 8), op=ALU.is_equal),
                     [R["esel2"], R["m2"]], [R["mask2"]])
            self.DVE(lambda: V.tensor_tensor(out=R["w1"][:], in0=R["m1"][:], in1=R["m2"][:], op=ALU.subtract), [R["m1"], R["m2"]], [R["w1"]])
            self.ACT(lambda: nc.scalar.activation(out=R["w1"][:], in_=R["w1"][:], func=AF.Sigmoid), [R["w1"]], [R["w1"]])
            self.DVE(lambda: V.tensor_scalar(out=R["w2"][:], in0=R["w1"][:], scalar1=-1.0, scalar2=1.0, op0=ALU.mult, op1=ALU.add),
                     [R["w1"]], [R["w2"]])
            self.DVE(lambda: V.tensor_tensor(out=R["w1"][:], in0=R["w1"][:], in1=R["gval"][:], op=ALU.mult), [R["w1"], R["gval"]], [R["w1"]])
            self.DVE(lambda: V.tensor_tensor(out=R["w2"][:], in0=R["w2"][:], in1=R["gval"][:], op=ALU.mult), [R["w2"], R["gval"]], [R["w2"]])
            self.DVE(lambda: V.tensor_tensor(out=R["ws"][:], in0=R["mask1"][:], in1=b3(R["w1"], 8), op=ALU.mult), [R["mask1"], R["w1"]], [R["ws"]])
            self.DVE(lambda: V.tensor_tensor(out=R["ws2"][:], in0=R["mask2"][:], in1=b3(R["w2"], 8), op=ALU.mult), [R["mask2"], R["w2"]], [R["ws2"]])
            self.DVE(lambda: V.tensor_tensor(out=R["ws"][:], in0=R["ws"][:], in1=R["ws2"][:], op=ALU.add), [R["ws"], R["ws2"]], [R["ws"]])
            cv_ = self.comb[:, j * 4:(j + 1) * 4, :].rearrange("p s (g e) -> p s g e", g=4)
            self.DVE(lambda: V.tensor_tensor(out=cv_, in0=R["geq"][:].unsqueeze(3).to_broadcast([128, 4, 4, 8]),
                                             in1=R["ws"][:].unsqueeze(2).to_broadcast([128, 4, 4, 8]), op=ALU.mult),
                     [R["geq"], R["ws"]], [self.comb])

        S1(0)
        for j in range(self.nch):
            S2(j)
            if j + 1 < self.nch:
                S1(j + 1)
            S34(j)
        if "comb" in self.debug and l == 0:
            d1 = self.dbg_out("comb", [128, (self.ntok // 128) * 32])
            self.store("sp", d1[:, :], self.comb[:].rearrange("p a b -> p (a b)"), self.comb)
        P.close()

    def phase_c(self, l):
        nc = self.nc
        PW = Phase(nc, self.fw)
        w_co = PW.sb("w_co", [128, 4, D], BF16)
        w_oa = PW.sb("w_oa", [128, 4, D], BF16)
        w_out = PW.sb("w_out", [128, 8, D], BF16)
        w_r = PW.sb("w_r", [128, 8, 36], BF16)
        brow = PW.sb("brow", [1, 36], F32)
        self.c2w = (w_co, w_oa, w_out, w_r, brow)
        self.phase_c1(l, PW)
        self.phase_c2(l)
        PW.close()

    def phase_d(self, l):
        nc = self.nc
        P = Phase(nc, self.fw)
        last = (l == self.depth - 1)
        wgs = [[P.sb(f"wg{i}{q}", [128, 8, 4 * FE], BF16) for q in range(2)] for i in range(2)]
        wus = [[P.sb(f"wu{i}{q}", [128, 8, 4 * FE], BF16) for q in range(2)] for i in range(2)]
        wds = [[P.sb(f"wd{i}{q}", [128, 4, D], BF16) for q in range(2)] for i in range(2)]

        def load_w(g):
            i = g % 2
            gv = self.e_g[l, g].rearrange("p (c e f) -> p c e f", c=8, e=NE)
            uv = self.e_u[l, g].rearrange("p (c e f) -> p c e f", c=8, e=NE)
            dv = self.e_d[l, g].rearrange("p (e n) -> p e n", e=NE)
            fl = []
            for q in range(2):
                fl.append(lambda q=q: self.load("pool", wgs[i][q][:].rearrange("p c (e f) -> p c e f", e=4),
                                                gv[:, :, q * 4:(q + 1) * 4, :], wgs[i][q]))
                fl.append(lambda q=q: self.load("pool", wus[i][q][:].rearrange("p c (e f) -> p c e f", e=4),
                                                uv[:, :, q * 4:(q + 1) * 4, :], wus[i][q]))
                fl.append(lambda q=q: self.load("pool", wds[i][q][:], dv[:, q * 4:(q + 1) * 4, :], wds[i][q]))
            return fl
        for f_ in load_w(0):
            f_()
        h2s = Rot([P.sb(f"h2{i}", [128, 8, CH], BF16) for i in range(2)])
        pas = Rot([P.ps(f"pa{i}", [128, CH]) for i in range(2)])
        pus = Rot([P.ps(f"pu{i}", [128, CH]) for i in range(2)])
        pts_ = Rot([P.ps(f"ptr{i}", [128, 2 * CH], BF16) for i in range(2)])
        pos_ = [P.ps(f"pod{i}", [128, CH]) for i in range(2)]
        sas = Rot([P.sb(f"sa{i}", [128, CH], F32) for i in range(2)])
        tts = Rot([P.sb(f"tt{i}", [128, CH], F32) for i in range(2)])
        hids = Rot([P.sb(f"hid{i}", [128, 4, FE], BF16) for i in range(2)])
        hTs = Rot([P.sb(f"hidT{i}", [128, 4, 128], BF16) for i in range(2)])
        xts = Rot([P.sb(f"xd{i}", [128, D], F32) for i in range(4)])
        tys = Rot([P.sb(f"tyd{i}", [128, CH], F32) for i in range(2)])
        if last:
            fr = P.sb("fr", [1, D], F32)
            self.load("sp", fr[:], self.fng[:, :], fr)
            fgb = P.sb("fgb", [128, D], F32)
            for hf in range(2):
                pb = pas.next()
                self.PE(lambda: nc.tensor.matmul(pb[:], lhsT=self.ones_f[0:1, :], rhs=fr[0:1, hf * 512:(hf + 1) * 512],
                                                 start=True, stop=True), [self.ones_f, fr], [pb])
                self.ACT(lambda: nc.scalar.copy(out=fgb[:, hf * 512:(hf + 1) * 512], in_=pb[:]), [pb], [fgb])
            fss = Rot([P.sb(f"fss{i}", [128, 1], F32) for i in range(2)])
            fjunk = P.sb("fjunk", [128, D], BF16)
            fos = Rot([P.sb(f"fo{i}", [128, D], F32) for i in range(2)])
        units = [(g, j, s_, eg) for g in range(NG) for j in range(self.nch) for s_ in range(4) for eg in range(2)]
        st = {}

        def GU(n):
            g, j, s_, eg = units[n]
            if eg == 1:
                xt = xts.next()
                rows_ = slice(j * CH + s_ * 128, j * CH + (s_ + 1) * 128)
                self.load(self.dq.next(), xt[:], self.xa[rows_, :], xt, dram=[self.R_xa[j]])
                st[("xt", n)] = xt
            if s_ == 0 and eg == 0:
                h2 = h2s.next()
                sl = slice(j * CH, (j + 1) * CH)
                self.load(self.dq.next(), h2[:], self.h2_d[:, sl].rearrange("(c p) t -> p c t", p=128), h2, dram=[self.R_h2[j]])
                st["h2"] = h2
            h2 = st["h2"]
            wg, wu = wgs[g % 2][eg], wus[g % 2][eg]
            tsl = slice(s_ * 128, (s_ + 1) * 128)
            pa, pu = pas.next(), pus.next()
            for c in range(8):
                self.PE(lambda: nc.tensor.matmul(pa[:], lhsT=h2[:, c, tsl], rhs=wg[:, c, :], start=(c == 0), stop=(c == 7)),
                        [h2, wg], [pa])
            for c in range(8):
                self.PE(lambda: nc.tensor.matmul(pu[:], lhsT=h2[:, c, tsl], rhs=wu[:, c, :], start=(c == 0), stop=(c == 7)),
                        [h2, wu], [pu])
            st[("pp", n)] = (pa, pu)

        def MID(n):
            g, j, s_, eg = units[n]
            ti = j * 4 + s_
            pa, pu = st.pop(("pp", n))
            sa, tt, hid, hT = sas.next(), tts.next(), hids.next(), hTs.next()
            self.ACT(lambda: nc.scalar.activation(out=sa[:], in_=pa[:], func=AF.Silu), [pa], [sa])
            self.DVE(lambda: nc.vector.tensor_tensor(out=tt[:], in0=sa[:], in1=pu[:], op=ALU.mult), [sa, pu], [tt])
            cw_ = self.comb[:, ti, g * 8 + eg * 4: g * 8 + eg * 4 + 4].unsqueeze(2).to_broadcast([128, 4, FE])
            self.POOL(lambda: nc.gpsimd.tensor_tensor(out=hid[:], in0=tt[:].rearrange("p (e f) -> p e f", e=4), in1=cw_,
                                                      op=ALU.mult), [tt, self.comb], [hid])
            ptr = pts_.next()
            for e in range(4):
                self.PE(lambda: nc.tensor.transpose(out=ptr[:, e * 128:(e + 1) * 128], in_=hid[:, e, :], identity=self.ident[:]),
                        [hid, self.ident], [ptr])
            if n % 2 == 0:
                self.ACT(lambda: nc.scalar.copy(out=hT[:].rearrange("p e t -> p (e t)"), in_=ptr[:, 0:CH]), [ptr], [hT])
            else:
                self.DVE(lambda: nc.vector.tensor_copy(out=hT[:].rearrange("p e t -> p (e t)"), in_=ptr[:, 0:CH]), [ptr], [hT])
            st[("hT", n)] = hT

        def DN(n):
            g, j, s_, eg = units[n]
            wd = wds[g % 2][eg]
            hT = st.pop(("hT", n))
            rows = slice(j * CH + s_ * 128, j * CH + (s_ + 1) * 128)
            for hf in range(2):
                for e in range(4):
                    self.PE(lambda: nc.tensor.matmul(pos_[hf][:], lhsT=hT[:, e, :], rhs=wd[:, e, hf * 512:(hf + 1) * 512],
                                                     start=(eg == 0 and e == 0), stop=(eg == 1 and e == 3)),
                            [hT, wd], [pos_[hf]])
            if eg == 0:
                return
            xt = st.pop(("xt", n))
            for hf in range(2):
                hsl = slice(hf * 512, (hf + 1) * 512)
                ty = tys.next()
                self.DVE(lambda: nc.vector.tensor_tensor(out=ty[:], in0=pos_[hf][:], in1=self.gtb[:, 1, hsl], op=ALU.mult),
                         [pos_[hf], self.gtb], [ty])
                self.POOL(lambda: nc.gpsimd.tensor_tensor(out=xt[:, hsl], in0=xt[:, hsl], in1=ty[:], op=ALU.add), [xt, ty], [xt])
            if last and g == NG - 1:
                fs, fo = fss.next(), fos.next()
                self.ACT(lambda: nc.scalar.activation(out=fjunk[:], in_=xt[:], func=AF.Square, accum_out=fs[:]), [xt], [fjunk, fs])
                self.DVE(lambda: nc.vector.tensor_scalar(out=fs[:], in0=fs[:], scalar1=1.0 / D, scalar2=EPS, op0=ALU.mult, op1=ALU.add),
                         [fs], [fs])
                self.ACT(lambda: nc.scalar.activation(out=fs[:], in_=fs[:], func=AF.Sqrt), [fs], [fs])
                self.DVE(lambda: nc.vector.reciprocal(out=fs[:], in_=fs[:]), [fs], [fs])
                self.DVE(lambda: nc.vector.scalar_tensor_tensor(out=fo[:], in0=xt[:], scalar=fs[:, 0:1], in1=fgb[:],
                                                                 op0=ALU.mult, op1=ALU.mult), [xt, fs, fgb], [fo])
                self.store("sp", self.out[rows, :], fo[:], fo, dram=[self.R_out[j]])
            else:
                self.store("sp", self.xa[rows, :], xt[:], xt, dram=[self.R_xa[j]])

        N = len(units)
        per_stage = N // NG
        wq = load_w(1)
        GU(0)
        for n in range(N):
            if n + 1 < N:
                GU(n + 1)
            MID(n)
            if n >= 1:
                DN(n - 1)
                if n % per_stage == 0 and n // per_stage + 1 < NG:
                    wq = load_w(n // per_stage + 1)
            if wq and n % 2 == 1:
                wq.pop(0)()
        DN(N - 1)
        P.close()


def _const_tables(npar):
    consts = np.zeros((128, 4), np.float32)
    inv_freq = (10000.0 ** (-np.arange(0, ROPE, 2, dtype=np.float32) / ROPE)).astype(np.float32)
    for r in range(32):
        consts[64 + r, 0] = inv_freq[r % 16]
        consts[64 + r, 1] = -1.0 if r < 16 else 1.0
    ident = np.eye(128, dtype=np.float32)
    pk = np.arange(128)[:, None]
    f = np.arange(CH)[None, :]
    diag = [((pk + d * 128) <= f).astype(np.float32) for d in range(4)]
    zeros = np.zeros((128, CH), np.float32)
    ones = np.ones((128, CH), np.float32)
    lo = diag + [zeros] * 4
    hi = [ones] * 4 + diag
    return consts, ident, np.stack(lo), np.stack(hi)


def prep_inputs(inputs, npar, depth=DEPTH, used=None):
    f32 = np.float32
    L = DEPTH
    consts, ident, m_lo, m_hi = _const_tables(npar)
    g = lambda k: np.asarray(inputs[k])
    col = lambda v, n: np.ascontiguousarray(v.reshape(L, n, 128).transpose(0, 2, 1)).astype(f32)
    shared = {
        "consts": consts, "ident": ident,
        "ada_w": g("ada_w"), "ada_b": g("ada_b").reshape(L, 1, 6 * D),
        "n1g": col(g("norm1_g"), 8), "n2g": col(g("norm2_g"), 8),
        "w_in": g("w_in"), "qng": col(g("q_norm_g"), 3), "w_uq": g("w_uq"),
        "kvng": col(g("kv_norm_g"), 2), "w_ukv": g("w_ukv"), "w_oa": g("w_o_attn"),
        "conv_w": np.ascontiguousarray(g("conv_w").transpose(0, 2, 1).reshape(L, 4, 128, CK).transpose(0, 2, 1, 3)),
        "conv_v": np.ascontiguousarray(np.stack([g("conv_b"), g("conv_ln_g"), g("conv_ln_b")], 1)
                                       .reshape(L, 3, 4, 128).transpose(0, 3, 1, 2)),
        "w_co": g("w_conv_out"), "w_out": g("w_out"),
        "w_r": np.ascontiguousarray(np.concatenate([g("router_group_w"), g("router_expert_w")], -1)),
        "b_r": np.concatenate([g("router_group_b"), g("router_expert_b")], -1).reshape(L, 1, 36),
        "e_g": np.ascontiguousarray(g("expert_w_gate").reshape(L, NG, NE, 8, 128, FE).transpose(0, 1, 4, 3, 2, 5)
                                    ).reshape(L, NG, 128, 8 * NE * FE),
        "e_u": np.ascontiguousarray(g("expert_w_up").reshape(L, NG, NE, 8, 128, FE).transpose(0, 1, 4, 3, 2, 5)
                                    ).reshape(L, NG, 128, 8 * NE * FE),
        "e_d": np.ascontiguousarray(g("expert_w_down").transpose(0, 1, 3, 2, 4)).reshape(L, NG, 128, NE * D),
        "fng": g("final_norm_g").reshape(1, D),
    }
    x = g("x")
    c = g("c")
    pos = g("positions")
    nch = 16 // npar
    maps = []
    for core in range(8):
        b, p = core // 2, core % 2
        if npar == 1:
            chunks = list(range(16))
        else:
            chunks = own_chunks(2, p)
        rows = np.concatenate([np.arange(cj * CH, (cj + 1) * CH) for cj in chunks])
        masks = np.zeros((2, 8, 128, CH), ml_dtypes.bfloat16)
        hsel = np.zeros((1, 2 * nch), f32)
        for j in range(nch):
            if npar == 1:
                masks[0, :4] = m_lo[:4]
                masks[1, :4] = m_lo[:4]
                hsel[0, 2 * j] = 1.0
            else:
                is_hi = (j + p) % 2 == 1
                masks[j % 2] = m_hi if is_hi else m_lo
                hsel[0, 2 * j] = 0.0 if is_hi else 1.0
                hsel[0, 2 * j + 1] = 1.0 if is_hi else 0.0
        m = {k: (v[:depth] if (v.ndim >= 3 and v.shape[0] == L and k not in ("masks",)) else v) for k, v in shared.items()}
        m["x_own"] = np.ascontiguousarray(x[b][rows])
        m["pos_own"] = np.ascontiguousarray(pos[b][rows]).reshape(1, -1).astype(np.int32)
        m["c_col"] = np.ascontiguousarray(c[b].reshape(8, 128).T)
        m["masks"] = masks
        m["halo_sel"] = hsel
        if used is not None:
            m = {k: v for k, v in m.items() if k in used}
        maps.append((m, b, rows))
    return maps


_PROG_CACHE = {}


def get_prog(npar, **kw):
    key = (npar, tuple(sorted((k, str(v)) for k, v in kw.items())))
    if key not in _PROG_CACHE:
        _PROG_CACHE[key] = Prog(npar=npar, **kw)
    return _PROG_CACHE[key]


NPAR = 2


def kernel(**inputs):
    prog = get_prog(NPAR)
    maps = prep_inputs(inputs, NPAR, DEPTH, set(prog.used_inputs))
    res = run_bass_kernel_spmd(prog.nc, [m for m, _, _ in maps], core_ids=list(range(8)))
    out = np.zeros((B, S, D), np.float32)
    for core, (m, b, rows) in enumerate(maps):
        if NPAR == 1 and core % 2 == 1:
            continue
        out[b][rows] = res.results[core]["out"]
    return out
```

```python
import math
from contextlib import ExitStack

import numpy as np
import ml_dtypes

import concourse.bass as bass
import concourse.mybir as mybir
from concourse.bass_utils import run_bass_kernel_spmd

F32 = mybir.dt.float32
BF16 = mybir.dt.bfloat16
I32 = mybir.dt.int32
ALU = mybir.AluOpType
AF = mybir.ActivationFunctionType
AX = mybir.AxisListType

D = 1024
NH = 8
QK = 96
NOPE = 64
ROPE = 32
VD = 64
QL = 384
KVL = 256
CC = 512
CK = 31
NG = 4
NE = 8
FE = 128
INC = 3744
S = 8192
B = 4
CH = 512
EPS = 1e-6
DEPTH = 2
TWO_PI = 2.0 * math.pi
CW1 = 6.28125
CW2 = TWO_PI - CW1

EPOCH = 12000


class Res:
    __slots__ = ("name", "w", "r", "dsem", "dcnt", "excl")

    def __init__(self, name):
        self.excl = False
        self.name = name
        self.w = {}
        self.r = {}
        self.dsem = None
        self.dcnt = 0


def _res(x):
    return x.r if isinstance(x, TL) else x


class FW:
    def __init__(self, nc, same_engine_sync=True):
        self.nc = nc
        self.eng = {"pe": nc.tensor, "act": nc.scalar, "dve": nc.vector,
                    "pool": nc.gpsimd, "sp": nc.sync}
        self.esem = {}
        self.ecnt = {}
        self.waited = {k: {} for k in self.eng}
        self.same = same_engine_sync
        self.nsem = 0
        self.all_sems = {}
        self.free_dsems = []
        self.ninst = {k: 0 for k in self.eng}
        for k in self.eng:
            self._new_epoch(k)

    def _alloc_sem(self, name):
        s = self.nc.alloc_semaphore(name)
        self.nsem += 1
        return s

    def _new_epoch(self, k):
        self.esem[k] = self._alloc_sem(f"e_{k}_{self.nsem}")
        self.ecnt[k] = 0

    def _collect(self, reads, writes, skip_waw_sem=None):
        deps = {}

        def add(tok):
            key = id(tok[0])
            if key not in deps or deps[key][1] < tok[1]:
                deps[key] = tok
        for r in reads:
            for tok in r.w.values():
                add(tok)
        for w in writes:
            for tok in w.w.values():
                if skip_waw_sem is not None and tok[0] is skip_waw_sem:
                    continue
                add(tok)
            for tok in w.r.values():
                add(tok)
        return deps

    def _wait(self, k, deps, skip_own=False):
        e = self.eng[k]
        wd = self.waited[k]
        for key, (sem, val) in deps.items():
            if skip_own and sem is self.esem[k]:
                continue
            if wd.get(key, 0) >= val:
                continue
            e.wait_ge(sem, val)
            self.ninst[k] += 1
            wd[key] = val

    def _record(self, tok, reads, writes, partial=False):
        key = id(tok[0])
        self.all_sems[key] = (tok[0], tok[1])
        for r in reads:
            r.r[key] = tok
        for w in writes:
            if partial:
                w.w[key] = tok
            else:
                w.w = {key: tok}
            w.r = {}

    def op(self, k, ins_fn, reads=(), writes=()):
        reads = [_res(x) for x in reads]
        writes = [_res(x) for x in writes]
        writes = writes + [x for x in reads if x.excl]
        reads = [x for x in reads if not x.excl]
        deps = self._collect(reads, writes)
        skip_own = (k == "pe") or (not self.same)
        self._wait(k, deps, skip_own=skip_own)
        ins = ins_fn()
        if self.ecnt[k] >= EPOCH:
            self._new_epoch(k)
        self.ecnt[k] += 1
        ins.then_inc(self.esem[k], 1)
        self.ninst[k] += 1
        tok = (self.esem[k], self.ecnt[k])
        self._record(tok, reads, writes)
        return ins

    def dma(self, q, out, in_, sb, reads=(), writes=(), **kw):
        sb = _res(sb)
        reads = [_res(x) for x in reads]
        writes = [_res(x) for x in writes]
        deps = self._collect(reads, writes, skip_waw_sem=sb.dsem)
        self._wait(q, deps)
        if sb.dsem is None or sb.dcnt >= 30000:
            if self.free_dsems:
                sb.dsem, sb.dcnt = self.free_dsems.pop()
            else:
                sb.dsem = self._alloc_sem(f"d_{sb.name}_{self.nsem}")
                sb.dcnt = 0
        sb.dcnt += 16
        ins = self.eng[q].dma_start(out=out, in_=in_, **kw)
        ins.then_inc(sb.dsem, 16)
        self.ninst[q] += 1
        tok = (sb.dsem, sb.dcnt)
        self._record(tok, reads, writes, partial=True)
        return ins

    def barrier(self):
        deps = dict(self.all_sems)
        for k in self.eng:
            self._wait(k, {kk: v for kk, v in deps.items()})


class TL:
    __slots__ = ("t", "r")

    def __init__(self, t, name, excl=False):
        self.t = t
        self.r = Res(name)
        self.r.excl = excl

    def __getitem__(self, key):
        return self.t[key]


class Phase:
    def __init__(self, nc, fw):
        self.nc = nc
        self.fw = fw
        self.es = ExitStack()
        self.n = 0
        self.tiles = []

    def sb(self, name, shape, dtype):
        self.n += 1
        t = self.es.enter_context(self.nc.sbuf_tensor(f"{name}_{id(self) % 100000}_{self.n}", list(shape), dtype))
        tl = TL(t, name)
        self.tiles.append(tl)
        return tl

    def ps(self, name, shape, dtype=F32):
        self.n += 1
        t = self.es.enter_context(self.nc.psum_tensor(f"{name}_{id(self) % 100000}_{self.n}", list(shape), dtype))
        return TL(t, name, excl=True)

    def close(self):
        self.fw.barrier()
        for tl in self.tiles:
            if tl.r.dsem is not None and tl.r.dcnt < 30000:
                self.fw.free_dsems.append((tl.r.dsem, tl.r.dcnt))
                tl.r.dsem = None
        self.es.close()


class Rot:
    def __init__(self, tiles):
        self.tiles = tiles
        self.i = 0

    def next(self):
        t = self.tiles[self.i % len(self.tiles)]
        self.i += 1
        return t


def own_chunks(npar, p):
    if npar == 1:
        return list(range(16))
    return [2 * j + ((j + p) % 2) for j in range(8)]


class Prog:
    def __init__(self, npar=2, depth=DEPTH, debug=None, stop_after=None, a_chunks=None, a_stop=99):
        self.a_chunks = a_chunks
        self.a_stop = a_stop
        self.npar = npar
        self.depth = depth
        self.debug = debug or set()
        self.stop_after = stop_after
        self.nch = 16 // npar
        self.ntok = self.nch * CH
        self.nc = bass.Bass("TRN2", target_bir_lowering=False)
        self.fw = FW(self.nc)
        self.dq = Rot(["sp"])
        self.build()

    def PE(self, fn, r=(), w=()):
        return self.fw.op("pe", fn, r, w)

    def ACT(self, fn, r=(), w=()):
        return self.fw.op("act", fn, r, w)

    def DVE(self, fn, r=(), w=()):
        return self.fw.op("dve", fn, r, w)

    def POOL(self, fn, r=(), w=()):
        return self.fw.op("pool", fn, r, w)

    def load(self, q, out_ap, in_ap, tile, dram=(), **kw):
        return self.fw.dma(q, out_ap, in_ap, tile, reads=list(dram), writes=[tile], **kw)

    def store(self, q, out_ap, in_ap, tile, dram=(), **kw):
        return self.fw.dma(q, out_ap, in_ap, tile, reads=[tile], writes=list(dram), **kw)

    def din(self, name, shape, dtype=F32):
        return self.nc.dram_tensor(name, list(shape), dtype, kind="ExternalInput").ap()

    def dscr(self, name, shape, dtype):
        kind = "ExternalOutput" if name in self.debug else "Internal"
        return self.nc.dram_tensor(name, list(shape), dtype, kind=kind).ap()

    def __getattr__(self, name):
        specs = self.__dict__.get("_specs", {})
        if name in specs:
            ap = self.din(*specs[name])
            self.__dict__[name] = ap
            self.used_inputs.append(specs[name][0])
            return ap
        raise AttributeError(name)

    def declare(self):
        self._specs = {}
        self.used_inputs = []
        L = self.depth
        nt = self.ntok
        self._specs["x_in"] = ("x_own", [nt, D])
        self._specs["pos_in"] = ("pos_own", [1, nt], I32)
        self._specs["c_col"] = ("c_col", [128, 8])
        self._specs["consts"] = ("consts", [128, 4])
        self._specs["ident_in"] = ("ident", [128, 128])
        self._specs["masks_in"] = ("masks", [2, 8, 128, CH], BF16)
        self._specs["halo_sel"] = ("halo_sel", [1, 2 * self.nch])
        self._specs["ada_w"] = ("ada_w", [L, D, 6 * D])
        self._specs["ada_b"] = ("ada_b", [L, 1, 6 * D])
        self._specs["n1g"] = ("n1g", [L, 128, 8])
        self._specs["n2g"] = ("n2g", [L, 128, 8])
        self._specs["w_in"] = ("w_in", [L, D, INC])
        self._specs["qng"] = ("qng", [L, 128, 3])
        self._specs["w_uq"] = ("w_uq", [L, QL, NH * QK])
        self._specs["kvng"] = ("kvng", [L, 128, 2])
        self._specs["w_ukv"] = ("w_ukv", [L, KVL, NH * 128])
        self._specs["w_oa"] = ("w_oa", [L, CC, D])
        self._specs["conv_w"] = ("conv_w", [L, 128, 4, CK])
        self._specs["conv_v"] = ("conv_v", [L, 128, 3, 4])
        self._specs["w_co"] = ("w_co", [L, CC, D])
        self._specs["w_out"] = ("w_out", [L, D, D])
        self._specs["w_r"] = ("w_r", [L, D, 36])
        self._specs["b_r"] = ("b_r", [L, 1, 36])
        self._specs["e_g"] = ("e_g", [L, NG, 128, 8 * NE * FE])
        self._specs["e_u"] = ("e_u", [L, NG, 128, 8 * NE * FE])
        self._specs["e_d"] = ("e_d", [L, NG, 128, NE * D])
        self._specs["fng"] = ("fng", [1, D])
        self.out = self.nc.dram_tensor("out", [nt, D], F32, kind="ExternalOutput").ap()
        self.xa = self.dscr("xa", [nt, D], F32)
        self.cs_d = self.dscr("cs_d", [2, ROPE, nt], F32)
        self.qT_d = self.dscr("qT_d", [NH * QK, nt], BF16)
        self.nparts = nt // 2048
        self.ex_d = [self.dscr(f"ex_d{i}", [KVL + ROPE, 2048], BF16) for i in range(self.nparts)]
        self.exh_d = self.dscr("exh_d", [CC, self.nch * 32], BF16)
        self.uT_d = self.dscr("uT_d", [CC, nt], BF16)
        self.gt_d = self.dscr("gt_d", [2 * D, nt], BF16)
        self.at_d = self.dscr("at_d", [NH * VD, nt], BF16)
        self.h2_d = self.dscr("h2_d", [D, nt], BF16)
        self.uc_d = self.dscr("uc_d", [CC, nt], BF16)
        if self.npar == 2:
            self.exg_d = [self.dscr(f"exg_d{i}", [2 * (KVL + ROPE), 2048], BF16) for i in range(self.nparts)]
            self.exhg_d = self.dscr("exhg_d", [2 * CC, self.nch * 32], BF16)
        n = self.nch
        self.R_x = [Res(f"Rx{j}") for j in range(n)]
        self.R_xa = [Res(f"Rxa{j}") for j in range(n)]
        self.R_cs = [Res(f"Rcs{j}") for j in range(n)]
        self.R_q = [Res(f"Rq{j}") for j in range(n)]
        self.R_ex = [Res(f"Rex{j}") for j in range(n)]
        self.R_exh = [Res(f"Rexh{j}") for j in range(n)]
        self.R_u = [Res(f"Ru{j}") for j in range(n)]
        self.R_gt = [Res(f"Rgt{j}") for j in range(n)]
        self.R_at = [Res(f"Rat{j}") for j in range(n)]
        self.R_h2 = [Res(f"Rh2{j}") for j in range(n)]
        self.R_out = [Res(f"Rout{j}") for j in range(n)]
        self.R_uc = [Res(f"Ruc{j}") for j in range(n)]
        self.R_exg = Res("Rexg")
        self.dbg = {}

    def dbg_out(self, name, shape, dtype=F32):
        ap = self.nc.dram_tensor("dbg_" + name, list(shape), dtype, kind="ExternalOutput").ap()
        self.dbg[name] = ap
        return ap

    def build(self):
        nc, fw = self.nc, self.fw
        self.declare()
        G = Phase(nc, fw)
        self.G = G
        self.ident_f = G.sb("ident_f", [128, 128], F32)
        self.ident = G.sb("ident", [128, 128], BF16)
        self.ones_f = G.sb("ones_f", [128, 128], F32)
        self.ones_b = G.sb("ones_b", [128, 128], BF16)
        self.cst = G.sb("cst", [128, 4], F32)
        self.load("sp", self.ident_f[:], self.ident_in[:, :], self.ident_f)
        self.load("sp", self.cst[:], self.consts[:, :], self.cst)
        self.DVE(lambda: nc.vector.tensor_copy(out=self.ident[:], in_=self.ident_f[:]), [self.ident_f], [self.ident])
        self.POOL(lambda: nc.gpsimd.memset(self.ones_f[:], 1.0), [], [self.ones_f])
        self.POOL(lambda: nc.gpsimd.memset(self.ones_b[:], 1.0), [], [self.ones_b])
        self.modc = G.sb("modc", [128, 4, 8], F32)
        self.gtb = G.sb("gtb", [128, 2, D], F32)
        self.comb = G.sb("comb", [128, self.ntok // 128, NG * NE], F32)
        self.cact = G.sb("cact", [128, 8], F32)
        self.load("sp", self.cact[:], self.c_col[:, :], self.cact)
        self.ACT(lambda: nc.scalar.activation(out=self.cact[:], in_=self.cact[:], func=AF.Silu), [self.cact], [self.cact])

        PR = self.rope_tables()
        if self.stop_after == "rope":
            PR.close()
            return self.finish()
        for l in range(self.depth):
            self.layer_mod(l)
            if l == 0:
                PR.close()
            if self.stop_after == "mod":
                return self.finish()
            self.phase_a(l)
            if self.stop_after == "a":
                return self.finish()
            self.exchange(l)
            self.phase_b(l)
            if self.stop_after == "b":
                return self.finish()
            self.phase_c(l)
            if self.stop_after == "c":
                return self.finish()
            self.phase_d(l)
        return self.finish()

    def finish(self):
        self.fw.barrier()
        print("ninst", self.fw.ninst, "nsem", self.fw.nsem)

    def rope_tables(self):
        nc = self.nc
        P = Phase(nc, self.fw)
        lo, hi = 64, 96
        pis = Rot([P.sb(f"pi{i}", [128, CH], I32) for i in range(2)])
        as_ = Rot([P.sb(f"a{i}", [128, CH], F32) for i in range(2)])
        t = P.sb("t", [128, CH], F32)
        ki = P.sb("ki", [128, CH], I32)
        kf = P.sb("kf", [128, CH], F32)
        r = P.sb("r", [128, CH], F32)
        m = P.sb("m", [128, CH], F32)
        os_ = Rot([P.sb(f"o{i}", [128, CH], F32) for i in range(2)])
        V = nc.vector
        for j in range(self.nch):
            sl = slice(j * CH, (j + 1) * CH)
            pi = pis.next()
            a = as_.next()
            self.load("sp", pi[lo:hi, :], self.pos_in[0:1, sl].partition_broadcast(32), pi)
            self.DVE(lambda: V.tensor_copy(out=a[lo:hi, :], in_=pi[lo:hi, :]), [pi], [a])
            self.DVE(lambda: V.tensor_scalar(out=a[lo:hi, :], in0=a[lo:hi, :], scalar1=self.cst[lo:hi, 0:1],
                                             scalar2=None, op0=ALU.mult), [a, self.cst], [a])
            for ti, ph in ((1, 0.0), (0, 0.5 * math.pi)):
                o = os_.next()
                self.DVE(lambda: V.tensor_scalar(out=t[lo:hi, :], in0=a[lo:hi, :], scalar1=ph, scalar2=1.0 / TWO_PI,
                                                 op0=ALU.add, op1=ALU.mult), [a], [t])
                self.DVE(lambda: V.tensor_copy(out=ki[lo:hi, :], in_=t[lo:hi, :]), [t], [ki])
                self.DVE(lambda: V.tensor_copy(out=kf[lo:hi, :], in_=ki[lo:hi, :]), [ki], [kf])
                self.DVE(lambda: V.scalar_tensor_tensor(out=r[lo:hi, :], in0=kf[lo:hi, :], scalar=-CW1, in1=a[lo:hi, :],
                                                        op0=ALU.mult, op1=ALU.add), [kf, a], [r])
                self.DVE(lambda: V.tensor_scalar(out=r[lo:hi, :], in0=r[lo:hi, :], scalar1=ph, scalar2=None,
                                                 op0=ALU.add), [r], [r])
                self.DVE(lambda: V.scalar_tensor_tensor(out=r[lo:hi, :], in0=kf[lo:hi, :], scalar=-CW2, in1=r[lo:hi, :],
                                                        op0=ALU.mult, op1=ALU.add), [kf, r], [r])
                self.DVE(lambda: V.tensor_scalar(out=m[lo:hi, :], in0=r[lo:hi, :], scalar1=math.pi, scalar2=None,
                                                 op0=ALU.is_gt), [r], [m])
                self.DVE(lambda: V.scalar_tensor_tensor(out=r[lo:hi, :], in0=m[lo:hi, :], scalar=-TWO_PI, in1=r[lo:hi, :],
                                                        op0=ALU.mult, op1=ALU.add), [m, r], [r])
                self.DVE(lambda: V.tensor_scalar(out=m[lo:hi, :], in0=r[lo:hi, :], scalar1=-math.pi, scalar2=None,
                                                 op0=ALU.is_lt), [r], [m])
                self.DVE(lambda: V.scalar_tensor_tensor(out=r[lo:hi, :], in0=m[lo:hi, :], scalar=TWO_PI, in1=r[lo:hi, :],
                                                        op0=ALU.mult, op1=ALU.add), [m, r], [r])
                self.DVE(lambda: V.tensor_scalar(out=r[lo:hi, :], in0=r[lo:hi, :], scalar1=-math.pi, scalar2=math.pi,
                                                 op0=ALU.max, op1=ALU.min), [r], [r])
                if ti == 1:
                    self.ACT(lambda: nc.scalar.activation(out=o[lo:hi, :], in_=r[lo:hi, :], func=AF.Sin,
                                                          scale=self.cst[lo:hi, 1:2]), [r, self.cst], [o])
                else:
                    self.ACT(lambda: nc.scalar.activation(out=o[lo:hi, :], in_=r[lo:hi, :], func=AF.Sin), [r], [o])
                self.store("act", self.cs_d[ti, :, sl], o[lo:hi, :], o, dram=[self.R_cs[j]])
        return P

    def layer_mod(self, l):
        nc = self.nc
        P = Phase(nc, self.fw)
        row = P.sb("adarow", [1, 6 * D], F32)
        brow = P.sb("adab", [1, 6 * D], F32)
        self.load("sp", brow[:], self.ada_b[l, :, :], brow)
        wv = self.ada_w[l].rearrange("(c p) n -> p c n", p=128)
        wts = Rot([P.sb(f"adaw{i}", [128, 8, 512], F32) for i in range(2)])
        pss = Rot([P.ps(f"adaps{i}", [128, 512]) for i in range(2)])
        for n in range(12):
            wt = wts.next()
            ps = pss.next()
            self.load(self.dq.next(), wt[:], wv[:, :, n * 512:(n + 1) * 512], wt)
            for c in range(8):
                self.PE(lambda c=c: nc.tensor.matmul(ps[0:1, :], lhsT=self.cact[:, c:c + 1], rhs=wt[:, c, :],
                                                     start=(c == 0), stop=(c == 7)), [self.cact, wt], [ps])
            self.DVE(lambda: nc.vector.tensor_tensor(out=row[:, n * 512:(n + 1) * 512], in0=ps[0:1, :],
                                                      in1=brow[:, n * 512:(n + 1) * 512], op=ALU.add), [ps, brow], [row])
        g1 = P.sb("g1", [128, 8], F32)
        g2 = P.sb("g2", [128, 8], F32)
        self.load("sp", g1[:], self.n1g[l], g1)
        self.load("sp", g2[:], self.n2g[l], g2)
        pc = P.ps("pc", [128, 128, 4])
        for vi, off in enumerate((0, 1, 3, 4)):
            for c in range(8):
                col = off * D + c * 128
                self.PE(lambda vi=vi, c=c, col=col: nc.tensor.matmul(pc[:, vi * 8 + c, 0:1], lhsT=row[0:1, col:col + 128],
                                                                     rhs=self.ones_f[0:1, 0:1], start=True, stop=True),
                        [row, self.ones_f], [pc])
        cols = P.sb("cols", [128, 4, 8], F32)
        self.DVE(lambda: nc.vector.tensor_copy(out=cols[:].rearrange("p a b -> p (a b)"), in_=pc[:, 0:32, 0]), [pc], [cols])
        for k, (g, sci, shi) in enumerate(((g1, 1, 0), (g2, 3, 2))):
            self.DVE(lambda g=g, sci=sci, k=k: nc.vector.scalar_tensor_tensor(
                out=self.modc[:, 2 * k, :], in0=cols[:, sci, :], scalar=1.0, in1=g[:], op0=ALU.add, op1=ALU.mult),
                [cols, g], [self.modc])
            self.DVE(lambda shi=shi, k=k: nc.vector.tensor_copy(out=self.modc[:, 2 * k + 1, :], in_=cols[:, shi, :]),
                     [cols], [self.modc])
        pbs = Rot([P.ps(f"pbb{i}", [128, 512]) for i in range(2)])
        for k, off in enumerate((2, 5)):
            for hf in range(2):
                pbb = pbs.next()
                col = off * D + hf * 512
                self.PE(lambda: nc.tensor.matmul(pbb[:], lhsT=self.ones_f[0:1, :], rhs=row[0:1, col:col + 512],
                                                 start=True, stop=True), [row, self.ones_f], [pbb])
                self.ACT(lambda: nc.scalar.copy(out=self.gtb[:, k, hf * 512:(hf + 1) * 512], in_=pbb[:]),
                         [pbb], [self.gtb])
        if "mod" in self.debug and l == 0:
            d1 = self.dbg_out("modc", [128, 32])
            d2 = self.dbg_out("gtb", [128, 2 * D])
            self.store("sp", d1[:, :], self.modc[:].rearrange("p a b -> p (a b)"), self.modc)
            self.store("sp", d2[:, :], self.gtb[:].rearrange("p a b -> p (a b)"), self.gtb)
        P.close()

    def norm_hT(self, W, xs, k, hT):
        nc = self.nc
        ss = W["ss"].next()
        for s_ in range(4):
            junk = W["junk"].next()
            self.ACT(lambda: nc.scalar.activation(out=junk[:], in_=xs[s_][:], func=AF.Square,
                                                  accum_out=ss[:, s_:s_ + 1]), [xs[s_]], [junk, ss])
        rstd = W["rstd"].next()
        self.DVE(lambda: nc.vector.tensor_scalar(out=rstd[:], in0=ss[:], scalar1=1.0 / D, scalar2=EPS,
                                                  op0=ALU.mult, op1=ALU.add), [ss], [rstd])
        self.ACT(lambda: nc.scalar.activation(out=rstd[:], in_=rstd[:], func=AF.Sqrt), [rstd], [rstd])
        self.DVE(lambda: nc.vector.reciprocal(out=rstd[:], in_=rstd[:]), [rstd], [rstd])
        xn = W["xn"].next()
        for s_ in range(4):
            self.DVE(lambda: nc.vector.tensor_scalar(out=xn[:, s_, :], in0=xs[s_][:], scalar1=rstd[:, s_:s_ + 1],
                                                      scalar2=None, op0=ALU.mult), [xs[s_], rstd], [xn])
        for c in range(8):
            pT = W["pT"].next()
            for s_ in range(4):
                self.PE(lambda: nc.tensor.transpose(out=pT[:, s_ * 128:(s_ + 1) * 128],
                                                    in_=xn[:, s_, c * 128:(c + 1) * 128], identity=self.ident[:]),
                        [xn, self.ident], [pT])
            if c % 2 == 0:
                self.DVE(lambda: nc.vector.tensor_scalar(out=hT[:, c, :], in0=pT[:, 0:CH], scalar1=self.modc[:, 2 * k, c:c + 1],
                                                          scalar2=self.modc[:, 2 * k + 1, c:c + 1], op0=ALU.mult, op1=ALU.add),
                         [pT, self.modc], [hT])
            else:
                self.ACT(lambda: nc.scalar.activation(out=hT[:, c, :], in_=pT[:, 0:CH], func=AF.Identity,
                                                      scale=self.modc[:, 2 * k, c:c + 1], bias=self.modc[:, 2 * k + 1, c:c + 1]),
                         [pT, self.modc], [hT])

    def rstd_bcast(self, ps, n, out, tmp):
        nc = self.nc
        self.DVE(lambda: nc.vector.tensor_scalar(out=out[:], in0=ps[:], scalar1=1.0 / n, scalar2=EPS,
                                                  op0=ALU.mult, op1=ALU.add), [ps], [out])
        self.ACT(lambda: nc.scalar.activation(out=out[:], in_=out[:], func=AF.Sqrt), [out], [out])
        self.DVE(lambda: nc.vector.reciprocal(out=out[:], in_=out[:]), [out], [out])

    def phase_a(self, l):
        nc = self.nc
        P = Phase(nc, self.fw)
        x_src = self.x_in if l == 0 else self.xa
        R_src = self.R_x if l == 0 else self.R_xa
        scale = QK ** -0.5
        wv = self.w_in[l].rearrange("(c p) n -> p c n", p=128)
        WSPL = [0, QL + KVL + ROPE, QL + KVL + ROPE + 2 * CC, INC]
        wparts = []
        for i in range(3):
            wt_ = P.sb(f"w_in{i}", [128, 8, WSPL[i + 1] - WSPL[i]], BF16)
            self.load("pool", wt_[:], wv[:, :, WSPL[i]:WSPL[i + 1]], wt_)
            wparts.append(wt_)
        w_in = wparts[0]
        wkr = P.sb("wkr", [128, 8, 2, QK], BF16)
        self.POOL(lambda: nc.gpsimd.memset(wkr[:], 0.0), [], [wkr])
        KR0 = QL + KVL
        self.POOL(lambda: nc.gpsimd.tensor_copy(out=wkr[:, :, 0, 64:96], in_=w_in[:, :, KR0:KR0 + 32]), [w_in], [wkr])
        self.POOL(lambda: nc.gpsimd.tensor_copy(out=wkr[:, :, 1, 64:80], in_=w_in[:, :, KR0 + 16:KR0 + 32]), [w_in], [wkr])
        self.POOL(lambda: nc.gpsimd.tensor_copy(out=wkr[:, :, 1, 80:96], in_=w_in[:, :, KR0:KR0 + 16]), [w_in], [wkr])
        stg = P.sb("stg", [128, 3, NH * QK], F32)
        qg = P.sb("qg", [128, 3], F32)
        self.load("sp", stg[:], self.w_uq[l].rearrange("(c p) n -> p c n", p=128), stg)
        self.load("sp", qg[:], self.qng[l], qg)
        w_uq = P.sb("w_uq", [128, 3, NH * QK], BF16)
        w_uqr = P.sb("w_uqr", [128, 3, NH, QK], BF16)
        self.POOL(lambda: nc.gpsimd.memset(w_uqr[:], 0.0), [], [w_uqr])
        for c in range(3):
            self.DVE(lambda: nc.vector.tensor_scalar(out=w_uq[:, c, :], in0=stg[:, c, :], scalar1=qg[:, c:c + 1],
                                                      scalar2=None, op0=ALU.mult), [stg, qg], [w_uq])
            v = w_uq[:, c, :].rearrange("p (h r) -> p h r", r=QK)
            self.POOL(lambda: nc.gpsimd.tensor_copy(out=w_uqr[:, c, :, 64:80], in_=v[:, :, 80:96]), [w_uq], [w_uqr])
            self.POOL(lambda: nc.gpsimd.tensor_copy(out=w_uqr[:, c, :, 80:96], in_=v[:, :, 64:80]), [w_uq], [w_uqr])
        W = {
            "ss": Rot([P.sb(f"ss{i}", [128, 4], F32) for i in range(2)]),
            "rstd": Rot([P.sb(f"rstd{i}", [128, 4], F32) for i in range(2)]),
            "junk": Rot([P.sb("junk", [128, D], BF16)]),
            "xn": Rot([P.sb("xn", [128, 4, D], BF16)]),
            "pT": Rot([P.ps(f"pT{i}", [128, 2 * CH], BF16) for i in range(2)]),
        }
        xts = Rot([P.sb(f"xt{i}", [128, D], F32) for i in range(5)])
        hTs = [P.sb(f"hT{i}", [128, 8, CH], BF16) for i in range(2)]
        mps = Rot([P.ps(f"mps{i}", [128, CH]) for i in range(4)])
        ps_sq = P.ps("ps_sq", [128, CH])
        ps_skv = P.ps("ps_skv", [128, CH])
        sqt = P.sb("sqt", [128, 5, CH], BF16)
        qlT = P.sb("qlT", [128, 3, CH], BF16)
        kvraw = P.sb("kvraw", [128, 2, CH], F32)
        csts = Rot([P.sb(f"cst{i}", [128, 2, CH], F32) for i in range(2)])
        for t in csts.tiles:
            self.POOL(lambda: nc.gpsimd.memset(t[0:64, 0, :], 1.0), [], [t])
            self.POOL(lambda: nc.gpsimd.memset(t[0:64, 1, :], 0.0), [], [t])
        t1s = Rot([P.sb(f"t1{i}", [128, CH], F32) for i in range(2)])
        t2s = Rot([P.sb(f"t2{i}", [128, CH], F32) for i in range(2)])
        kro = P.sb("kro", [128, CH], BF16)
        sg = P.sb("sg", [128, 4, CH], F32)
        uT = P.sb("uT", [128, 4, CH], BF16)
        rq = P.sb("rq", [128, CH], F32)
        rkv = P.sb("rkv", [128, CH], F32)
        kvn = P.sb("kvn", [128, 2, CH], BF16)
        Cp = P.sb("Cp", [128, CH], F32)
        Sp = P.sb("Sp", [128, CH], F32)
        gts = Rot([P.sb(f"gts{i}", [128, 4, CH], BF16) for i in range(2)])
        qTc = P.sb("qTc", [128, NH, CH], BF16)

        cur = {}

        def proj(ps, lhs_fn, M, wt):
            hT = cur["hT"]
            for c in range(8):
                self.PE(lambda: nc.tensor.matmul(ps[0:M, :], lhsT=lhs_fn(c), rhs=hT[:, c, :],
                                                 start=(c == 0), stop=(c == 7)), [wt, hT], [ps])

        def wpart(off):
            i = 0 if off < WSPL[1] else (1 if off < WSPL[2] else 2)
            return wparts[i], off - WSPL[i]

        def colblk(off):
            wt_, o_ = wpart(off)
            return lambda c: wt_[:, c, o_:o_ + 128]

        nchunks = self.a_chunks or self.nch
        prepped = {}

        def prep(j):
            sl = slice(j * CH, (j + 1) * CH)
            xs = []
            for s_ in range(4):
                xt = xts.next()
                self.load(self.dq.next(), xt[:], x_src[j * CH + s_ * 128: j * CH + (s_ + 1) * 128, :], xt, dram=[R_src[j]])
                xs.append(xt)
            cst_c = csts.next()
            self.load("sp", cst_c[64:96, 0, :], self.cs_d[0, :, sl], cst_c, dram=[self.R_cs[j]])
            self.load("sp", cst_c[64:96, 1, :], self.cs_d[1, :, sl], cst_c, dram=[self.R_cs[j]])
            self.norm_hT(W, xs, 0, hTs[j % 2])
            prepped[j] = cst_c

        prep(0)
        for j in range(nchunks):
            sl = slice(j * CH, (j + 1) * CH)
            cur["hT"] = hTs[j % 2]
            cst_c = prepped.pop(j)
            for i in range(3):
                ps = mps.next()
                proj(ps, colblk(i * 128), 128, wpart(i * 128)[0])
                self.ACT(lambda: nc.scalar.activation(out=sqt[:, i, :], in_=ps[:], func=AF.Square), [ps], [sqt])
                self.DVE(lambda: nc.vector.tensor_copy(out=qlT[:, i, :], in_=ps[:]), [ps], [qlT])
            for i in range(2):
                ps = mps.next()
                proj(ps, colblk(QL + i * 128), 128, wpart(QL + i * 128)[0])
                self.ACT(lambda: nc.scalar.activation(out=sqt[:, 3 + i, :], in_=ps[:], func=AF.Square), [ps], [sqt])
                self.DVE(lambda: nc.vector.tensor_copy(out=kvraw[:, i, :], in_=ps[:]), [ps], [kvraw])
            ps_k = mps.next()
            proj(ps_k, lambda c: wkr[:, c, 0, :], QK, wkr)
            ps_kr = mps.next()
            proj(ps_kr, lambda c: wkr[:, c, 1, :], QK, wkr)
            t1 = t1s.next()
            t2 = t2s.next()
            self.DVE(lambda: nc.vector.tensor_tensor(out=t1[64:96, :], in0=ps_k[64:96, :], in1=cst_c[64:96, 0, :], op=ALU.mult),
                     [ps_k, cst_c], [t1])
            self.DVE(lambda: nc.vector.tensor_tensor(out=t2[64:96, :], in0=ps_kr[64:96, :], in1=cst_c[64:96, 1, :], op=ALU.mult),
                     [ps_kr, cst_c], [t2])
            self.POOL(lambda: nc.gpsimd.tensor_tensor(out=kro[64:96, :], in0=t1[64:96, :], in1=t2[64:96, :], op=ALU.add),
                      [t1, t2], [kro])
            exp_, eo = self.ex_d[j // 4], (j % 4) * CH
            self.store("pool", exp_[KVL:KVL + ROPE, eo:eo + CH], kro[64:96, :], kro, dram=[self.R_ex[j]])
            C0 = QL + KVL + ROPE
            for i in range(4):
                ps = mps.next()
                proj(ps, colblk(C0 + CC + i * 128), 128, wpart(C0 + CC + i * 128)[0])
                self.ACT(lambda: nc.scalar.activation(out=sg[:, i, :], in_=ps[:], func=AF.Sigmoid), [ps], [sg])
            for i in range(4):
                ps = mps.next()
                proj(ps, colblk(C0 + i * 128), 128, wpart(C0 + i * 128)[0])
                self.DVE(lambda: nc.vector.tensor_tensor(out=uT[:, i, :], in0=ps[:], in1=sg[:, i, :], op=ALU.mult),
                         [ps, sg], [uT])
            self.store("pool", self.uT_d[:, sl].rearrange("(i p) t -> p i t", p=128), uT[:], uT, dram=[self.R_u[j]])
            self.store("pool", self.exh_d[:, j * 32:(j + 1) * 32].rearrange("(i p) t -> p i t", p=128), uT[:, :, CH - 32:CH],
                       uT, dram=[self.R_exh[j]])
            if j + 1 < nchunks:
                prep(j + 1)
            for i in range(3):
                self.PE(lambda: nc.tensor.matmul(ps_sq[:], lhsT=self.ones_b[:], rhs=sqt[:, i, :], start=(i == 0), stop=(i == 2)),
                        [self.ones_b, sqt], [ps_sq])
            for i in range(2):
                self.PE(lambda: nc.tensor.matmul(ps_skv[:], lhsT=self.ones_b[:], rhs=sqt[:, 3 + i, :], start=(i == 0), stop=(i == 1)),
                        [self.ones_b, sqt], [ps_skv])
            self.rstd_bcast(ps_sq, QL, rq, None)
            self.rstd_bcast(ps_skv, KVL, rkv, None)
            for i in range(2):
                self.DVE(lambda: nc.vector.tensor_tensor(out=kvn[:, i, :], in0=kvraw[:, i, :], in1=rkv[:], op=ALU.mult),
                         [kvraw, rkv], [kvn])
            self.store("pool", exp_[0:KVL, eo:eo + CH].rearrange("(i p) t -> p i t", p=128), kvn[:], kvn, dram=[self.R_ex[j]])
            self.DVE(lambda: nc.vector.scalar_tensor_tensor(out=Cp[0:QK, :], in0=cst_c[0:QK, 0, :], scalar=scale, in1=rq[0:QK, :],
                                                             op0=ALU.mult, op1=ALU.mult), [cst_c, rq], [Cp])
            self.DVE(lambda: nc.vector.scalar_tensor_tensor(out=Sp[0:QK, :], in0=cst_c[0:QK, 1, :], scalar=scale, in1=rq[0:QK, :],
                                                             op0=ALU.mult, op1=ALU.mult), [cst_c, rq], [Sp])
            G0 = C0 + 2 * CC
            for i in range(16):
                if i % 4 == 0:
                    gt = gts.next()
                ps = mps.next()
                proj(ps, colblk(G0 + i * 128), 128, wpart(G0 + i * 128)[0])
                self.ACT(lambda: nc.scalar.activation(out=gt[:, i % 4, :], in_=ps[:], func=AF.Sigmoid), [ps], [gt])
                if i % 4 == 3:
                    r0 = (i - 3) * 128
                    self.store("act", self.gt_d[r0:r0 + 512, sl].rearrange("(i p) t -> p i t", p=128), gt[:], gt,
                               dram=[self.R_gt[j]])
            for h in range(NH):
                ps_q = mps.next()
                ps_r = mps.next()
                for c in range(3):
                    self.PE(lambda: nc.tensor.matmul(ps_q[0:QK, :], lhsT=w_uq[:, c, h * QK:(h + 1) * QK], rhs=qlT[:, c, :],
                                                     start=(c == 0), stop=(c == 2)), [w_uq, qlT], [ps_q])
                for c in range(3):
                    self.PE(lambda: nc.tensor.matmul(ps_r[0:QK, :], lhsT=w_uqr[:, c, h, :], rhs=qlT[:, c, :],
                                                     start=(c == 0), stop=(c == 2)), [w_uqr, qlT], [ps_r])
                t1 = t1s.next()
                t2 = t2s.next()
                self.DVE(lambda: nc.vector.tensor_tensor(out=t1[0:QK, :], in0=ps_q[0:QK, :], in1=Cp[0:QK, :], op=ALU.mult),
                         [ps_q, Cp], [t1])
                self.DVE(lambda: nc.vector.tensor_tensor(out=t2[0:QK, :], in0=ps_r[0:QK, :], in1=Sp[0:QK, :], op=ALU.mult),
                         [ps_r, Sp], [t2])
                self.POOL(lambda: nc.gpsimd.tensor_tensor(out=qTc[0:QK, h, :], in0=t1[0:QK, :], in1=t2[0:QK, :], op=ALU.add),
                          [t1, t2], [qTc])
            self.store("pool", self.qT_d[:, sl].rearrange("(h r) t -> r h t", r=QK), qTc[0:QK, :, :], qTc, dram=[self.R_q[j]])
        P.close()


    def gmap(self):
        if self.npar == 1:
            return {g: (0, g) for g in range(16)}
        m = {}
        for p in range(2):
            for j, g in enumerate(own_chunks(2, p)):
                m[g] = (p, j)
        return m

    def exchange(self, l):
        nc = self.nc
        if self.npar == 1:
            self.exsrc, self.exhsrc = self.ex_d, self.exh_d
            self.R_exsrc = lambda g: [self.R_ex[g]]
            self.R_exhsrc = lambda g: [self.R_exh[g]]
            return
        fw = self.fw
        fw.barrier()
        groups = [[0, 1], [2, 3], [4, 5], [6, 7]]
        cc_sem = fw._alloc_sem(f"cc{l}")
        n = 0
        for i in range(self.nparts):
            nc.gpsimd.collective_compute("AllGather", ALU.bypass, replica_groups=groups,
                                         ins=[self.ex_d[i][:, :]], outs=[self.exg_d[i][:, :]]).then_inc(cc_sem, 1)
            n += 1
        nc.gpsimd.collective_compute("AllGather", ALU.bypass, replica_groups=groups,
                                     ins=[self.exh_d[:, :]], outs=[self.exhg_d[:, :]]).then_inc(cc_sem, 1)
        n += 1
        fw.all_sems[id(cc_sem)] = (cc_sem, n)
        fw.barrier()
        self.exsrc, self.exhsrc = self.exg_d, self.exhg_d
        self.R_exsrc = lambda g: []
        self.R_exhsrc = lambda g: []

    def phase_b(self, l):
        nc = self.nc
        P = Phase(nc, self.fw)
        gm = self.gmap()
        EXR = KVL + ROPE
        stg = P.sb("stgkv", [128, 2, NH * 128], F32)
        kg = P.sb("kg", [128, 2], F32)
        self.load("sp", stg[:], self.w_ukv[l].rearrange("(c p) n -> p c n", p=128), stg)
        self.load("sp", kg[:], self.kvng[l], kg)
        w_ukv = P.sb("w_ukv", [128, 2, NH * 128], BF16)
        for c in range(2):
            self.DVE(lambda: nc.vector.tensor_scalar(out=w_ukv[:, c, :], in0=stg[:, c, :], scalar1=kg[:, c:c + 1],
                                                      scalar2=None, op0=ALU.mult), [stg, kg], [w_ukv])
        mk = P.sb("mk", [128, 16, CH], BF16)
        self.load("sp", mk[:], self.masks_in.rearrange("a r p f -> p (a r) f"), mk)
        kvT = P.sb("kvT", [128, 2, S], BF16)
        kTs = [P.sb(f"kT{i}", [128, S], BF16) for i in range(2)]
        nloc = self.nch

        def kcol(g):
            p_, m_ = gm[g]
            return (p_ * nloc + m_) * CH
        for p_ in range(self.npar):
            for i in range(self.nparts):
                ext = self.exsrc[i]
                dst = slice((p_ * nloc + 4 * i) * CH, (p_ * nloc + 4 * i + 4) * CH)
                rr = [r_ for m_ in range(4 * i, 4 * i + 4) for r_ in self.R_exsrc(m_)]
                for c in range(2):
                    self.load("sp", kvT[:, c, dst], ext[p_ * EXR + c * 128: p_ * EXR + (c + 1) * 128, :], kvT, dram=rr)
                for kT in kTs:
                    self.load("sp", kT[64:96, dst], ext[p_ * EXR + KVL: p_ * EXR + EXR, :], kT, dram=rr)
        vaugs = [P.sb(f"vaug{i}", [128, S // 128, 2 * VD], BF16) for i in range(2)]
        for v in vaugs:
            self.POOL(lambda: nc.gpsimd.memset(v[:, :, VD:2 * VD], 1.0), [], [v])
        qTs = [P.sb(f"qT{i}", [128, self.ntok], BF16) for i in range(2)]
        pts = Rot([P.sb(f"pt{i}", [128, CH], BF16) for i in range(6)])
        scs = Rot([P.ps(f"sc{i}", [128, CH]) for i in range(3)])
        pos_ = Rot([P.ps(f"po{i}", [128, CH]) for i in range(2)])
        blds = Rot([P.ps(f"bld{i}", [128, CH]) for i in range(2)])
        obs = Rot([P.sb(f"ob{i}", [128, CH], F32) for i in range(2)])
        lrows = Rot([P.sb(f"lrow{i}", [128, CH], F32) for i in range(2)])
        rlss = Rot([P.sb(f"rls{i}", [128, CH], F32) for i in range(2)])
        atts = Rot([P.sb(f"att{i}", [128, CH], BF16) for i in range(2)])
        nkt_per = 4 * self.npar
        ev = 0
        def build_groups(h):
            kT, vaug = kTs[h % 2], vaugs[h % 2]
            gl = []

            def kgrp(s16):
                ps = blds.next()
                sl = slice(s16 * CH, (s16 + 1) * CH)
                for c in range(2):
                    self.PE(lambda: nc.tensor.matmul(ps[0:64, :], lhsT=w_ukv[:, c, h * 128:h * 128 + 64], rhs=kvT[:, c, sl],
                                                     start=(c == 0), stop=(c == 1)), [w_ukv, kvT], [ps])
                self.DVE(lambda: nc.vector.tensor_copy(out=kT[0:64, sl], in_=ps[0:64, :]), [ps], [kT])

            def vgrp(g8):
                ps = blds.next()
                for i in range(8):
                    kt = g8 * 8 + i
                    for c in range(2):
                        self.PE(lambda: nc.tensor.matmul(ps[:, i * 64:(i + 1) * 64], lhsT=kvT[:, c, kt * 128:(kt + 1) * 128],
                                                         rhs=w_ukv[:, c, h * 128 + 64:h * 128 + 128],
                                                         start=(c == 0), stop=(c == 1)), [w_ukv, kvT], [ps])
                src = ps[:, :].rearrange("p (i d) -> p i d", d=64)
                self.DVE(lambda: nc.vector.tensor_copy(out=vaug[:, g8 * 8:(g8 + 1) * 8, 0:VD], in_=src), [ps], [vaug])
            for s16 in range(16):
                gl.append(lambda s16=s16: kgrp(s16))
            for g8 in range(8):
                gl.append(lambda g8=g8: vgrp(g8))
            return gl

        for g_ in build_groups(0):
            g_()
        self.load("sp", qTs[0][0:QK, :], self.qT_d[0:QK, :], qTs[0], dram=self.R_q)
        for h in range(NH):
            kT, vaug, qT = kTs[h % 2], vaugs[h % 2], qTs[h % 2]
            if h + 1 < NH:
                self.load("sp", qTs[(h + 1) % 2][0:QK, :], self.qT_d[(h + 1) * QK:(h + 2) * QK, :], qTs[(h + 1) % 2], dram=self.R_q)
                pending = build_groups(h + 1)
            else:
                pending = []
            blocks = [(j, kt) for j in range(self.nch) for kt in range(nkt_per * (j + 1))]
            every = max(1, len(blocks) // 26)
            sc_of = {}
            po_of = {}
            nq = [0]

            def qk_ahead(upto):
                while nq[0] < min(upto, len(blocks)):
                    j, kt = blocks[nq[0]]
                    sc = scs.next()
                    kc = kcol(kt // 4) + (kt % 4) * 128
                    self.PE(lambda: nc.tensor.matmul(sc[:], lhsT=kT[0:QK, kc:kc + 128], rhs=qT[0:QK, j * CH:(j + 1) * CH],
                                                     start=True, stop=True), [kT, qT], [sc])
                    sc_of[nq[0]] = sc
                    nq[0] += 1

            tails = []

            def tail2(j, po, ob, rls):
                att = atts.next()
                self.DVE(lambda: nc.vector.tensor_tensor(out=att[0:VD, :], in0=ob[0:VD, :], in1=rls[0:VD, :], op=ALU.mult),
                         [ob, rls], [att])
                self.store("sp", self.at_d[h * VD:(h + 1) * VD, j * CH:(j + 1) * CH], att[0:VD, :], att, dram=[self.R_at[j]])

            for idx, (j, kt) in enumerate(blocks):
                nkt = nkt_per * (j + 1)
                qk_ahead(idx + 3)
                if kt == 0:
                    po_of[j] = pos_.next()
                po = po_of[j]
                sc = sc_of.pop(idx)
                pt = pts.next()
                self.ACT(lambda: nc.scalar.activation(out=pt[:], in_=sc[:], func=AF.Exp), [sc], [pt])
                r = kt - nkt_per * j
                if r >= 0:
                    mi = (j % 2) * 8 + r if self.npar == 2 else r
                    if r % 3 == 2:
                        self.POOL(lambda: nc.gpsimd.tensor_tensor(out=pt[:], in0=pt[:], in1=mk[:, mi, :], op=ALU.mult),
                                  [pt, mk], [pt])
                    else:
                        self.DVE(lambda: nc.vector.tensor_tensor(out=pt[:], in0=pt[:], in1=mk[:, mi, :], op=ALU.mult),
                                 [pt, mk], [pt])
                kvi = (kcol(kt // 4) + (kt % 4) * 128) // 128
                self.PE(lambda: nc.tensor.matmul(po[:], lhsT=vaug[:, kvi, :], rhs=pt[:],
                                                 start=(kt == 0), stop=(kt == nkt - 1)), [vaug, pt], [po])
                if pending and idx % every == every - 1:
                    pending.pop(0)()
                if tails and (idx >= tails[0][0] + 4):
                    _, a_ = tails.pop(0)
                    tail2(*a_)
                if kt == nkt - 1:
                    ob, lrow, rls = obs.next(), lrows.next(), rlss.next()
                    self.DVE(lambda: nc.vector.tensor_copy(out=ob[0:VD, :], in_=po[0:VD, :]), [po], [ob])
                    self.DVE(lambda: nc.vector.reciprocal(out=lrow[VD:2 * VD, :], in_=po[VD:2 * VD, :]), [po], [lrow])
                    self.fw.dma("sp", rls[0:VD, :], lrow[VD:2 * VD, :], rls, reads=[lrow], writes=[rls])
                    tails.append((idx, (j, po, ob, rls)))
            for _, a_ in tails:
                tail2(*a_)
            for g_ in pending:
                g_()
        P.close()


    def phase_c1(self, l, PW):
        nc = self.nc
        P = Phase(nc, self.fw)
        gm = self.gmap()
        cw = P.sb("cw", [128, 4, CK], F32)
        cv = P.sb("cv", [128, 3, 4], F32)
        self.load("sp", cw[:], self.conv_w[l], cw)
        self.load("sp", cv[:], self.conv_v[l], cv)
        diagw = P.sb("diagw", [128, 4, CK, 128], BF16)
        for blk in range(4):
            self.DVE(lambda: nc.vector.tensor_tensor(out=diagw[:, blk, :, :],
                                                      in0=self.ident_f[:].unsqueeze(1).to_broadcast([128, CK, 128]),
                                                      in1=cw[:, blk, :].unsqueeze(2).to_broadcast([128, CK, 128]), op=ALU.mult),
                     [self.ident_f, cw], [diagw])
        w_co, w_oa, w_out, w_r, brow = self.c2w
        self.load("pool", w_co[:], self.w_co[l].rearrange("(i p) n -> p i n", p=128), w_co)
        self.load("pool", w_oa[:], self.w_oa[l].rearrange("(i p) n -> p i n", p=128), w_oa)
        self.load("pool", w_out[:], self.w_out[l].rearrange("(i p) n -> p i n", p=128), w_out)
        self.load("pool", w_r[:], self.w_r[l].rearrange("(i p) n -> p i n", p=128), w_r)
        self.load("sp", brow[:], self.b_r[l], brow)
        if self.npar == 2:
            hs = P.sb("hs", [128, 2 * self.nch], F32)
            self.load("sp", hs[:], self.halo_sel[0:1, :].partition_broadcast(128), hs)
            has = Rot([P.sb(f"ha{i}", [128, 4, 32], BF16) for i in range(3)])
            hbs = Rot([P.sb(f"hb{i}", [128, 4, 32], BF16) for i in range(3)])
            htmp = P.sb("htmp", [128, 4, 32], F32)
        uhs = Rot([P.sb(f"uh{i}", [128, 4, 32 + CH], BF16) for i in range(3)])
        cps = Rot([P.ps(f"cps{i}", [128, CH]) for i in range(4)])
        ps_mu = P.ps("ps_mu", [128, CH])
        ps_m2 = P.ps("ps_m2", [128, CH])
        ycs = Rot([P.sb(f"yc{i}", [128, 4, CH], F32) for i in range(2)])
        ycbs = Rot([P.sb(f"ycb{i}", [128, 4, CH], BF16) for i in range(2)])
        sqbs = Rot([P.sb(f"sqb{i}", [128, 4, CH], BF16) for i in range(2)])
        mu = P.sb("mu", [128, CH], F32)
        msq = P.sb("msq", [128, CH], F32)
        rs = P.sb("rs", [128, CH], F32)
        ds_ = Rot([P.sb(f"d{i}", [128, CH], F32) for i in range(4)])
        ucs = Rot([P.sb(f"uc{i}", [128, 4, CH], BF16) for i in range(2)])

        def tail_src(g):
            p_, m_ = gm[g]
            return self.exhsrc[p_ * CC:(p_ + 1) * CC, m_ * 32:(m_ + 1) * 32].rearrange("(i p) t -> p i t", p=128), self.R_exhsrc(m_)

        ybuf = {}

        uhb = {}

        def prep(j):
            sl = slice(j * CH, (j + 1) * CH)
            uh = uhs.next()
            uhb[j] = uh
            self.load(self.dq.next(), uh[:, :, 32:32 + CH], self.uT_d[:, sl].rearrange("(i p) t -> p i t", p=128), uh,
                      dram=[self.R_u[j]])
            if self.npar == 1:
                if j == 0:
                    self.POOL(lambda: nc.gpsimd.memset(uh[:, :, 0:32], 0.0), [], [uh])
                else:
                    src, rr = tail_src(j - 1)
                    self.load(self.dq.next(), uh[:, :, 0:32], src, uh, dram=rr)
            else:
                ha, hb = has.next(), hbs.next()
                if j == 0:
                    self.POOL(lambda: nc.gpsimd.memset(ha[:], 0.0), [], [ha])
                else:
                    src, rr = tail_src(2 * j - 1)
                    self.load(self.dq.next(), ha[:], src, ha, dram=rr)
                src, rr = tail_src(2 * j)
                self.load(self.dq.next(), hb[:], src, hb, dram=rr)
                self.DVE(lambda: nc.vector.tensor_scalar(out=htmp[:], in0=ha[:], scalar1=hs[:, 2 * j:2 * j + 1], scalar2=None,
                                                          op0=ALU.mult), [ha, hs], [htmp])
                self.DVE(lambda: nc.vector.scalar_tensor_tensor(out=uh[:, :, 0:32], in0=hb[:], scalar=hs[:, 2 * j + 1:2 * j + 2],
                                                                 in1=htmp[:], op0=ALU.mult, op1=ALU.add), [hb, hs, htmp], [uh])

        def conv(j):
            yc, ycb, sqb = ycs.next(), ycbs.next(), sqbs.next()
            ybuf[j] = (yc, ycb, sqb)
            uh = uhb.pop(j)
            for blk in range(4):
                ps = cps.next()
                for k in range(CK):
                    self.PE(lambda: nc.tensor.matmul(ps[:], lhsT=diagw[:, blk, k, :], rhs=uh[:, blk, 2 + k:2 + k + CH],
                                                     start=(k == 0), stop=(k == CK - 1)), [diagw, uh], [ps])
                self.ACT(lambda: nc.scalar.activation(out=yc[:, blk, :], in_=ps[:], func=AF.Identity, bias=cv[:, 0, blk:blk + 1]),
                         [ps, cv], [yc])
                self.ACT(lambda: nc.scalar.activation(out=sqb[:, blk, :], in_=ps[:], func=AF.Square, bias=cv[:, 0, blk:blk + 1]),
                         [ps, cv], [sqb])
                self.POOL(lambda: nc.gpsimd.tensor_copy(out=ycb[:, blk, :], in_=yc[:, blk, :]), [yc], [ycb])

        def stats(j):
            sl = slice(j * CH, (j + 1) * CH)
            yc, ycb, sqb = ybuf.pop(j)
            for blk in range(4):
                self.PE(lambda: nc.tensor.matmul(ps_mu[:], lhsT=self.ones_b[:], rhs=ycb[:, blk, :], start=(blk == 0), stop=(blk == 3)),
                        [self.ones_b, ycb], [ps_mu])
            for blk in range(4):
                self.PE(lambda: nc.tensor.matmul(ps_m2[:], lhsT=self.ones_b[:], rhs=sqb[:, blk, :], start=(blk == 0), stop=(blk == 3)),
                        [self.ones_b, sqb], [ps_m2])
            self.DVE(lambda: nc.vector.tensor_scalar(out=mu[:], in0=ps_mu[:], scalar1=1.0 / CC, scalar2=None, op0=ALU.mult), [ps_mu], [mu])
            self.DVE(lambda: nc.vector.tensor_tensor(out=msq[:], in0=mu[:], in1=mu[:], op=ALU.mult), [mu], [msq])
            self.DVE(lambda: nc.vector.scalar_tensor_tensor(out=rs[:], in0=ps_m2[:], scalar=1.0 / CC, in1=msq[:],
                                                             op0=ALU.mult, op1=ALU.subtract), [ps_m2, msq], [rs])
            self.DVE(lambda: nc.vector.tensor_scalar(out=rs[:], in0=rs[:], scalar1=EPS, scalar2=None, op0=ALU.add), [rs], [rs])
            self.ACT(lambda: nc.scalar.activation(out=rs[:], in_=rs[:], func=AF.Sqrt), [rs], [rs])
            self.DVE(lambda: nc.vector.reciprocal(out=rs[:], in_=rs[:]), [rs], [rs])
            uc = ucs.next()
            for blk in range(4):
                d = ds_.next()
                self.DVE(lambda: nc.vector.tensor_tensor(out=d[:], in0=yc[:, blk, :], in1=mu[:], op=ALU.subtract), [yc, mu], [d])
                self.POOL(lambda: nc.gpsimd.tensor_tensor(out=d[:], in0=d[:], in1=rs[:], op=ALU.mult), [d, rs], [d])
                self.ACT(lambda: nc.scalar.activation(out=uc[:, blk, :], in_=d[:], func=AF.Silu, scale=cv[:, 1, blk:blk + 1],
                                                      bias=cv[:, 2, blk:blk + 1]), [d, cv], [uc])
            self.store("act", self.uc_d[:, sl].rearrange("(i p) t -> p i t", p=128), uc[:], uc, dram=[self.R_uc[j]])

        prep(0)
        if self.nch > 1:
            prep(1)
        conv(0)
        for j in range(self.nch):
            if j + 1 < self.nch:
                conv(j + 1)
            if j + 2 < self.nch:
                prep(j + 2)
            stats(j)
        P.close()

    def phase_c2(self, l):
        nc = self.nc
        P = Phase(nc, self.fw)
        x_src = self.x_in if l == 0 else self.xa
        R_src = self.R_x if l == 0 else self.R_xa
        w_co, w_oa, w_out, w_r, brow = self.c2w
        rb_b = P.sb("rb_b", [128, 36], F32)
        mps = Rot([P.ps(f"mps{i}", [128, CH]) for i in range(4)])
        pr = P.ps("pr", [128, 4, 128])
        ps0 = mps.next()
        self.PE(lambda: nc.tensor.matmul(ps0[:, 0:36], lhsT=self.ones_f[0:1, :], rhs=brow[0:1, :], start=True, stop=True),
                [self.ones_f, brow], [ps0])
        self.DVE(lambda: nc.vector.tensor_copy(out=rb_b[:], in_=ps0[:, 0:36]), [ps0], [rb_b])
        W = {
            "ss": Rot([P.sb(f"ss{i}", [128, 4], F32) for i in range(2)]),
            "rstd": Rot([P.sb(f"rstd{i}", [128, 4], F32) for i in range(2)]),
            "junk": Rot([P.sb("junk", [128, D], BF16)]),
            "xn": Rot([P.sb("xn", [128, 4, D], BF16)]),
            "pT": Rot([P.ps(f"pT{i}", [128, 2 * CH], BF16) for i in range(2)]),
        }
        xts = Rot([P.sb(f"xt{i}", [128, D], F32) for i in range(9)])
        ucs = Rot([P.sb(f"ucl{i}", [128, 4, CH], BF16) for i in range(2)])
        ats = Rot([P.sb(f"atl{i}", [128, 4, CH], BF16) for i in range(2)])
        gcs = [P.sb(f"gc{i}", [128, 16, CH], BF16) for i in range(2)]
        t1s = Rot([P.sb(f"t1{i}", [128, CH], F32) for i in range(2)])
        t2s = Rot([P.sb(f"t2{i}", [128, CH], F32) for i in range(2)])
        tys = Rot([P.sb(f"ty{i}", [128, CH], F32) for i in range(2)])
        mTs = [P.sb(f"mT{i}", [128, 8, CH], BF16) for i in range(2)]
        h2T = P.sb("h2T", [128, 8, CH], BF16)
        R = {k: P.sb(k, shp, F32) for k, shp in dict(
            lg=[128, 4, 36], gmax=[128, 4], geq=[128, 4, 4], gsh=[128, 4, 4], gsum=[128, 4], gval=[128, 4],
            tmp=[128, 4, 4, 8], esel=[128, 4, 8], m1=[128, 4], mask1=[128, 4, 8], esel2=[128, 4, 8], m2=[128, 4],
            mask2=[128, 4, 8], w1=[128, 4], w2=[128, 4], ws=[128, 4, 8], ws2=[128, 4, 8]).items()}
        V = nc.vector
        cst = {}

        def S1(j):
            mT, gc = mTs[j % 2], gcs[j % 2]
            sl = slice(j * CH, (j + 1) * CH)
            ucl, atl = ucs.next(), ats.next()
            self.load(self.dq.next(), ucl[:], self.uc_d[:, sl].rearrange("(i p) t -> p i t", p=128), ucl, dram=[self.R_uc[j]])
            self.load(self.dq.next(), atl[:], self.at_d[:, sl].rearrange("(i p) t -> p i t", p=128), atl, dram=[self.R_at[j]])
            self.load(self.dq.next(), gc[:], self.gt_d[:, sl].rearrange("(i p) t -> p i t", p=128), gc, dram=[self.R_gt[j]])
            xs = []
            for s_ in range(4):
                xt = xts.next()
                self.load(self.dq.next(), xt[:], x_src[j * CH + s_ * 128: j * CH + (s_ + 1) * 128, :], xt, dram=[R_src[j]])
                xs.append(xt)
            for dm in range(8):
                dsl = slice(dm * 128, (dm + 1) * 128)
                ps_c, ps_a = mps.next(), mps.next()
                for i in range(4):
                    self.PE(lambda: nc.tensor.matmul(ps_c[:], lhsT=w_co[:, i, dsl], rhs=ucl[:, i, :], start=(i == 0), stop=(i == 3)),
                            [w_co, ucl], [ps_c])
                for i in range(4):
                    self.PE(lambda: nc.tensor.matmul(ps_a[:], lhsT=w_oa[:, i, dsl], rhs=atl[:, i, :], start=(i == 0), stop=(i == 3)),
                            [w_oa, atl], [ps_a])
                t1, t2 = t1s.next(), t2s.next()
                self.DVE(lambda: V.tensor_tensor(out=t1[:], in0=ps_a[:], in1=gc[:, dm, :], op=ALU.mult), [ps_a, gc], [t1])
                self.DVE(lambda: V.tensor_tensor(out=t2[:], in0=ps_c[:], in1=gc[:, 8 + dm, :], op=ALU.mult), [ps_c, gc], [t2])
                self.POOL(lambda: nc.gpsimd.tensor_tensor(out=mT[:, dm, :], in0=t1[:], in1=t2[:], op=ALU.add), [t1, t2], [mT])
            cst[j] = (xs, mT)

        def S2(j):
            xs, mT = cst[j]
            for s_ in range(4):
                for hf in range(2):
                    hsl = slice(hf * 512, (hf + 1) * 512)
                    ps_y = mps.next()
                    for dm in range(8):
                        self.PE(lambda: nc.tensor.matmul(ps_y[:], lhsT=mT[:, dm, s_ * 128:(s_ + 1) * 128], rhs=w_out[:, dm, hsl],
                                                         start=(dm == 0), stop=(dm == 7)), [mT, w_out], [ps_y])
                    ty = tys.next()
                    self.DVE(lambda: V.tensor_tensor(out=ty[:], in0=ps_y[:], in1=self.gtb[:, 0, hsl], op=ALU.mult),
                             [ps_y, self.gtb], [ty])
                    self.POOL(lambda: nc.gpsimd.tensor_tensor(out=xs[s_][:, hsl], in0=xs[s_][:, hsl], in1=ty[:], op=ALU.add),
                              [xs[s_], ty], [xs[s_]])
                self.store("pool", self.xa[j * CH + s_ * 128: j * CH + (s_ + 1) * 128, :], xs[s_][:], xs[s_],
                           dram=[self.R_xa[j]])

        def S34(j):
            xs, mT = cst.pop(j)
            sl = slice(j * CH, (j + 1) * CH)
            self.norm_hT(W, xs, 1, h2T)
            self.store("act", self.h2_d[:, sl].rearrange("(c p) t -> p c t", p=128), h2T[:], h2T, dram=[self.R_h2[j]])
            for s_ in range(4):
                for c in range(8):
                    self.PE(lambda: nc.tensor.matmul(pr[:, s_, 0:36], lhsT=h2T[:, c, s_ * 128:(s_ + 1) * 128], rhs=w_r[:, c, :],
                                                     start=(c == 0), stop=(c == 7)), [h2T, w_r], [pr])
            lg = R["lg"]
            self.DVE(lambda: V.tensor_tensor(out=lg[:], in0=pr[:, :, 0:36], in1=rb_b[:].unsqueeze(1).to_broadcast([128, 4, 36]),
                                             op=ALU.add), [pr, rb_b], [lg])
            gl = lg[:, :, 0:4]
            el = lg[:, :, 4:36].rearrange("p s (g e) -> p s g e", g=4)

            def b3(t, n):
                return t[:].unsqueeze(2).to_broadcast([128, 4, n])
            self.DVE(lambda: V.tensor_reduce(out=R["gmax"][:], in_=gl, axis=AX.X, op=ALU.max), [lg], [R["gmax"]])
            self.DVE(lambda: V.tensor_tensor(out=R["geq"][:], in0=gl, in1=b3(R["gmax"], 4), op=ALU.is_equal), [lg, R["gmax"]], [R["geq"]])
            self.DVE(lambda: V.tensor_tensor(out=R["gsh"][:], in0=gl, in1=b3(R["gmax"], 4), op=ALU.subtract), [lg, R["gmax"]], [R["gsh"]])
            self.ACT(lambda: nc.scalar.activation(out=R["gsh"][:], in_=R["gsh"][:], func=AF.Exp), [R["gsh"]], [R["gsh"]])
            self.DVE(lambda: V.tensor_reduce(out=R["gsum"][:], in_=R["gsh"][:], axis=AX.X, op=ALU.add), [R["gsh"]], [R["gsum"]])
            self.DVE(lambda: V.reciprocal(out=R["gval"][:], in_=R["gsum"][:]), [R["gsum"]], [R["gval"]])
            self.DVE(lambda: V.tensor_tensor(out=R["tmp"][:], in0=el, in1=R["geq"][:].unsqueeze(3).to_broadcast([128, 4, 4, 8]),
                                             op=ALU.mult), [lg, R["geq"]], [R["tmp"]])
            self.DVE(lambda: V.tensor_reduce(out=R["esel"][:], in_=R["tmp"][:].rearrange("p s g e -> p s e g"), axis=AX.X, op=ALU.add),
                     [R["tmp"]], [R["esel"]])
            self.DVE(lambda: V.tensor_reduce(out=R["m1"][:], in_=R["esel"][:], axis=AX.X, op=ALU.max), [R["esel"]], [R["m1"]])
            self.DVE(lambda: V.tensor_tensor(out=R["mask1"][:], in0=R["esel"][:], in1=b3(R["m1"], 8), op=ALU.is_equal),
                     [R["esel"], R["m1"]], [R["mask1"]])
            self.DVE(lambda: V.scalar_tensor_tensor(out=R["esel2"][:], in0=R["mask1"][:], scalar=-1e30, in1=R["esel"][:],
                                                    op0=ALU.mult, op1=ALU.add), [R["mask1"], R["esel"]], [R["esel2"]])
            self.DVE(lambda: V.tensor_reduce(out=R["m2"][:], in_=R["esel2"][:], axis=AX.X, op=ALU.max), [R["esel2"]], [R["m2"]])
            self.DVE(lambda: V.tensor_tensor(out=R["mask2"][:], in0=R["esel2"][:], in1=b3(R["m2"], 8), op=ALU.is_equal),
                     [R["esel2"], R["m2"]], [R["mask2"]])
            self.DVE(lambda: V.tensor_tensor(out=R["w1"][:], in0=R["m1"][:], in1=R["m2"][:], op=ALU.subtract), [R["m1"], R["m2"]], [R["w1"]])
            self.ACT(lambda: nc.scalar.activation(out=R["w1"][:], in_=R["w1"][:], func=AF.Sigmoid), [R["w1"]], [R["w1"]])
            self.DVE(lambda: V.tensor_scalar(out=R["w2"][:], in0=R["w1"][:], scalar1=-1.0, scalar2=1.0, op0=ALU.mult, op1=ALU.add),
                     [R["w1"]], [R["w2"]])
            self.DVE(lambda: V.tensor_tensor(out=R["w1"][:], in0=R["w1"][:], in1=R["gval"][:], op=ALU.mult), [R["w1"], R["gval"]], [R["w1"]])
            self.DVE(lambda: V.tensor_tensor(out=R["w2"][:], in0=R["w2"][:], in1=R["gval"][:], op=ALU.mult), [R["w2"], R["gval"]], [R["w2"]])
            self.DVE(lambda: V.tensor_tensor(out=R["ws"][:], in0=R["mask1"][:], in1=b3(R["w1"], 8), op=ALU.mult), [R["mask1"], R["w1"]], [R["ws"]])
            self.DVE(lambda: V.tensor_tensor(out=R["ws2"][:], in0=R["mask2"][:], in1=b3(R["w2"], 8), op=ALU.mult), [R["mask2"], R["w2"]], [R["ws2"]])
            self.DVE(lambda: V.tensor_tensor(out=R["ws"][:], in0=R["ws"][:], in1=R["ws2"][:], op=ALU.add), [R["ws"], R["ws2"]], [R["ws"]])
            cv_ = self.comb[:, j * 4:(j + 1) * 4, :].rearrange("p s (g e) -> p s g e", g=4)
            self.DVE(lambda: V.tensor_tensor(out=cv_, in0=R["geq"][:].unsqueeze(3).to_broadcast([128, 4, 4, 8]),
                                             in1=R["ws"][:].unsqueeze(2).to_broadcast([128, 4, 4, 8]), op=ALU.mult),
                     [R["geq"], R["ws"]], [self.comb])

        S1(0)
        for j in range(self.nch):
            S2(j)
            if j + 1 < self.nch:
                S1(j + 1)
            S34(j)
        if "comb" in self.debug and l == 0:
            d1 = self.dbg_out("comb", [128, (self.ntok // 128) * 32])
            self.store("sp", d1[:, :], self.comb[:].rearrange("p a b -> p (a b)"), self.comb)
        P.close()

    def phase_c(self, l):
        nc = self.nc
        PW = Phase(nc, self.fw)
        w_co = PW.sb("w_co", [128, 4, D], BF16)
        w_oa = PW.sb("w_oa", [128, 4, D], BF16)
        w_out = PW.sb("w_out", [128, 8, D], BF16)
        w_r = PW.sb("w_r", [128, 8, 36], BF16)
        brow = PW.sb("brow", [1, 36], F32)
        self.c2w = (w_co, w_oa, w_out, w_r, brow)
        self.phase_c1(l, PW)
        self.phase_c2(l)
        PW.close()

    def phase_d(self, l):
        nc = self.nc
        P = Phase(nc, self.fw)
        last = (l == self.depth - 1)
        wgs = [[P.sb(f"wg{i}{q}", [128, 8, 4 * FE], BF16) for q in range(2)] for i in range(2)]
        wus = [[P.sb(f"wu{i}{q}", [128, 8, 4 * FE], BF16) for q in range(2)] for i in range(2)]
        wds = [[P.sb(f"wd{i}{q}", [128, 4, D], BF16) for q in range(2)] for i in range(2)]

        def load_w(g):
            i = g % 2
            gv = self.e_g[l, g].rearrange("p (c e f) -> p c e f", c=8, e=NE)
            uv = self.e_u[l, g].rearrange("p (c e f) -> p c e f", c=8, e=NE)
            dv = self.e_d[l, g].rearrange("p (e n) -> p e n", e=NE)
            fl = []
            for q in range(2):
                fl.append(lambda q=q: self.load("pool", wgs[i][q][:].rearrange("p c (e f) -> p c e f", e=4),
                                                gv[:, :, q * 4:(q + 1) * 4, :], wgs[i][q]))
                fl.append(lambda q=q: self.load("pool", wus[i][q][:].rearrange("p c (e f) -> p c e f", e=4),
                                                uv[:, :, q * 4:(q + 1) * 4, :], wus[i][q]))
                fl.append(lambda q=q: self.load("pool", wds[i][q][:], dv[:, q * 4:(q + 1) * 4, :], wds[i][q]))
            return fl
        for f_ in load_w(0):
            f_()
        h2s = Rot([P.sb(f"h2{i}", [128, 8, CH], BF16) for i in range(2)])
        pas = Rot([P.ps(f"pa{i}", [128, CH]) for i in range(2)])
        pus = Rot([P.ps(f"pu{i}", [128, CH]) for i in range(2)])
        pts_ = Rot([P.ps(f"ptr{i}", [128, 2 * CH], BF16) for i in range(2)])
        pos_ = [P.ps(f"pod{i}", [128, CH]) for i in range(2)]
        sas = Rot([P.sb(f"sa{i}", [128, CH], F32) for i in range(2)])
        tts = Rot([P.sb(f"tt{i}", [128, CH], F32) for i in range(2)])
        hids = Rot([P.sb(f"hid{i}", [128, 4, FE], BF16) for i in range(2)])
        hTs = Rot([P.sb(f"hidT{i}", [128, 4, 128], BF16) for i in range(2)])
        xts = Rot([P.sb(f"xd{i}", [128, D], F32) for i in range(4)])
        tys = Rot([P.sb(f"tyd{i}", [128, CH], F32) for i in range(2)])
        if last:
            fr = P.sb("fr", [1, D], F32)
            self.load("sp", fr[:], self.fng[:, :], fr)
            fgb = P.sb("fgb", [128, D], F32)
            for hf in range(2):
                pb = pas.next()
                self.PE(lambda: nc.tensor.matmul(pb[:], lhsT=self.ones_f[0:1, :], rhs=fr[0:1, hf * 512:(hf + 1) * 512],
                                                 start=True, stop=True), [self.ones_f, fr], [pb])
                self.ACT(lambda: nc.scalar.copy(out=fgb[:, hf * 512:(hf + 1) * 512], in_=pb[:]), [pb], [fgb])
            fss = Rot([P.sb(f"fss{i}", [128, 1], F32) for i in range(2)])
            fjunk = P.sb("fjunk", [128, D], BF16)
            fos = Rot([P.sb(f"fo{i}", [128, D], F32) for i in range(2)])
        units = [(g, j, s_, eg) for g in range(NG) for j in range(self.nch) for s_ in range(4) for eg in range(2)]
        st = {}

        def GU(n):
            g, j, s_, eg = units[n]
            if eg == 1:
                xt = xts.next()
                rows_ = slice(j * CH + s_ * 128, j * CH + (s_ + 1) * 128)
                self.load(self.dq.next(), xt[:], self.xa[rows_, :], xt, dram=[self.R_xa[j]])
                st[("xt", n)] = xt
            def fetch_h2(jj):
                h2_ = h2s.next()
                sl_ = slice(jj * CH, (jj + 1) * CH)
                self.load(self.dq.next(), h2_[:], self.h2_d[:, sl_].rearrange("(c p) t -> p c t", p=128), h2_, dram=[self.R_h2[jj]])
                return h2_
            if s_ == 0 and eg == 0:
                st["h2"] = st.pop("h2next") if "h2next" in st else fetch_h2(j)
            if s_ == 2 and eg == 0 and n + 4 < len(units):
                st["h2next"] = fetch_h2(units[n + 4][1])
            h2 = st["h2"]
            wg, wu = wgs[g % 2][eg], wus[g % 2][eg]
            tsl = slice(s_ * 128, (s_ + 1) * 128)
            pa, pu = pas.next(), pus.next()
            for c in range(8):
                self.PE(lambda: nc.tensor.matmul(pa[:], lhsT=h2[:, c, tsl], rhs=wg[:, c, :], start=(c == 0), stop=(c == 7)),
                        [h2, wg], [pa])
            for c in range(8):
                self.PE(lambda: nc.tensor.matmul(pu[:], lhsT=h2[:, c, tsl], rhs=wu[:, c, :], start=(c == 0), stop=(c == 7)),
                        [h2, wu], [pu])
            st[("pp", n)] = (pa, pu)

        def MID(n):
            g, j, s_, eg = units[n]
            ti = j * 4 + s_
            pa, pu = st.pop(("pp", n))
            sa, tt, hid, hT = sas.next(), tts.next(), hids.next(), hTs.next()
            self.ACT(lambda: nc.scalar.activation(out=sa[:], in_=pa[:], func=AF.Silu), [pa], [sa])
            for e in range(4):
                ce = g * 8 + eg * 4 + e
                self.DVE(lambda: nc.vector.scalar_tensor_tensor(out=hid[:, e, :], in0=sa[:, e * FE:(e + 1) * FE],
                                                                 scalar=self.comb[:, ti, ce:ce + 1], in1=pu[:, e * FE:(e + 1) * FE],
                                                                 op0=ALU.mult, op1=ALU.mult), [sa, pu, self.comb], [hid])
            ptr = pts_.next()
            for e in range(4):
                self.PE(lambda: nc.tensor.transpose(out=ptr[:, e * 128:(e + 1) * 128], in_=hid[:, e, :], identity=self.ident[:]),
                        [hid, self.ident], [ptr])
            if n % 2 == 0:
                self.ACT(lambda: nc.scalar.copy(out=hT[:].rearrange("p e t -> p (e t)"), in_=ptr[:, 0:CH]), [ptr], [hT])
            else:
                self.DVE(lambda: nc.vector.tensor_copy(out=hT[:].rearrange("p e t -> p (e t)"), in_=ptr[:, 0:CH]), [ptr], [hT])
            st[("hT", n)] = hT

        def DN(n):
            g, j, s_, eg = units[n]
            wd = wds[g % 2][eg]
            hT = st.pop(("hT", n))
            rows = slice(j * CH + s_ * 128, j * CH + (s_ + 1) * 128)
            for hf in range(2):
                for e in range(4):
                    self.PE(lambda: nc.tensor.matmul(pos_[hf][:], lhsT=hT[:, e, :], rhs=wd[:, e, hf * 512:(hf + 1) * 512],
                                                     start=(eg == 0 and e == 0), stop=(eg == 1 and e == 3)),
                            [hT, wd], [pos_[hf]])
            if eg == 0:
                return
            xt = st.pop(("xt", n))
            for hf in range(2):
                hsl = slice(hf * 512, (hf + 1) * 512)
                ty = tys.next()
                self.DVE(lambda: nc.vector.tensor_tensor(out=ty[:], in0=pos_[hf][:], in1=self.gtb[:, 1, hsl], op=ALU.mult),
                         [pos_[hf], self.gtb], [ty])
                self.POOL(lambda: nc.gpsimd.tensor_tensor(out=xt[:, hsl], in0=xt[:, hsl], in1=ty[:], op=ALU.add), [xt, ty], [xt])
            if last and g == NG - 1:
                fs, fo = fss.next(), fos.next()
                self.ACT(lambda: nc.scalar.activation(out=fjunk[:], in_=xt[:], func=AF.Square, accum_out=fs[:]), [xt], [fjunk, fs])
                self.DVE(lambda: nc.vector.tensor_scalar(out=fs[:], in0=fs[:], scalar1=1.0 / D, scalar2=EPS, op0=ALU.mult, op1=ALU.add),
                         [fs], [fs])
                self.ACT(lambda: nc.scalar.activation(out=fs[:], in_=fs[:], func=AF.Sqrt), [fs], [fs])
                self.DVE(lambda: nc.vector.reciprocal(out=fs[:], in_=fs[:]), [fs], [fs])
                self.DVE(lambda: nc.vector.scalar_tensor_tensor(out=fo[:], in0=xt[:], scalar=fs[:, 0:1], in1=fgb[:],
                                                                 op0=ALU.mult, op1=ALU.mult), [xt, fs, fgb], [fo])
                self.store("pool", self.out[rows, :], fo[:], fo, dram=[self.R_out[j]])
            else:
                self.store("pool", self.xa[rows, :], xt[:], xt, dram=[self.R_xa[j]])

        N = len(units)
        per_stage = N // NG
        wq = load_w(1)
        GU(0)
        for n in range(N):
            if n + 1 < N:
                GU(n + 1)
            MID(n)
            if n >= 1:
                DN(n - 1)
                if n % per_stage == 0 and n // per_stage + 1 < NG:
                    wq = load_w(n // per_stage + 1)
            if wq and n % 2 == 1:
                wq.pop(0)()
        DN(N - 1)
        P.close()


def _const_tables(npar):
    consts = np.zeros((128, 4), np.float32)
    inv_freq = (10000.0 ** (-np.arange(0, ROPE, 2, dtype=np.float32) / ROPE)).astype(np.float32)
    for r in range(32):
        consts[64 + r, 0] = inv_freq[r % 16]
        consts[64 + r, 1] = -1.0 if r < 16 else 1.0
    ident = np.eye(128, dtype=np.float32)
    pk = np.arange(128)[:, None]
    f = np.arange(CH)[None, :]
    diag = [((pk + d * 128) <= f).astype(np.float32) for d in range(4)]
    zeros = np.zeros((128, CH), np.float32)
    ones = np.ones((128, CH), np.float32)
    lo = diag + [zeros] * 4
    hi = [ones] * 4 + diag
    return consts, ident, np.stack(lo), np.stack(hi)


def prep_inputs(inputs, npar, depth=DEPTH, used=None):
    f32 = np.float32
    L = DEPTH
    consts, ident, m_lo, m_hi = _const_tables(npar)
    g = lambda k: np.asarray(inputs[k])
    col = lambda v, n: np.ascontiguousarray(v.reshape(L, n, 128).transpose(0, 2, 1)).astype(f32)
    shared = {
        "consts": consts, "ident": ident,
        "ada_w": g("ada_w"), "ada_b": g("ada_b").reshape(L, 1, 6 * D),
        "n1g": col(g("norm1_g"), 8), "n2g": col(g("norm2_g"), 8),
        "w_in": g("w_in"), "qng": col(g("q_norm_g"), 3), "w_uq": g("w_uq"),
        "kvng": col(g("kv_norm_g"), 2), "w_ukv": g("w_ukv"), "w_oa": g("w_o_attn"),
        "conv_w": np.ascontiguousarray(g("conv_w").transpose(0, 2, 1).reshape(L, 4, 128, CK).transpose(0, 2, 1, 3)),
        "conv_v": np.ascontiguousarray(np.stack([g("conv_b"), g("conv_ln_g"), g("conv_ln_b")], 1)
                                       .reshape(L, 3, 4, 128).transpose(0, 3, 1, 2)),
        "w_co": g("w_conv_out"), "w_out": g("w_out"),
        "w_r": np.ascontiguousarray(np.concatenate([g("router_group_w"), g("router_expert_w")], -1)),
        "b_r": np.concatenate([g("router_group_b"), g("router_expert_b")], -1).reshape(L, 1, 36),
        "e_g": np.ascontiguousarray(g("expert_w_gate").reshape(L, NG, NE, 8, 128, FE).transpose(0, 1, 4, 3, 2, 5)
                                    ).reshape(L, NG, 128, 8 * NE * FE),
        "e_u": np.ascontiguousarray(g("expert_w_up").reshape(L, NG, NE, 8, 128, FE).transpose(0, 1, 4, 3, 2, 5)
                                    ).reshape(L, NG, 128, 8 * NE * FE),
        "e_d": np.ascontiguousarray(g("expert_w_down").transpose(0, 1, 3, 2, 4)).reshape(L, NG, 128, NE * D),
        "fng": g("final_norm_g").reshape(1, D),
    }
    x = g("x")
    c = g("c")
    pos = g("positions")
    nch = 16 // npar
    maps = []
    for core in range(8):
        b, p = core // 2, core % 2
        if npar == 1:
            chunks = list(range(16))
        else:
            chunks = own_chunks(2, p)
        rows = np.concatenate([np.arange(cj * CH, (cj + 1) * CH) for cj in chunks])
        masks = np.zeros((2, 8, 128, CH), ml_dtypes.bfloat16)
        hsel = np.zeros((1, 2 * nch), f32)
        for j in range(nch):
            if npar == 1:
                masks[0, :4] = m_lo[:4]
                masks[1, :4] = m_lo[:4]
                hsel[0, 2 * j] = 1.0
            else:
                is_hi = (j + p) % 2 == 1
                masks[j % 2] = m_hi if is_hi else m_lo
                hsel[0, 2 * j] = 0.0 if is_hi else 1.0
                hsel[0, 2 * j + 1] = 1.0 if is_hi else 0.0
        m = {k: (v[:depth] if (v.ndim >= 3 and v.shape[0] == L and k not in ("masks",)) else v) for k, v in shared.items()}
        m["x_own"] = np.ascontiguousarray(x[b][rows])
        m["pos_own"] = np.ascontiguousarray(pos[b][rows]).reshape(1, -1).astype(np.int32)
        m["c_col"] = np.ascontiguousarray(c[b].reshape(8, 128).T)
        m["masks"] = masks
        m["halo_sel"] = hsel
        if used is not None:
            m = {k: v for k, v in m.items() if k in used}
        maps.append((m, b, rows))
    return maps


_PROG_CACHE = {}


def get_prog(npar, **kw):
    key = (npar, tuple(sorted((k, str(v)) for k, v in kw.items())))
    if key not in _PROG_CACHE:
        _PROG_CACHE[key] = Prog(npar=npar, **kw)
    return _PROG_CACHE[key]


NPAR = 2


def kernel(**inputs):
    prog = get_prog(NPAR)
    maps = prep_inputs(inputs, NPAR, DEPTH, set(prog.used_inputs))
    res = run_bass_kernel_spmd(prog.nc, [m for m, _, _ in maps], core_ids=list(range(8)))
    out = np.zeros((B, S, D), np.float32)
    for core, (m, b, rows) in enumerate(maps):
        if NPAR == 1 and core % 2 == 1:
            continue
        out[b][rows] = res.results[core]["out"]
    return out
```

```python
import math
from contextlib import ExitStack

import numpy as np
import ml_dtypes

import concourse.bass as bass
import concourse.mybir as mybir
from concourse.bass_utils import run_bass_kernel_spmd

F32 = mybir.dt.float32
BF16 = mybir.dt.bfloat16
I32 = mybir.dt.int32
ALU = mybir.AluOpType
AF = mybir.ActivationFunctionType
AX = mybir.AxisListType

D = 1024
NH = 8
QK = 96
NOPE = 64
ROPE = 32
VD = 64
QL = 384
KVL = 256
CC = 512
CK = 31
NG = 4
NE = 8
FE = 128
INC = 3744
S = 8192
B = 4
CH = 512
EPS = 1e-6
DEPTH = 2
TWO_PI = 2.0 * math.pi
CW1 = 6.28125
CW2 = TWO_PI - CW1

EPOCH = 12000


class Res:
    __slots__ = ("name", "w", "r", "dsem", "dcnt", "excl")

    def __init__(self, name):
        self.excl = False
        self.name = name
        self.w = {}
        self.r = {}
        self.dsem = None
        self.dcnt = 0


def _res(x):
    return x.r if isinstance(x, TL) else x


class FW:
    def __init__(self, nc, same_engine_sync=True):
        self.nc = nc
        self.eng = {"pe": nc.tensor, "act": nc.scalar, "dve": nc.vector,
                    "pool": nc.gpsimd, "sp": nc.sync}
        self.esem = {}
        self.ecnt = {}
        self.waited = {k: {} for k in self.eng}
        self.same = same_engine_sync
        self.nsem = 0
        self.all_sems = {}
        self.free_dsems = []
        self.ninst = {k: 0 for k in self.eng}
        for k in self.eng:
            self._new_epoch(k)

    def _alloc_sem(self, name):
        s = self.nc.alloc_semaphore(name)
        self.nsem += 1
        return s

    def _new_epoch(self, k):
        self.esem[k] = self._alloc_sem(f"e_{k}_{self.nsem}")
        self.ecnt[k] = 0

    def _collect(self, reads, writes, skip_waw_sem=None):
        deps = {}

        def add(tok):
            key = id(tok[0])
            if key not in deps or deps[key][1] < tok[1]:
                deps[key] = tok
        for r in reads:
            for tok in r.w.values():
                add(tok)
        for w in writes:
            for tok in w.w.values():
                if skip_waw_sem is not None and tok[0] is skip_waw_sem:
                    continue
                add(tok)
            for tok in w.r.values():
                add(tok)
        return deps

    def _wait(self, k, deps, skip_own=False):
        e = self.eng[k]
        wd = self.waited[k]
        for key, (sem, val) in deps.items():
            if skip_own and sem is self.esem[k]:
                continue
            if wd.get(key, 0) >= val:
                continue
            e.wait_ge(sem, val)
            self.ninst[k] += 1
            wd[key] = val

    def _record(self, tok, reads, writes, partial=False):
        key = id(tok[0])
        self.all_sems[key] = (tok[0], tok[1])
        for r in reads:
            r.r[key] = tok
        for w in writes:
            if partial:
                w.w[key] = tok
            else:
                w.w = {key: tok}
            w.r = {}

    def op(self, k, ins_fn, reads=(), writes=()):
        reads = [_res(x) for x in reads]
        writes = [_res(x) for x in writes]
        writes = writes + [x for x in reads if x.excl]
        reads = [x for x in reads if not x.excl]
        deps = self._collect(reads, writes)
        skip_own = (k == "pe") or (not self.same)
        self._wait(k, deps, skip_own=skip_own)
        ins = ins_fn()
        if self.ecnt[k] >= EPOCH:
            self._new_epoch(k)
        self.ecnt[k] += 1
        ins.then_inc(self.esem[k], 1)
        self.ninst[k] += 1
        tok = (self.esem[k], self.ecnt[k])
        self._record(tok, reads, writes)
        return ins

    def dma(self, q, out, in_, sb, reads=(), writes=(), **kw):
        sb = _res(sb)
        reads = [_res(x) for x in reads]
        writes = [_res(x) for x in writes]
        deps = self._collect(reads, writes, skip_waw_sem=sb.dsem)
        self._wait(q, deps)
        if sb.dsem is None or sb.dcnt >= 30000:
            if self.free_dsems:
                sb.dsem, sb.dcnt = self.free_dsems.pop()
            else:
                sb.dsem = self._alloc_sem(f"d_{sb.name}_{self.nsem}")
                sb.dcnt = 0
        sb.dcnt += 16
        ins = self.eng[q].dma_start(out=out, in_=in_, **kw)
        ins.then_inc(sb.dsem, 16)
        self.ninst[q] += 1
        tok = (sb.dsem, sb.dcnt)
        self._record(tok, reads, writes, partial=True)
        return ins

    def barrier(self):
        deps = dict(self.all_sems)
        for k in self.eng:
            self._wait(k, {kk: v for kk, v in deps.items()})


class TL:
    __slots__ = ("t", "r")

    def __init__(self, t, name, excl=False):
        self.t = t
        self.r = Res(name)
        self.r.excl = excl

    def __getitem__(self, key):
        return self.t[key]


class Phase:
    def __init__(self, nc, fw):
        self.nc = nc
        self.fw = fw
        self.es = ExitStack()
        self.n = 0
        self.tiles = []

    def sb(self, name, shape, dtype):
        self.n += 1
        t = self.es.enter_context(self.nc.sbuf_tensor(f"{name}_{id(self) % 100000}_{self.n}", list(shape), dtype))
        tl = TL(t, name)
        self.tiles.append(tl)
        return tl

    def ps(self, name, shape, dtype=F32):
        self.n += 1
        t = self.es.enter_context(self.nc.psum_tensor(f"{name}_{id(self) % 100000}_{self.n}", list(shape), dtype))
        return TL(t, name, excl=True)

    def close(self):
        self.fw.barrier()
        for tl in self.tiles:
            if tl.r.dsem is not None and tl.r.dcnt < 30000:
                self.fw.free_dsems.append((tl.r.dsem, tl.r.dcnt))
                tl.r.dsem = None
        self.es.close()


class Rot:
    def __init__(self, tiles):
        self.tiles = tiles
        self.i = 0

    def next(self):
        t = self.tiles[self.i % len(self.tiles)]
        self.i += 1
        return t


def own_chunks(npar, p):
    if npar == 1:
        return list(range(16))
    return [2 * j + ((j + p) % 2) for j in range(8)]


class Prog:
    def __init__(self, npar=2, depth=DEPTH, debug=None, stop_after=None, a_chunks=None, a_stop=99):
        self.a_chunks = a_chunks
        self.a_stop = a_stop
        self.npar = npar
        self.depth = depth
        self.debug = debug or set()
        self.stop_after = stop_after
        self.nch = 16 // npar
        self.ntok = self.nch * CH
        self.nc = bass.Bass("TRN2", target_bir_lowering=False)
        self.fw = FW(self.nc)
        self.dq = Rot(["sp"])
        self.build()

    def PE(self, fn, r=(), w=()):
        return self.fw.op("pe", fn, r, w)

    def ACT(self, fn, r=(), w=()):
        return self.fw.op("act", fn, r, w)

    def DVE(self, fn, r=(), w=()):
        return self.fw.op("dve", fn, r, w)

    def POOL(self, fn, r=(), w=()):
        return self.fw.op("pool", fn, r, w)

    def load(self, q, out_ap, in_ap, tile, dram=(), **kw):
        return self.fw.dma(q, out_ap, in_ap, tile, reads=list(dram), writes=[tile], **kw)

    def store(self, q, out_ap, in_ap, tile, dram=(), **kw):
        return self.fw.dma(q, out_ap, in_ap, tile, reads=[tile], writes=list(dram), **kw)

    def din(self, name, shape, dtype=F32):
        return self.nc.dram_tensor(name, list(shape), dtype, kind="ExternalInput").ap()

    def dscr(self, name, shape, dtype):
        kind = "ExternalOutput" if name in self.debug else "Internal"
        return self.nc.dram_tensor(name, list(shape), dtype, kind=kind).ap()

    def __getattr__(self, name):
        specs = self.__dict__.get("_specs", {})
        if name in specs:
            ap = self.din(*specs[name])
            self.__dict__[name] = ap
            self.used_inputs.append(specs[name][0])
            return ap
        raise AttributeError(name)

    def declare(self):
        self._specs = {}
        self.used_inputs = []
        L = self.depth
        nt = self.ntok
        self._specs["x_in"] = ("x_own", [nt, D])
        self._specs["pos_in"] = ("pos_own", [1, nt], I32)
        self._specs["c_col"] = ("c_col", [128, 8])
        self._specs["consts"] = ("consts", [128, 4])
        self._specs["ident_in"] = ("ident", [128, 128])
        self._specs["masks_in"] = ("masks", [2, 8, 128, CH], BF16)
        self._specs["halo_sel"] = ("halo_sel", [1, 2 * self.nch])
        self._specs["ada_w"] = ("ada_w", [L, D, 6 * D])
        self._specs["ada_b"] = ("ada_b", [L, 1, 6 * D])
        self._specs["n1g"] = ("n1g", [L, 128, 8])
        self._specs["n2g"] = ("n2g", [L, 128, 8])
        self._specs["w_in"] = ("w_in", [L, D, INC])
        self._specs["qng"] = ("qng", [L, 128, 3])
        self._specs["w_uq"] = ("w_uq", [L, QL, NH * QK])
        self._specs["kvng"] = ("kvng", [L, 128, 2])
        self._specs["w_ukv"] = ("w_ukv", [L, KVL, NH * 128])
        self._specs["w_oa"] = ("w_oa", [L, CC, D])
        self._specs["conv_w"] = ("conv_w", [L, 128, 4, CK])
        self._specs["conv_v"] = ("conv_v", [L, 128, 3, 4])
        self._specs["w_co"] = ("w_co", [L, CC, D])
        self._specs["w_out"] = ("w_out", [L, D, D])
        self._specs["w_r"] = ("w_r", [L, D, 36])
        self._specs["b_r"] = ("b_r", [L, 1, 36])
        self._specs["e_g"] = ("e_g", [L, NG, 128, 8 * NE * FE])
        self._specs["e_u"] = ("e_u", [L, NG, 128, 8 * NE * FE])
        self._specs["e_d"] = ("e_d", [L, NG, 128, NE * D])
        self._specs["fng"] = ("fng", [1, D])
        self.out = self.nc.dram_tensor("out", [nt, D], F32, kind="ExternalOutput").ap()
        self.xa = self.dscr("xa", [nt, D], F32)
        self.cs_d = self.dscr("cs_d", [2, ROPE, nt], F32)
        self.qT_d = self.dscr("qT_d", [NH * QK, nt], BF16)
        self.nparts = nt // 2048
        self.ex_d = [self.dscr(f"ex_d{i}", [KVL + ROPE, 2048], BF16) for i in range(self.nparts)]
        self.exh_d = self.dscr("exh_d", [CC, self.nch * 32], BF16)
        self.uT_d = self.dscr("uT_d", [CC, nt], BF16)
        self.gt_d = self.dscr("gt_d", [2 * D, nt], BF16)
        self.at_d = self.dscr("at_d", [NH * VD, nt], BF16)
        self.h2_d = self.dscr("h2_d", [D, nt], BF16)
        self.uc_d = self.dscr("uc_d", [CC, nt], BF16)
        if self.npar == 2:
            self.exg_d = [self.dscr(f"exg_d{i}", [2 * (KVL + ROPE), 2048], BF16) for i in range(self.nparts)]
            self.exhg_d = self.dscr("exhg_d", [2 * CC, self.nch * 32], BF16)
        n = self.nch
        self.R_x = [Res(f"Rx{j}") for j in range(n)]
        self.R_xa = [Res(f"Rxa{j}") for j in range(n)]
        self.R_cs = [Res(f"Rcs{j}") for j in range(n)]
        self.R_q = [Res(f"Rq{j}") for j in range(n)]
        self.R_ex = [Res(f"Rex{j}") for j in range(n)]
        self.R_exh = [Res(f"Rexh{j}") for j in range(n)]
        self.R_u = [Res(f"Ru{j}") for j in range(n)]
        self.R_gt = [Res(f"Rgt{j}") for j in range(n)]
        self.R_at = [Res(f"Rat{j}") for j in range(n)]
        self.R_h2 = [Res(f"Rh2{j}") for j in range(n)]
        self.R_out = [Res(f"Rout{j}") for j in range(n)]
        self.R_uc = [Res(f"Ruc{j}") for j in range(n)]
        self.R_exg = Res("Rexg")
        self.dbg = {}

    def dbg_out(self, name, shape, dtype=F32):
        ap = self.nc.dram_tensor("dbg_" + name, list(shape), dtype, kind="ExternalOutput").ap()
        self.dbg[name] = ap
        return ap

    def build(self):
        nc, fw = self.nc, self.fw
        self.declare()
        G = Phase(nc, fw)
        self.G = G
        self.ident_f = G.sb("ident_f", [128, 128], F32)
        self.ident = G.sb("ident", [128, 128], BF16)
        self.ones_f = G.sb("ones_f", [128, 128], F32)
        self.ones_b = G.sb("ones_b", [128, 128], BF16)
        self.cst = G.sb("cst", [128, 4], F32)
        self.load("sp", self.ident_f[:], self.ident_in[:, :], self.ident_f)
        self.load("sp", self.cst[:], self.consts[:, :], self.cst)
        self.DVE(lambda: nc.vector.tensor_copy(out=self.ident[:], in_=self.ident_f[:]), [self.ident_f], [self.ident])
        self.POOL(lambda: nc.gpsimd.memset(self.ones_f[:], 1.0), [], [self.ones_f])
        self.POOL(lambda: nc.gpsimd.memset(self.ones_b[:], 1.0), [], [self.ones_b])
        self.modc = G.sb("modc", [128, 4, 8], F32)
        self.gtb = G.sb("gtb", [128, 2, D], F32)
        self.comb = G.sb("comb", [128, self.ntok // 128, NG * NE], F32)
        self.cact = G.sb("cact", [128, 8], F32)
        self.load("sp", self.cact[:], self.c_col[:, :], self.cact)
        self.ACT(lambda: nc.scalar.activation(out=self.cact[:], in_=self.cact[:], func=AF.Silu), [self.cact], [self.cact])

        PR = self.rope_tables()
        if self.stop_after == "rope":
            PR.close()
            return self.finish()
        for l in range(self.depth):
            self.layer_mod(l)
            if l == 0:
                PR.close()
            if self.stop_after == "mod":
                return self.finish()
            self.phase_a(l)
            if self.stop_after == "a":
                return self.finish()
            self.exchange(l)
            self.phase_b(l)
            if self.stop_after == "b":
                return self.finish()
            self.phase_c(l)
            if self.stop_after == "c":
                return self.finish()
            self.phase_d(l)
        return self.finish()

    def finish(self):
        self.fw.barrier()
        print("ninst", self.fw.ninst, "nsem", self.fw.nsem)

    def rope_tables(self):
        nc = self.nc
        P = Phase(nc, self.fw)
        lo, hi = 64, 96
        pis = Rot([P.sb(f"pi{i}", [128, CH], I32) for i in range(2)])
        as_ = Rot([P.sb(f"a{i}", [128, CH], F32) for i in range(2)])
        t = P.sb("t", [128, CH], F32)
        ki = P.sb("ki", [128, CH], I32)
        kf = P.sb("kf", [128, CH], F32)
        r = P.sb("r", [128, CH], F32)
        m = P.sb("m", [128, CH], F32)
        os_ = Rot([P.sb(f"o{i}", [128, CH], F32) for i in range(2)])
        V = nc.vector
        for j in range(self.nch):
            sl = slice(j * CH, (j + 1) * CH)
            pi = pis.next()
            a = as_.next()
            self.load("sp", pi[lo:hi, :], self.pos_in[0:1, sl].partition_broadcast(32), pi)
            self.DVE(lambda: V.tensor_copy(out=a[lo:hi, :], in_=pi[lo:hi, :]), [pi], [a])
            self.DVE(lambda: V.tensor_scalar(out=a[lo:hi, :], in0=a[lo:hi, :], scalar1=self.cst[lo:hi, 0:1],
                                             scalar2=None, op0=ALU.mult), [a, self.cst], [a])
            for ti, ph in ((1, 0.0), (0, 0.5 * math.pi)):
                o = os_.next()
                self.DVE(lambda: V.tensor_scalar(out=t[lo:hi, :], in0=a[lo:hi, :], scalar1=ph, scalar2=1.0 / TWO_PI,
                                                 op0=ALU.add, op1=ALU.mult), [a], [t])
                self.DVE(lambda: V.tensor_copy(out=ki[lo:hi, :], in_=t[lo:hi, :]), [t], [ki])
                self.DVE(lambda: V.tensor_copy(out=kf[lo:hi, :], in_=ki[lo:hi, :]), [ki], [kf])
                self.DVE(lambda: V.scalar_tensor_tensor(out=r[lo:hi, :], in0=kf[lo:hi, :], scalar=-CW1, in1=a[lo:hi, :],
                                                        op0=ALU.mult, op1=ALU.add), [kf, a], [r])
                self.DVE(lambda: V.tensor_scalar(out=r[lo:hi, :], in0=r[lo:hi, :], scalar1=ph, scalar2=None,
                                                 op0=ALU.add), [r], [r])
                self.DVE(lambda: V.scalar_tensor_tensor(out=r[lo:hi, :], in0=kf[lo:hi, :], scalar=-CW2, in1=r[lo:hi, :],
                                                        op0=ALU.mult, op1=ALU.add), [kf, r], [r])
                self.DVE(lambda: V.tensor_scalar(out=m[lo:hi, :], in0=r[lo:hi, :], scalar1=math.pi, scalar2=None,
                                                 op0=ALU.is_gt), [r], [m])
                self.DVE(lambda: V.scalar_tensor_tensor(out=r[lo:hi, :], in0=m[lo:hi, :], scalar=-TWO_PI, in1=r[lo:hi, :],
                                                        op0=ALU.mult, op1=ALU.add), [m, r], [r])
                self.DVE(lambda: V.tensor_scalar(out=m[lo:hi, :], in0=r[lo:hi, :], scalar1=-math.pi, scalar2=None,
                                                 op0=ALU.is_lt), [r], [m])
                self.DVE(lambda: V.scalar_tensor_tensor(out=r[lo:hi, :], in0=m[lo:hi, :], scalar=TWO_PI, in1=r[lo:hi, :],
                                                        op0=ALU.mult, op1=ALU.add), [m, r], [r])
                self.DVE(lambda: V.tensor_scalar(out=r[lo:hi, :], in0=r[lo:hi, :], scalar1=-math.pi, scalar2=math.pi,
                                                 op0=ALU.max, op1=ALU.min), [r], [r])
                if ti == 1:
                    self.ACT(lambda: nc.scalar.activation(out=o[lo:hi, :], in_=r[lo:hi, :], func=AF.Sin,
                                                          scale=self.cst[lo:hi, 1:2]), [r, self.cst], [o])
                else:
                    self.ACT(lambda: nc.scalar.activation(out=o[lo:hi, :], in_=r[lo:hi, :], func=AF.Sin), [r], [o])
                self.store("act", self.cs_d[ti, :, sl], o[lo:hi, :], o, dram=[self.R_cs[j]])
        return P

    def layer_mod(self, l):
        nc = self.nc
        P = Phase(nc, self.fw)
        row = P.sb("adarow", [1, 6 * D], F32)
        brow = P.sb("adab", [1, 6 * D], F32)
        self.load("sp", brow[:], self.ada_b[l, :, :], brow)
        wv = self.ada_w[l].rearrange("(c p) n -> p c n", p=128)
        wts = Rot([P.sb(f"adaw{i}", [128, 8, 512], F32) for i in range(4)])
        pss = Rot([P.ps(f"adaps{i}", [128, 512]) for i in range(2)])
        for n in range(12):
            wt = wts.next()
            ps = pss.next()
            self.load(self.dq.next(), wt[:], wv[:, :, n * 512:(n + 1) * 512], wt)
            for c in range(8):
                self.PE(lambda c=c: nc.tensor.matmul(ps[0:1, :], lhsT=self.cact[:, c:c + 1], rhs=wt[:, c, :],
                                                     start=(c == 0), stop=(c == 7)), [self.cact, wt], [ps])
            self.DVE(lambda: nc.vector.tensor_tensor(out=row[:, n * 512:(n + 1) * 512], in0=ps[0:1, :],
                                                      in1=brow[:, n * 512:(n + 1) * 512], op=ALU.add), [ps, brow], [row])
        g1 = P.sb("g1", [128, 8], F32)
        g2 = P.sb("g2", [128, 8], F32)
        self.load("sp", g1[:], self.n1g[l], g1)
        self.load("sp", g2[:], self.n2g[l], g2)
        pc = P.ps("pc", [128, 128, 4])
        for vi, off in enumerate((0, 1, 3, 4)):
            for c in range(8):
                col = off * D + c * 128
                self.PE(lambda vi=vi, c=c, col=col: nc.tensor.matmul(pc[:, vi * 8 + c, 0:1], lhsT=row[0:1, col:col + 128],
                                                                     rhs=self.ones_f[0:1, 0:1], start=True, stop=True),
                        [row, self.ones_f], [pc])
        cols = P.sb("cols", [128, 4, 8], F32)
        self.DVE(lambda: nc.vector.tensor_copy(out=cols[:].rearrange("p a b -> p (a b)"), in_=pc[:, 0:32, 0]), [pc], [cols])
        for k, (g, sci, shi) in enumerate(((g1, 1, 0), (g2, 3, 2))):
            self.DVE(lambda g=g, sci=sci, k=k: nc.vector.scalar_tensor_tensor(
                out=self.modc[:, 2 * k, :], in0=cols[:, sci, :], scalar=1.0, in1=g[:], op0=ALU.add, op1=ALU.mult),
                [cols, g], [self.modc])
            self.DVE(lambda shi=shi, k=k: nc.vector.tensor_copy(out=self.modc[:, 2 * k + 1, :], in_=cols[:, shi, :]),
                     [cols], [self.modc])
        pbs = Rot([P.ps(f"pbb{i}", [128, 512]) for i in range(2)])
        for k, off in enumerate((2, 5)):
            for hf in range(2):
                pbb = pbs.next()
                col = off * D + hf * 512
                self.PE(lambda: nc.tensor.matmul(pbb[:], lhsT=self.ones_f[0:1, :], rhs=row[0:1, col:col + 512],
                                                 start=True, stop=True), [row, self.ones_f], [pbb])
                self.ACT(lambda: nc.scalar.copy(out=self.gtb[:, k, hf * 512:(hf + 1) * 512], in_=pbb[:]),
                         [pbb], [self.gtb])
        if "mod" in self.debug and l == 0:
            d1 = self.dbg_out("modc", [128, 32])
            d2 = self.dbg_out("gtb", [128, 2 * D])
            self.store("sp", d1[:, :], self.modc[:].rearrange("p a b -> p (a b)"), self.modc)
            self.store("sp", d2[:, :], self.gtb[:].rearrange("p a b -> p (a b)"), self.gtb)
        P.close()

    def norm_hT(self, W, xs, k, hT):
        nc = self.nc
        ss = W["ss"].next()
        for s_ in range(4):
            junk = W["junk"].next()
            self.ACT(lambda: nc.scalar.activation(out=junk[:], in_=xs[s_][:], func=AF.Square,
                                                  accum_out=ss[:, s_:s_ + 1]), [xs[s_]], [junk, ss])
        rstd = W["rstd"].next()
        self.DVE(lambda: nc.vector.tensor_scalar(out=rstd[:], in0=ss[:], scalar1=1.0 / D, scalar2=EPS,
                                                  op0=ALU.mult, op1=ALU.add), [ss], [rstd])
        self.ACT(lambda: nc.scalar.activation(out=rstd[:], in_=rstd[:], func=AF.Sqrt), [rstd], [rstd])
        self.DVE(lambda: nc.vector.reciprocal(out=rstd[:], in_=rstd[:]), [rstd], [rstd])
        xn = W["xn"].next()
        for s_ in range(4):
            self.DVE(lambda: nc.vector.tensor_scalar(out=xn[:, s_, :], in0=xs[s_][:], scalar1=rstd[:, s_:s_ + 1],
                                                      scalar2=None, op0=ALU.mult), [xs[s_], rstd], [xn])
        for c in range(8):
            pT = W["pT"].next()
            for s_ in range(4):
                self.PE(lambda: nc.tensor.transpose(out=pT[:, s_ * 128:(s_ + 1) * 128],
                                                    in_=xn[:, s_, c * 128:(c + 1) * 128], identity=self.ident[:]),
                        [xn, self.ident], [pT])
            if c % 2 == 0:
                self.DVE(lambda: nc.vector.tensor_scalar(out=hT[:, c, :], in0=pT[:, 0:CH], scalar1=self.modc[:, 2 * k, c:c + 1],
                                                          scalar2=self.modc[:, 2 * k + 1, c:c + 1], op0=ALU.mult, op1=ALU.add),
                         [pT, self.modc], [hT])
            else:
                self.ACT(lambda: nc.scalar.activation(out=hT[:, c, :], in_=pT[:, 0:CH], func=AF.Identity,
                                                      scale=self.modc[:, 2 * k, c:c + 1], bias=self.modc[:, 2 * k + 1, c:c + 1]),
                         [pT, self.modc], [hT])

    def rstd_bcast(self, ps, n, out, tmp):
        nc = self.nc
        self.DVE(lambda: nc.vector.tensor_scalar(out=out[:], in0=ps[:], scalar1=1.0 / n, scalar2=EPS,
                                                  op0=ALU.mult, op1=ALU.add), [ps], [out])
        self.ACT(lambda: nc.scalar.activation(out=out[:], in_=out[:], func=AF.Sqrt), [out], [out])
        self.DVE(lambda: nc.vector.reciprocal(out=out[:], in_=out[:]), [out], [out])

    def phase_a(self, l):
        nc = self.nc
        P = Phase(nc, self.fw)
        x_src = self.x_in if l == 0 else self.xa
        R_src = self.R_x if l == 0 else self.R_xa
        scale = QK ** -0.5
        wv = self.w_in[l].rearrange("(c p) n -> p c n", p=128)
        WSPL = [0, QL + KVL + ROPE, QL + KVL + ROPE + 2 * CC, INC]
        wparts = []
        for i in range(3):
            wt_ = P.sb(f"w_in{i}", [128, 8, WSPL[i + 1] - WSPL[i]], BF16)
            self.load("pool", wt_[:], wv[:, :, WSPL[i]:WSPL[i + 1]], wt_)
            wparts.append(wt_)
        w_in = wparts[0]
        wkr = P.sb("wkr", [128, 8, 2, QK], BF16)
        self.POOL(lambda: nc.gpsimd.memset(wkr[:], 0.0), [], [wkr])
        KR0 = QL + KVL
        self.POOL(lambda: nc.gpsimd.tensor_copy(out=wkr[:, :, 0, 64:96], in_=w_in[:, :, KR0:KR0 + 32]), [w_in], [wkr])
        self.POOL(lambda: nc.gpsimd.tensor_copy(out=wkr[:, :, 1, 64:80], in_=w_in[:, :, KR0 + 16:KR0 + 32]), [w_in], [wkr])
        self.POOL(lambda: nc.gpsimd.tensor_copy(out=wkr[:, :, 1, 80:96], in_=w_in[:, :, KR0:KR0 + 16]), [w_in], [wkr])
        stg = P.sb("stg", [128, 3, NH * QK], F32)
        qg = P.sb("qg", [128, 3], F32)
        self.load("sp", stg[:], self.w_uq[l].rearrange("(c p) n -> p c n", p=128), stg)
        self.load("sp", qg[:], self.qng[l], qg)
        w_uq = P.sb("w_uq", [128, 3, NH * QK], BF16)
        w_uqr = P.sb("w_uqr", [128, 3, NH, QK], BF16)
        self.POOL(lambda: nc.gpsimd.memset(w_uqr[:], 0.0), [], [w_uqr])
        for c in range(3):
            self.DVE(lambda: nc.vector.tensor_scalar(out=w_uq[:, c, :], in0=stg[:, c, :], scalar1=qg[:, c:c + 1],
                                                      scalar2=None, op0=ALU.mult), [stg, qg], [w_uq])
            v = w_uq[:, c, :].rearrange("p (h r) -> p h r", r=QK)
            self.POOL(lambda: nc.gpsimd.tensor_copy(out=w_uqr[:, c, :, 64:80], in_=v[:, :, 80:96]), [w_uq], [w_uqr])
            self.POOL(lambda: nc.gpsimd.tensor_copy(out=w_uqr[:, c, :, 80:96], in_=v[:, :, 64:80]), [w_uq], [w_uqr])
        W = {
            "ss": Rot([P.sb(f"ss{i}", [128, 4], F32) for i in range(2)]),
            "rstd": Rot([P.sb(f"rstd{i}", [128, 4], F32) for i in range(2)]),
            "junk": Rot([P.sb("junk", [128, D], BF16)]),
            "xn": Rot([P.sb("xn", [128, 4, D], BF16)]),
            "pT": Rot([P.ps(f"pT{i}", [128, 2 * CH], BF16) for i in range(2)]),
        }
        xts = Rot([P.sb(f"xt{i}", [128, D], F32) for i in range(5)])
        hTs = [P.sb(f"hT{i}", [128, 8, CH], BF16) for i in range(2)]
        mps = Rot([P.ps(f"mps{i}", [128, CH]) for i in range(4)])
        ps_sq = P.ps("ps_sq", [128, CH])
        ps_skv = P.ps("ps_skv", [128, CH])
        sqt = P.sb("sqt", [128, 5, CH], BF16)
        qlT = P.sb("qlT", [128, 3, CH], BF16)
        kvraw = P.sb("kvraw", [128, 2, CH], F32)
        csts = Rot([P.sb(f"cst{i}", [128, 2, CH], F32) for i in range(2)])
        for t in csts.tiles:
            self.POOL(lambda: nc.gpsimd.memset(t[0:64, 0, :], 1.0), [], [t])
            self.POOL(lambda: nc.gpsimd.memset(t[0:64, 1, :], 0.0), [], [t])
        t1s = Rot([P.sb(f"t1{i}", [128, CH], F32) for i in range(2)])
        t2s = Rot([P.sb(f"t2{i}", [128, CH], F32) for i in range(2)])
        kro = P.sb("kro", [128, CH], BF16)
        sg = P.sb("sg", [128, 4, CH], F32)
        uT = P.sb("uT", [128, 4, CH], BF16)
        rq = P.sb("rq", [128, CH], F32)
        rkv = P.sb("rkv", [128, CH], F32)
        kvn = P.sb("kvn", [128, 2, CH], BF16)
        Cp = P.sb("Cp", [128, CH], F32)
        Sp = P.sb("Sp", [128, CH], F32)
        gts = Rot([P.sb(f"gts{i}", [128, 4, CH], BF16) for i in range(2)])
        qTc = P.sb("qTc", [128, NH, CH], BF16)

        cur = {}

        def proj(ps, lhs_fn, M, wt):
            hT = cur["hT"]
            for c in range(8):
                self.PE(lambda: nc.tensor.matmul(ps[0:M, :], lhsT=lhs_fn(c), rhs=hT[:, c, :],
                                                 start=(c == 0), stop=(c == 7)), [wt, hT], [ps])

        def wpart(off):
            i = 0 if off < WSPL[1] else (1 if off < WSPL[2] else 2)
            return wparts[i], off - WSPL[i]

        def colblk(off):
            wt_, o_ = wpart(off)
            return lambda c: wt_[:, c, o_:o_ + 128]

        nchunks = self.a_chunks or self.nch
        prepped = {}

        def prep(j):
            sl = slice(j * CH, (j + 1) * CH)
            xs = []
            for s_ in range(4):
                xt = xts.next()
                self.load(self.dq.next(), xt[:], x_src[j * CH + s_ * 128: j * CH + (s_ + 1) * 128, :], xt, dram=[R_src[j]])
                xs.append(xt)
            cst_c = csts.next()
            self.load("sp", cst_c[64:96, 0, :], self.cs_d[0, :, sl], cst_c, dram=[self.R_cs[j]])
            self.load("sp", cst_c[64:96, 1, :], self.cs_d[1, :, sl], cst_c, dram=[self.R_cs[j]])
            self.norm_hT(W, xs, 0, hTs[j % 2])
            prepped[j] = cst_c

        prep(0)
        for j in range(nchunks):
            sl = slice(j * CH, (j + 1) * CH)
            cur["hT"] = hTs[j % 2]
            cst_c = prepped.pop(j)
            for i in range(3):
                ps = mps.next()
                proj(ps, colblk(i * 128), 128, wpart(i * 128)[0])
                self.ACT(lambda: nc.scalar.activation(out=sqt[:, i, :], in_=ps[:], func=AF.Square), [ps], [sqt])
                self.DVE(lambda: nc.vector.tensor_copy(out=qlT[:, i, :], in_=ps[:]), [ps], [qlT])
            for i in range(2):
                ps = mps.next()
                proj(ps, colblk(QL + i * 128), 128, wpart(QL + i * 128)[0])
                self.ACT(lambda: nc.scalar.activation(out=sqt[:, 3 + i, :], in_=ps[:], func=AF.Square), [ps], [sqt])
                self.DVE(lambda: nc.vector.tensor_copy(out=kvraw[:, i, :], in_=ps[:]), [ps], [kvraw])
            ps_k = mps.next()
            proj(ps_k, lambda c: wkr[:, c, 0, :], QK, wkr)
            ps_kr = mps.next()
            proj(ps_kr, lambda c: wkr[:, c, 1, :], QK, wkr)
            t1 = t1s.next()
            t2 = t2s.next()
            self.DVE(lambda: nc.vector.tensor_tensor(out=t1[64:96, :], in0=ps_k[64:96, :], in1=cst_c[64:96, 0, :], op=ALU.mult),
                     [ps_k, cst_c], [t1])
            self.DVE(lambda: nc.vector.tensor_tensor(out=t2[64:96, :], in0=ps_kr[64:96, :], in1=cst_c[64:96, 1, :], op=ALU.mult),
                     [ps_kr, cst_c], [t2])
            self.POOL(lambda: nc.gpsimd.tensor_tensor(out=kro[64:96, :], in0=t1[64:96, :], in1=t2[64:96, :], op=ALU.add),
                      [t1, t2], [kro])
            exp_, eo = self.ex_d[j // 4], (j % 4) * CH
            self.store("pool", exp_[KVL:KVL + ROPE, eo:eo + CH], kro[64:96, :], kro, dram=[self.R_ex[j]])
            C0 = QL + KVL + ROPE
            for i in range(4):
                ps = mps.next()
                proj(ps, colblk(C0 + CC + i * 128), 128, wpart(C0 + CC + i * 128)[0])
                self.ACT(lambda: nc.scalar.activation(out=sg[:, i, :], in_=ps[:], func=AF.Sigmoid), [ps], [sg])
            for i in range(4):
                ps = mps.next()
                proj(ps, colblk(C0 + i * 128), 128, wpart(C0 + i * 128)[0])
                self.DVE(lambda: nc.vector.tensor_tensor(out=uT[:, i, :], in0=ps[:], in1=sg[:, i, :], op=ALU.mult),
                         [ps, sg], [uT])
            self.store("pool", self.uT_d[:, sl].rearrange("(i p) t -> p i t", p=128), uT[:], uT, dram=[self.R_u[j]])
            self.store("pool", self.exh_d[:, j * 32:(j + 1) * 32].rearrange("(i p) t -> p i t", p=128), uT[:, :, CH - 32:CH],
                       uT, dram=[self.R_exh[j]])
            if j + 1 < nchunks:
                prep(j + 1)
            for i in range(3):
                self.PE(lambda: nc.tensor.matmul(ps_sq[:], lhsT=self.ones_b[:], rhs=sqt[:, i, :], start=(i == 0), stop=(i == 2)),
                        [self.ones_b, sqt], [ps_sq])
            for i in range(2):
                self.PE(lambda: nc.tensor.matmul(ps_skv[:], lhsT=self.ones_b[:], rhs=sqt[:, 3 + i, :], start=(i == 0), stop=(i == 1)),
                        [self.ones_b, sqt], [ps_skv])
            self.rstd_bcast(ps_sq, QL, rq, None)
            self.rstd_bcast(ps_skv, KVL, rkv, None)
            for i in range(2):
                self.DVE(lambda: nc.vector.tensor_tensor(out=kvn[:, i, :], in0=kvraw[:, i, :], in1=rkv[:], op=ALU.mult),
                         [kvraw, rkv], [kvn])
            self.store("pool", exp_[0:KVL, eo:eo + CH].rearrange("(i p) t -> p i t", p=128), kvn[:], kvn, dram=[self.R_ex[j]])
            self.DVE(lambda: nc.vector.scalar_tensor_tensor(out=Cp[0:QK, :], in0=cst_c[0:QK, 0, :], scalar=scale, in1=rq[0:QK, :],
                                                             op0=ALU.mult, op1=ALU.mult), [cst_c, rq], [Cp])
            self.DVE(lambda: nc.vector.scalar_tensor_tensor(out=Sp[0:QK, :], in0=cst_c[0:QK, 1, :], scalar=scale, in1=rq[0:QK, :],
                                                             op0=ALU.mult, op1=ALU.mult), [cst_c, rq], [Sp])
            G0 = C0 + 2 * CC
            for i in range(16):
                if i % 4 == 0:
                    gt = gts.next()
                ps = mps.next()
                proj(ps, colblk(G0 + i * 128), 128, wpart(G0 + i * 128)[0])
                self.ACT(lambda: nc.scalar.activation(out=gt[:, i % 4, :], in_=ps[:], func=AF.Sigmoid), [ps], [gt])
                if i % 4 == 3:
                    r0 = (i - 3) * 128
                    self.store("act", self.gt_d[r0:r0 + 512, sl].rearrange("(i p) t -> p i t", p=128), gt[:], gt,
                               dram=[self.R_gt[j]])
            for h in range(NH):
                ps_q = mps.next()
                ps_r = mps.next()
                for c in range(3):
                    self.PE(lambda: nc.tensor.matmul(ps_q[0:QK, :], lhsT=w_uq[:, c, h * QK:(h + 1) * QK], rhs=qlT[:, c, :],
                                                     start=(c == 0), stop=(c == 2)), [w_uq, qlT], [ps_q])
                for c in range(3):
                    self.PE(lambda: nc.tensor.matmul(ps_r[0:QK, :], lhsT=w_uqr[:, c, h, :], rhs=qlT[:, c, :],
                                                     start=(c == 0), stop=(c == 2)), [w_uqr, qlT], [ps_r])
                t1 = t1s.next()
                t2 = t2s.next()
                self.DVE(lambda: nc.vector.tensor_tensor(out=t1[0:QK, :], in0=ps_q[0:QK, :], in1=Cp[0:QK, :], op=ALU.mult),
                         [ps_q, Cp], [t1])
                self.DVE(lambda: nc.vector.tensor_tensor(out=t2[0:QK, :], in0=ps_r[0:QK, :], in1=Sp[0:QK, :], op=ALU.mult),
                         [ps_r, Sp], [t2])
                self.POOL(lambda: nc.gpsimd.tensor_tensor(out=qTc[0:QK, h, :], in0=t1[0:QK, :], in1=t2[0:QK, :], op=ALU.add),
                          [t1, t2], [qTc])
            self.store("pool", self.qT_d[:, sl].rearrange("(h r) t -> r h t", r=QK), qTc[0:QK, :, :], qTc, dram=[self.R_q[j]])
        P.close()


    def gmap(self):
        if self.npar == 1:
            return {g: (0, g) for g in range(16)}
        m = {}
        for p in range(2):
            for j, g in enumerate(own_chunks(2, p)):
                m[g] = (p, j)
        return m

    def exchange(self, l):
        nc = self.nc
        if self.npar == 1:
            self.exsrc, self.exhsrc = self.ex_d, self.exh_d
            self.R_exsrc = lambda g: [self.R_ex[g]]
            self.R_exhsrc = lambda g: [self.R_exh[g]]
            return
        fw = self.fw
        fw.barrier()
        groups = [[0, 1], [2, 3], [4, 5], [6, 7]]
        cc_sem = fw._alloc_sem(f"cc{l}")
        n = 0
        for i in range(self.nparts):
            nc.gpsimd.collective_compute("AllGather", ALU.bypass, replica_groups=groups,
                                         ins=[self.ex_d[i][:, :]], outs=[self.exg_d[i][:, :]]).then_inc(cc_sem, 1)
            n += 1
        nc.gpsimd.collective_compute("AllGather", ALU.bypass, replica_groups=groups,
                                     ins=[self.exh_d[:, :]], outs=[self.exhg_d[:, :]]).then_inc(cc_sem, 1)
        n += 1
        fw.all_sems[id(cc_sem)] = (cc_sem, n)
        fw.barrier()
        self.exsrc, self.exhsrc = self.exg_d, self.exhg_d
        self.R_exsrc = lambda g: []
        self.R_exhsrc = lambda g: []

    def phase_b(self, l):
        nc = self.nc
        P = Phase(nc, self.fw)
        gm = self.gmap()
        EXR = KVL + ROPE
        stg = P.sb("stgkv", [128, 2, NH * 128], F32)
        kg = P.sb("kg", [128, 2], F32)
        self.load("sp", stg[:], self.w_ukv[l].rearrange("(c p) n -> p c n", p=128), stg)
        self.load("sp", kg[:], self.kvng[l], kg)
        w_ukv = P.sb("w_ukv", [128, 2, NH * 128], BF16)
        for c in range(2):
            self.DVE(lambda: nc.vector.tensor_scalar(out=w_ukv[:, c, :], in0=stg[:, c, :], scalar1=kg[:, c:c + 1],
                                                      scalar2=None, op0=ALU.mult), [stg, kg], [w_ukv])
        mk = P.sb("mk", [128, 16, CH], BF16)
        self.load("sp", mk[:], self.masks_in.rearrange("a r p f -> p (a r) f"), mk)
        kvT = P.sb("kvT", [128, 2, S], BF16)
        kTs = [P.sb(f"kT{i}", [128, S], BF16) for i in range(2)]
        nloc = self.nch

        def kcol(g):
            p_, m_ = gm[g]
            return (p_ * nloc + m_) * CH
        for p_ in range(self.npar):
            for i in range(self.nparts):
                ext = self.exsrc[i]
                dst = slice((p_ * nloc + 4 * i) * CH, (p_ * nloc + 4 * i + 4) * CH)
                rr = [r_ for m_ in range(4 * i, 4 * i + 4) for r_ in self.R_exsrc(m_)]
                for c in range(2):
                    self.load("sp", kvT[:, c, dst], ext[p_ * EXR + c * 128: p_ * EXR + (c + 1) * 128, :], kvT, dram=rr)
                for kT in kTs:
                    self.load("sp", kT[64:96, dst], ext[p_ * EXR + KVL: p_ * EXR + EXR, :], kT, dram=rr)
        vaugs = [P.sb(f"vaug{i}", [128, S // 128, 2 * VD], BF16) for i in range(2)]
        for v in vaugs:
            self.POOL(lambda: nc.gpsimd.memset(v[:, :, VD:2 * VD], 1.0), [], [v])
        qTs = [P.sb(f"qT{i}", [128, self.ntok], BF16) for i in range(2)]
        pts = Rot([P.sb(f"pt{i}", [128, CH], BF16) for i in range(6)])
        scs = Rot([P.ps(f"sc{i}", [128, CH]) for i in range(3)])
        pos_ = Rot([P.ps(f"po{i}", [128, CH]) for i in range(2)])
        blds = Rot([P.ps(f"bld{i}", [128, CH]) for i in range(2)])
        obs = Rot([P.sb(f"ob{i}", [128, CH], F32) for i in range(2)])
        lrows = Rot([P.sb(f"lrow{i}", [128, CH], F32) for i in range(2)])
        rlss = Rot([P.sb(f"rls{i}", [128, CH], F32) for i in range(2)])
        atts = Rot([P.sb(f"att{i}", [128, CH], BF16) for i in range(2)])
        nkt_per = 4 * self.npar
        ev = 0
        def build_groups(h):
            kT, vaug = kTs[h % 2], vaugs[h % 2]
            gl = []

            def kgrp(s16):
                ps = blds.next()
                sl = slice(s16 * CH, (s16 + 1) * CH)
                for c in range(2):
                    self.PE(lambda: nc.tensor.matmul(ps[0:64, :], lhsT=w_ukv[:, c, h * 128:h * 128 + 64], rhs=kvT[:, c, sl],
                                                     start=(c == 0), stop=(c == 1)), [w_ukv, kvT], [ps])
                self.DVE(lambda: nc.vector.tensor_copy(out=kT[0:64, sl], in_=ps[0:64, :]), [ps], [kT])

            def vgrp(g8):
                ps = blds.next()
                for i in range(8):
                    kt = g8 * 8 + i
                    for c in range(2):
                        self.PE(lambda: nc.tensor.matmul(ps[:, i * 64:(i + 1) * 64], lhsT=kvT[:, c, kt * 128:(kt + 1) * 128],
                                                         rhs=w_ukv[:, c, h * 128 + 64:h * 128 + 128],
                                                         start=(c == 0), stop=(c == 1)), [w_ukv, kvT], [ps])
                src = ps[:, :].rearrange("p (i d) -> p i d", d=64)
                self.DVE(lambda: nc.vector.tensor_copy(out=vaug[:, g8 * 8:(g8 + 1) * 8, 0:VD], in_=src), [ps], [vaug])
            for s16 in range(16):
                gl.append(lambda s16=s16: kgrp(s16))
            for g8 in range(8):
                gl.append(lambda g8=g8: vgrp(g8))
            return gl

        for g_ in build_groups(0):
            g_()
        self.load("sp", qTs[0][0:QK, :], self.qT_d[0:QK, :], qTs[0], dram=self.R_q)
        for h in range(NH):
            kT, vaug, qT = kTs[h % 2], vaugs[h % 2], qTs[h % 2]
            if h + 1 < NH:
                self.load("sp", qTs[(h + 1) % 2][0:QK, :], self.qT_d[(h + 1) * QK:(h + 2) * QK, :], qTs[(h + 1) % 2], dram=self.R_q)
                pending = build_groups(h + 1)
            else:
                pending = []
            blocks = [(j, kt) for j in range(self.nch) for kt in range(nkt_per * (j + 1))]
            every = max(1, len(blocks) // 26)
            sc_of = {}
            po_of = {}
            nq = [0]

            def qk_ahead(upto):
                while nq[0] < min(upto, len(blocks)):
                    j, kt = blocks[nq[0]]
                    sc = scs.next()
                    kc = kcol(kt // 4) + (kt % 4) * 128
                    self.PE(lambda: nc.tensor.matmul(sc[:], lhsT=kT[0:QK, kc:kc + 128], rhs=qT[0:QK, j * CH:(j + 1) * CH],
                                                     start=True, stop=True), [kT, qT], [sc])
                    sc_of[nq[0]] = sc
                    nq[0] += 1

            tails = []

            def tail2(j, po, ob, rls):
                att = atts.next()
                self.DVE(lambda: nc.vector.tensor_tensor(out=att[0:VD, :], in0=ob[0:VD, :], in1=rls[0:VD, :], op=ALU.mult),
                         [ob, rls], [att])
                self.store("sp", self.at_d[h * VD:(h + 1) * VD, j * CH:(j + 1) * CH], att[0:VD, :], att, dram=[self.R_at[j]])

            for idx, (j, kt) in enumerate(blocks):
                nkt = nkt_per * (j + 1)
                qk_ahead(idx + 3)
                if kt == 0:
                    po_of[j] = pos_.next()
                po = po_of[j]
                sc = sc_of.pop(idx)
                pt = pts.next()
                self.ACT(lambda: nc.scalar.activation(out=pt[:], in_=sc[:], func=AF.Exp), [sc], [pt])
                r = kt - nkt_per * j
                if r >= 0:
                    mi = (j % 2) * 8 + r if self.npar == 2 else r
                    if r % 3 == 2:
                        self.POOL(lambda: nc.gpsimd.tensor_tensor(out=pt[:], in0=pt[:], in1=mk[:, mi, :], op=ALU.mult),
                                  [pt, mk], [pt])
                    else:
                        self.DVE(lambda: nc.vector.tensor_tensor(out=pt[:], in0=pt[:], in1=mk[:, mi, :], op=ALU.mult),
                                 [pt, mk], [pt])
                kvi = (kcol(kt // 4) + (kt % 4) * 128) // 128
                self.PE(lambda: nc.tensor.matmul(po[:], lhsT=vaug[:, kvi, :], rhs=pt[:],
                                                 start=(kt == 0), stop=(kt == nkt - 1)), [vaug, pt], [po])
                if pending and idx % every == every - 1:
                    pending.pop(0)()
                if tails and (idx >= tails[0][0] + 4):
                    _, a_ = tails.pop(0)
                    tail2(*a_)
                if kt == nkt - 1:
                    ob, lrow, rls = obs.next(), lrows.next(), rlss.next()
                    self.DVE(lambda: nc.vector.tensor_copy(out=ob[0:VD, :], in_=po[0:VD, :]), [po], [ob])
                    self.DVE(lambda: nc.vector.reciprocal(out=lrow[VD:2 * VD, :], in_=po[VD:2 * VD, :]), [po], [lrow])
                    self.fw.dma("sp", rls[0:VD, :], lrow[VD:2 * VD, :], rls, reads=[lrow], writes=[rls])
                    tails.append((idx, (j, po, ob, rls)))
            for _, a_ in tails:
                tail2(*a_)
            for g_ in pending:
                g_()
        P.close()


    def phase_c1(self, l, PW):
        nc = self.nc
        P = Phase(nc, self.fw)
        gm = self.gmap()
        cw = P.sb("cw", [128, 4, CK], F32)
        cv = P.sb("cv", [128, 3, 4], F32)
        self.load("sp", cw[:], self.conv_w[l], cw)
        self.load("sp", cv[:], self.conv_v[l], cv)
        diagw = P.sb("diagw", [128, 4, CK, 128], BF16)
        for blk in range(4):
            self.DVE(lambda: nc.vector.tensor_tensor(out=diagw[:, blk, :, :],
                                                      in0=self.ident_f[:].unsqueeze(1).to_broadcast([128, CK, 128]),
                                                      in1=cw[:, blk, :].unsqueeze(2).to_broadcast([128, CK, 128]), op=ALU.mult),
                     [self.ident_f, cw], [diagw])
        w_co, w_oa, w_out, w_r, brow = self.c2w
        self.load("pool", w_co[:], self.w_co[l].rearrange("(i p) n -> p i n", p=128), w_co)
        self.load("pool", w_oa[:], self.w_oa[l].rearrange("(i p) n -> p i n", p=128), w_oa)
        self.load("pool", w_out[:], self.w_out[l].rearrange("(i p) n -> p i n", p=128), w_out)
        self.load("pool", w_r[:], self.w_r[l].rearrange("(i p) n -> p i n", p=128), w_r)
        self.load("sp", brow[:], self.b_r[l], brow)
        if self.npar == 2:
            hs = P.sb("hs", [128, 2 * self.nch], F32)
            self.load("sp", hs[:], self.halo_sel[0:1, :].partition_broadcast(128), hs)
            has = Rot([P.sb(f"ha{i}", [128, 4, 32], BF16) for i in range(3)])
            hbs = Rot([P.sb(f"hb{i}", [128, 4, 32], BF16) for i in range(3)])
            htmp = P.sb("htmp", [128, 4, 32], F32)
        uhs = Rot([P.sb(f"uh{i}", [128, 4, 32 + CH], BF16) for i in range(3)])
        cps = Rot([P.ps(f"cps{i}", [128, CH]) for i in range(4)])
        ps_mu = P.ps("ps_mu", [128, CH])
        ps_m2 = P.ps("ps_m2", [128, CH])
        ycs = Rot([P.sb(f"yc{i}", [128, 4, CH], F32) for i in range(2)])
        ycbs = Rot([P.sb(f"ycb{i}", [128, 4, CH], BF16) for i in range(2)])
        sqbs = Rot([P.sb(f"sqb{i}", [128, 4, CH], BF16) for i in range(2)])
        mu = P.sb("mu", [128, CH], F32)
        msq = P.sb("msq", [128, CH], F32)
        rs = P.sb("rs", [128, CH], F32)
        ds_ = Rot([P.sb(f"d{i}", [128, CH], F32) for i in range(4)])
        ucs = Rot([P.sb(f"uc{i}", [128, 4, CH], BF16) for i in range(2)])

        def tail_src(g):
            p_, m_ = gm[g]
            return self.exhsrc[p_ * CC:(p_ + 1) * CC, m_ * 32:(m_ + 1) * 32].rearrange("(i p) t -> p i t", p=128), self.R_exhsrc(m_)

        ybuf = {}

        uhb = {}

        def prep(j):
            sl = slice(j * CH, (j + 1) * CH)
            uh = uhs.next()
            uhb[j] = uh
            self.load(self.dq.next(), uh[:, :, 32:32 + CH], self.uT_d[:, sl].rearrange("(i p) t -> p i t", p=128), uh,
                      dram=[self.R_u[j]])
            if self.npar == 1:
                if j == 0:
                    self.POOL(lambda: nc.gpsimd.memset(uh[:, :, 0:32], 0.0), [], [uh])
                else:
                    src, rr = tail_src(j - 1)
                    self.load(self.dq.next(), uh[:, :, 0:32], src, uh, dram=rr)
            else:
                ha, hb = has.next(), hbs.next()
                if j == 0:
                    self.POOL(lambda: nc.gpsimd.memset(ha[:], 0.0), [], [ha])
                else:
                    src, rr = tail_src(2 * j - 1)
                    self.load(self.dq.next(), ha[:], src, ha, dram=rr)
                src, rr = tail_src(2 * j)
                self.load(self.dq.next(), hb[:], src, hb, dram=rr)
                self.DVE(lambda: nc.vector.tensor_scalar(out=htmp[:], in0=ha[:], scalar1=hs[:, 2 * j:2 * j + 1], scalar2=None,
                                                          op0=ALU.mult), [ha, hs], [htmp])
                self.DVE(lambda: nc.vector.scalar_tensor_tensor(out=uh[:, :, 0:32], in0=hb[:], scalar=hs[:, 2 * j + 1:2 * j + 2],
                                                                 in1=htmp[:], op0=ALU.mult, op1=ALU.add), [hb, hs, htmp], [uh])

        def conv(j):
            yc, ycb, sqb = ycs.next(), ycbs.next(), sqbs.next()
            ybuf[j] = (yc, ycb, sqb)
            uh = uhb.pop(j)
            for blk in range(4):
                ps = cps.next()
                for k in range(CK):
                    self.PE(lambda: nc.tensor.matmul(ps[:], lhsT=diagw[:, blk, k, :], rhs=uh[:, blk, 2 + k:2 + k + CH],
                                                     start=(k == 0), stop=(k == CK - 1)), [diagw, uh], [ps])
                self.ACT(lambda: nc.scalar.activation(out=yc[:, blk, :], in_=ps[:], func=AF.Identity, bias=cv[:, 0, blk:blk + 1]),
                         [ps, cv], [yc])
                self.ACT(lambda: nc.scalar.activation(out=sqb[:, blk, :], in_=ps[:], func=AF.Square, bias=cv[:, 0, blk:blk + 1]),
                         [ps, cv], [sqb])
                self.POOL(lambda: nc.gpsimd.tensor_copy(out=ycb[:, blk, :], in_=yc[:, blk, :]), [yc], [ycb])

        def stats(j):
            sl = slice(j * CH, (j + 1) * CH)
            yc, ycb, sqb = ybuf.pop(j)
            for blk in range(4):
                self.PE(lambda: nc.tensor.matmul(ps_mu[:], lhsT=self.ones_b[:], rhs=ycb[:, blk, :], start=(blk == 0), stop=(blk == 3)),
                        [self.ones_b, ycb], [ps_mu])
            for blk in range(4):
                self.PE(lambda: nc.tensor.matmul(ps_m2[:], lhsT=self.ones_b[:], rhs=sqb[:, blk, :], start=(blk == 0), stop=(blk == 3)),
                        [self.ones_b, sqb], [ps_m2])
            self.DVE(lambda: nc.vector.tensor_scalar(out=mu[:], in0=ps_mu[:], scalar1=1.0 / CC, scalar2=None, op0=ALU.mult), [ps_mu], [mu])
            self.DVE(lambda: nc.vector.tensor_tensor(out=msq[:], in0=mu[:], in1=mu[:], op=ALU.mult), [mu], [msq])
            self.DVE(lambda: nc.vector.scalar_tensor_tensor(out=rs[:], in0=ps_m2[:], scalar=1.0 / CC, in1=msq[:],
                                                             op0=ALU.mult, op1=ALU.subtract), [ps_m2, msq], [rs])
            self.DVE(lambda: nc.vector.tensor_scalar(out=rs[:], in0=rs[:], scalar1=EPS, scalar2=None, op0=ALU.add), [rs], [rs])
            self.ACT(lambda: nc.scalar.activation(out=rs[:], in_=rs[:], func=AF.Sqrt), [rs], [rs])
            self.DVE(lambda: nc.vector.reciprocal(out=rs[:], in_=rs[:]), [rs], [rs])
            uc = ucs.next()
            for blk in range(4):
                d = ds_.next()
                self.DVE(lambda: nc.vector.tensor_tensor(out=d[:], in0=yc[:, blk, :], in1=mu[:], op=ALU.subtract), [yc, mu], [d])
                self.POOL(lambda: nc.gpsimd.tensor_tensor(out=d[:], in0=d[:], in1=rs[:], op=ALU.mult), [d, rs], [d])
                self.ACT(lambda: nc.scalar.activation(out=uc[:, blk, :], in_=d[:], func=AF.Silu, scale=cv[:, 1, blk:blk + 1],
                                                      bias=cv[:, 2, blk:blk + 1]), [d, cv], [uc])
            self.store("act", self.uc_d[:, sl].rearrange("(i p) t -> p i t", p=128), uc[:], uc, dram=[self.R_uc[j]])

        prep(0)
        if self.nch > 1:
            prep(1)
        conv(0)
        for j in range(self.nch):
            if j + 1 < self.nch:
                conv(j + 1)
            if j + 2 < self.nch:
                prep(j + 2)
            stats(j)
        P.close()

    def phase_c2(self, l):
        nc = self.nc
        P = Phase(nc, self.fw)
        x_src = self.x_in if l == 0 else self.xa
        R_src = self.R_x if l == 0 else self.R_xa
        w_co, w_oa, w_out, w_r, brow = self.c2w
        rb_b = P.sb("rb_b", [128, 36], F32)
        mps = Rot([P.ps(f"mps{i}", [128, CH]) for i in range(4)])
        pr = P.ps("pr", [128, 4, 128])
        ps0 = mps.next()
        self.PE(lambda: nc.tensor.matmul(ps0[:, 0:36], lhsT=self.ones_f[0:1, :], rhs=brow[0:1, :], start=True, stop=True),
                [self.ones_f, brow], [ps0])
        self.DVE(lambda: nc.vector.tensor_copy(out=rb_b[:], in_=ps0[:, 0:36]), [ps0], [rb_b])
        W = {
            "ss": Rot([P.sb(f"ss{i}", [128, 4], F32) for i in range(2)]),
            "rstd": Rot([P.sb(f"rstd{i}", [128, 4], F32) for i in range(2)]),
            "junk": Rot([P.sb("junk", [128, D], BF16)]),
            "xn": Rot([P.sb("xn", [128, 4, D], BF16)]),
            "pT": Rot([P.ps(f"pT{i}", [128, 2 * CH], BF16) for i in range(2)]),
        }
        xts = Rot([P.sb(f"xt{i}", [128, D], F32) for i in range(9)])
        ucs = Rot([P.sb(f"ucl{i}", [128, 4, CH], BF16) for i in range(2)])
        ats = Rot([P.sb(f"atl{i}", [128, 4, CH], BF16) for i in range(2)])
        gcs = [P.sb(f"gc{i}", [128, 16, CH], BF16) for i in range(2)]
        t1s = Rot([P.sb(f"t1{i}", [128, CH], F32) for i in range(2)])
        t2s = Rot([P.sb(f"t2{i}", [128, CH], F32) for i in range(2)])
        tys = Rot([P.sb(f"ty{i}", [128, CH], F32) for i in range(2)])
        mTs = [P.sb(f"mT{i}", [128, 8, CH], BF16) for i in range(2)]
        h2T = P.sb("h2T", [128, 8, CH], BF16)
        R = {k: P.sb(k, shp, F32) for k, shp in dict(
            lg=[128, 4, 36], gmax=[128, 4], geq=[128, 4, 4], gsh=[128, 4, 4], gsum=[128, 4], gval=[128, 4],
            tmp=[128, 4, 4, 8], esel=[128, 4, 8], m1=[128, 4], mask1=[128, 4, 8], esel2=[128, 4, 8], m2=[128, 4],
            mask2=[128, 4, 8], w1=[128, 4], w2=[128, 4], ws=[128, 4, 8], ws2=[128, 4, 8]).items()}
        V = nc.vector
        cst = {}

        ld = {}

        def S1L(j):
            gc = gcs[j % 2]
            sl = slice(j * CH, (j + 1) * CH)
            ucl, atl = ucs.next(), ats.next()
            self.load(self.dq.next(), ucl[:], self.uc_d[:, sl].rearrange("(i p) t -> p i t", p=128), ucl, dram=[self.R_uc[j]])
            self.load(self.dq.next(), atl[:], self.at_d[:, sl].rearrange("(i p) t -> p i t", p=128), atl, dram=[self.R_at[j]])
            self.load(self.dq.next(), gc[:], self.gt_d[:, sl].rearrange("(i p) t -> p i t", p=128), gc, dram=[self.R_gt[j]])
            xs = []
            for s_ in range(4):
                xt = xts.next()
                self.load(self.dq.next(), xt[:], x_src[j * CH + s_ * 128: j * CH + (s_ + 1) * 128, :], xt, dram=[R_src[j]])
                xs.append(xt)
            ld[j] = (ucl, atl, gc, xs)

        def S1(j):
            mT = mTs[j % 2]
            ucl, atl, gc, xs = ld.pop(j)
            for dm in range(8):
                dsl = slice(dm * 128, (dm + 1) * 128)
                ps_c, ps_a = mps.next(), mps.next()
                for i in range(4):
                    self.PE(lambda: nc.tensor.matmul(ps_c[:], lhsT=w_co[:, i, dsl], rhs=ucl[:, i, :], start=(i == 0), stop=(i == 3)),
                            [w_co, ucl], [ps_c])
                for i in range(4):
                    self.PE(lambda: nc.tensor.matmul(ps_a[:], lhsT=w_oa[:, i, dsl], rhs=atl[:, i, :], start=(i == 0), stop=(i == 3)),
                            [w_oa, atl], [ps_a])
                t1, t2 = t1s.next(), t2s.next()
                self.DVE(lambda: V.tensor_tensor(out=t1[:], in0=ps_a[:], in1=gc[:, dm, :], op=ALU.mult), [ps_a, gc], [t1])
                self.DVE(lambda: V.tensor_tensor(out=t2[:], in0=ps_c[:], in1=gc[:, 8 + dm, :], op=ALU.mult), [ps_c, gc], [t2])
                self.POOL(lambda: nc.gpsimd.tensor_tensor(out=mT[:, dm, :], in0=t1[:], in1=t2[:], op=ALU.add), [t1, t2], [mT])
            cst[j] = (xs, mT)

        def S2(j):
            xs, mT = cst[j]
            for s_ in range(4):
                for hf in range(2):
                    hsl = slice(hf * 512, (hf + 1) * 512)
                    ps_y = mps.next()
                    for dm in range(8):
                        self.PE(lambda: nc.tensor.matmul(ps_y[:], lhsT=mT[:, dm, s_ * 128:(s_ + 1) * 128], rhs=w_out[:, dm, hsl],
                                                         start=(dm == 0), stop=(dm == 7)), [mT, w_out], [ps_y])
                    ty = tys.next()
                    self.DVE(lambda: V.tensor_tensor(out=ty[:], in0=ps_y[:], in1=self.gtb[:, 0, hsl], op=ALU.mult),
                             [ps_y, self.gtb], [ty])
                    self.POOL(lambda: nc.gpsimd.tensor_tensor(out=xs[s_][:, hsl], in0=xs[s_][:, hsl], in1=ty[:], op=ALU.add),
                              [xs[s_], ty], [xs[s_]])
                self.store("pool", self.xa[j * CH + s_ * 128: j * CH + (s_ + 1) * 128, :], xs[s_][:], xs[s_],
                           dram=[self.R_xa[j]])

        def S34(j):
            xs, mT = cst.pop(j)
            sl = slice(j * CH, (j + 1) * CH)
            self.norm_hT(W, xs, 1, h2T)
            self.store("act", self.h2_d[:, sl].rearrange("(c p) t -> p c t", p=128), h2T[:], h2T, dram=[self.R_h2[j]])
            for s_ in range(4):
                for c in range(8):
                    self.PE(lambda: nc.tensor.matmul(pr[:, s_, 0:36], lhsT=h2T[:, c, s_ * 128:(s_ + 1) * 128], rhs=w_r[:, c, :],
                                                     start=(c == 0), stop=(c == 7)), [h2T, w_r], [pr])
            lg = R["lg"]
            self.DVE(lambda: V.tensor_tensor(out=lg[:], in0=pr[:, :, 0:36], in1=rb_b[:].unsqueeze(1).to_broadcast([128, 4, 36]),
                                             op=ALU.add), [pr, rb_b], [lg])
            gl = lg[:, :, 0:4]
            el = lg[:, :, 4:36].rearrange("p s (g e) -> p s g e", g=4)

            def b3(t, n):
                return t[:].unsqueeze(2).to_broadcast([128, 4, n])
            self.DVE(lambda: V.tensor_reduce(out=R["gmax"][:], in_=gl, axis=AX.X, op=ALU.max), [lg], [R["gmax"]])
            self.DVE(lambda: V.tensor_tensor(out=R["geq"][:], in0=gl, in1=b3(R["gmax"], 4), op=ALU.is_equal), [lg, R["gmax"]], [R["geq"]])
            self.DVE(lambda: V.tensor_tensor(out=R["gsh"][:], in0=gl, in1=b3(R["gmax"], 4), op=ALU.subtract), [lg, R["gmax"]], [R["gsh"]])
            self.ACT(lambda: nc.scalar.activation(out=R["gsh"][:], in_=R["gsh"][:], func=AF.Exp), [R["gsh"]], [R["gsh"]])
            self.DVE(lambda: V.tensor_reduce(out=R["gsum"][:], in_=R["gsh"][:], axis=AX.X, op=ALU.add), [R["gsh"]], [R["gsum"]])
            self.DVE(lambda: V.reciprocal(out=R["gval"][:], in_=R["gsum"][:]), [R["gsum"]], [R["gval"]])
            self.DVE(lambda: V.tensor_tensor(out=R["tmp"][:], in0=el, in1=R["geq"][:].unsqueeze(3).to_broadcast([128, 4, 4, 8]),
                                             op=ALU.mult), [lg, R["geq"]], [R["tmp"]])
            self.DVE(lambda: V.tensor_reduce(out=R["esel"][:], in_=R["tmp"][:].rearrange("p s g e -> p s e g"), axis=AX.X, op=ALU.add),
                     [R["tmp"]], [R["esel"]])
            self.DVE(lambda: V.tensor_reduce(out=R["m1"][:], in_=R["esel"][:], axis=AX.X, op=ALU.max), [R["esel"]], [R["m1"]])
            self.DVE(lambda: V.tensor_tensor(out=R["mask1"][:], in0=R["esel"][:], in1=b3(R["m1"], 8), op=ALU.is_equal),
                     [R["esel"], R["m1"]], [R["mask1"]])
            self.DVE(lambda: V.scalar_tensor_tensor(out=R["esel2"][:], in0=R["mask1"][:], scalar=-1e30, in1=R["esel"][:],
                                                    op0=ALU.mult, op1=ALU.add), [R["mask1"], R["esel"]], [R["esel2"]])
            self.DVE(lambda: V.tensor_reduce(out=R["m2"][:], in_=R["esel2"][:], axis=AX.X, op=ALU.max), [R["esel2"]], [R["m2"]])
            self.DVE(lambda: V.tensor_tensor(out=R["mask2"][:], in0=R["esel2"][:], in1=b3(R["m2"], 8), op=ALU.is_equal),
                     [R["esel2"], R["m2"]], [R["mask2"]])
            self.DVE(lambda: V.tensor_tensor(out=R["w1"][:], in0=R["m1"][:], in1=R["m2"][:], op=ALU.subtract), [R["m1"], R["m2"]], [R["w1"]])
            self.ACT(lambda: nc.scalar.activation(out=R["w1"][:], in_=R["w1"][:], func=AF.Sigmoid), [R["w1"]], [R["w1"]])
            self.DVE(lambda: V.tensor_scalar(out=R["w2"][:], in0=R["w1"][:], scalar1=-1.0, scalar2=1.0, op0=ALU.mult, op1=ALU.add),
                     [R["w1"]], [R["w2"]])
            self.DVE(lambda: V.tensor_tensor(out=R["w1"][:], in0=R["w1"][:], in1=R["gval"][:], op=ALU.mult), [R["w1"], R["gval"]], [R["w1"]])
            self.DVE(lambda: V.tensor_tensor(out=R["w2"][:], in0=R["w2"][:], in1=R["gval"][:], op=ALU.mult), [R["w2"], R["gval"]], [R["w2"]])
            self.DVE(lambda: V.tensor_tensor(out=R["ws"][:], in0=R["mask1"][:], in1=b3(R["w1"], 8), op=ALU.mult), [R["mask1"], R["w1"]], [R["ws"]])
            self.DVE(lambda: V.tensor_tensor(out=R["ws2"][:], in0=R["mask2"][:], in1=b3(R["w2"], 8), op=ALU.mult), [R["mask2"], R["w2"]], [R["ws2"]])
            self.DVE(lambda: V.tensor_tensor(out=R["ws"][:], in0=R["ws"][:], in1=R["ws2"][:], op=ALU.add), [R["ws"], R["ws2"]], [R["ws"]])
            cv_ = self.comb[:, j * 4:(j + 1) * 4, :].rearrange("p s (g e) -> p s g e", g=4)
            self.DVE(lambda: V.tensor_tensor(out=cv_, in0=R["geq"][:].unsqueeze(3).to_broadcast([128, 4, 4, 8]),
                                             in1=R["ws"][:].unsqueeze(2).to_broadcast([128, 4, 4, 8]), op=ALU.mult),
                     [R["geq"], R["ws"]], [self.comb])

        S1L(0)
        S1(0)
        for j in range(self.nch):
            if j + 1 < self.nch:
                S1L(j + 1)
            S2(j)
            if j + 1 < self.nch:
                S1(j + 1)
            S34(j)
        if "comb" in self.debug and l == 0:
            d1 = self.dbg_out("comb", [128, (self.ntok // 128) * 32])
            self.store("sp", d1[:, :], self.comb[:].rearrange("p a b -> p (a b)"), self.comb)
        P.close()

    def phase_c(self, l):
        nc = self.nc
        PW = Phase(nc, self.fw)
        w_co = PW.sb("w_co", [128, 4, D], BF16)
        w_oa = PW.sb("w_oa", [128, 4, D], BF16)
        w_out = PW.sb("w_out", [128, 8, D], BF16)
        w_r = PW.sb("w_r", [128, 8, 36], BF16)
        brow = PW.sb("brow", [1, 36], F32)
        self.c2w = (w_co, w_oa, w_out, w_r, brow)
        self.phase_c1(l, PW)
        self.phase_c2(l)
        PW.close()

    def phase_d(self, l):
        nc = self.nc
        P = Phase(nc, self.fw)
        last = (l == self.depth - 1)
        wgs = [[P.sb(f"wg{i}{q}", [128, 8, 4 * FE], BF16) for q in range(2)] for i in range(2)]
        wus = [[P.sb(f"wu{i}{q}", [128, 8, 4 * FE], BF16) for q in range(2)] for i in range(2)]
        wds = [[P.sb(f"wd{i}{q}", [128, 4, D], BF16) for q in range(2)] for i in range(2)]

        def load_w(g):
            i = g % 2
            gv = self.e_g[l, g].rearrange("p (c e f) -> p c e f", c=8, e=NE)
            uv = self.e_u[l, g].rearrange("p (c e f) -> p c e f", c=8, e=NE)
            dv = self.e_d[l, g].rearrange("p (e n) -> p e n", e=NE)
            fl = []
            for q in range(2):
                fl.append(lambda q=q: self.load("pool", wgs[i][q][:].rearrange("p c (e f) -> p c e f", e=4),
                                                gv[:, :, q * 4:(q + 1) * 4, :], wgs[i][q]))
                fl.append(lambda q=q: self.load("pool", wus[i][q][:].rearrange("p c (e f) -> p c e f", e=4),
                                                uv[:, :, q * 4:(q + 1) * 4, :], wus[i][q]))
                fl.append(lambda q=q: self.load("pool", wds[i][q][:], dv[:, q * 4:(q + 1) * 4, :], wds[i][q]))
            return fl
        for f_ in load_w(0):
            f_()
        h2s = Rot([P.sb(f"h2{i}", [128, 8, CH], BF16) for i in range(2)])
        pas = Rot([P.ps(f"pa{i}", [128, CH]) for i in range(2)])
        pus = Rot([P.ps(f"pu{i}", [128, CH]) for i in range(2)])
        pts_ = Rot([P.ps(f"ptr{i}", [128, 2 * CH], BF16) for i in range(2)])
        pos_ = [P.ps(f"pod{i}", [128, CH]) for i in range(2)]
        sas = Rot([P.sb(f"sa{i}", [128, CH], F32) for i in range(2)])
        tts = Rot([P.sb(f"tt{i}", [128, CH], F32) for i in range(2)])
        hids = Rot([P.sb(f"hid{i}", [128, 4, FE], BF16) for i in range(2)])
        hTs = Rot([P.sb(f"hidT{i}", [128, 4, 128], BF16) for i in range(2)])
        xts = Rot([P.sb(f"xd{i}", [128, D], F32) for i in range(4)])
        tys = Rot([P.sb(f"tyd{i}", [128, CH], F32) for i in range(2)])
        if last:
            fr = P.sb("fr", [1, D], F32)
            self.load("sp", fr[:], self.fng[:, :], fr)
            fgb = P.sb("fgb", [128, D], F32)
            for hf in range(2):
                pb = pas.next()
                self.PE(lambda: nc.tensor.matmul(pb[:], lhsT=self.ones_f[0:1, :], rhs=fr[0:1, hf * 512:(hf + 1) * 512],
                                                 start=True, stop=True), [self.ones_f, fr], [pb])
                self.ACT(lambda: nc.scalar.copy(out=fgb[:, hf * 512:(hf + 1) * 512], in_=pb[:]), [pb], [fgb])
            fss = Rot([P.sb(f"fss{i}", [128, 1], F32) for i in range(2)])
            fjunk = P.sb("fjunk", [128, D], BF16)
            fos = Rot([P.sb(f"fo{i}", [128, D], F32) for i in range(2)])
        units = [(g, j, s_, eg) for g in range(NG) for j in range(self.nch) for s_ in range(4) for eg in range(2)]
        st = {}

        def GU(n):
            g, j, s_, eg = units[n]
            if eg == 1:
                xt = xts.next()
                rows_ = slice(j * CH + s_ * 128, j * CH + (s_ + 1) * 128)
                self.load(self.dq.next(), xt[:], self.xa[rows_, :], xt, dram=[self.R_xa[j]])
                st[("xt", n)] = xt
            def fetch_h2(jj):
                h2_ = h2s.next()
                sl_ = slice(jj * CH, (jj + 1) * CH)
                self.load(self.dq.next(), h2_[:], self.h2_d[:, sl_].rearrange("(c p) t -> p c t", p=128), h2_, dram=[self.R_h2[jj]])
                return h2_
            if s_ == 0 and eg == 0:
                st["h2"] = st.pop("h2next") if "h2next" in st else fetch_h2(j)
            if s_ == 2 and eg == 0 and n + 4 < len(units):
                st["h2next"] = fetch_h2(units[n + 4][1])
            h2 = st["h2"]
            wg, wu = wgs[g % 2][eg], wus[g % 2][eg]
            tsl = slice(s_ * 128, (s_ + 1) * 128)
            pa, pu = pas.next(), pus.next()
            for c in range(8):
                self.PE(lambda: nc.tensor.matmul(pa[:], lhsT=h2[:, c, tsl], rhs=wg[:, c, :], start=(c == 0), stop=(c == 7)),
                        [h2, wg], [pa])
            for c in range(8):
                self.PE(lambda: nc.tensor.matmul(pu[:], lhsT=h2[:, c, tsl], rhs=wu[:, c, :], start=(c == 0), stop=(c == 7)),
                        [h2, wu], [pu])
            st[("pp", n)] = (pa, pu)

        def MID(n):
            g, j, s_, eg = units[n]
            ti = j * 4 + s_
            pa, pu = st.pop(("pp", n))
            sa, tt, hid, hT = sas.next(), tts.next(), hids.next(), hTs.next()
            self.ACT(lambda: nc.scalar.activation(out=sa[:], in_=pa[:], func=AF.Silu), [pa], [sa])
            for e in range(4):
                ce = g * 8 + eg * 4 + e
                self.DVE(lambda: nc.vector.scalar_tensor_tensor(out=hid[:, e, :], in0=sa[:, e * FE:(e + 1) * FE],
                                                                 scalar=self.comb[:, ti, ce:ce + 1], in1=pu[:, e * FE:(e + 1) * FE],
                                                                 op0=ALU.mult, op1=ALU.mult), [sa, pu, self.comb], [hid])
            ptr = pts_.next()
            for e in range(4):
                self.PE(lambda: nc.tensor.transpose(out=ptr[:, e * 128:(e + 1) * 128], in_=hid[:, e, :], identity=self.ident[:]),
                        [hid, self.ident], [ptr])
            if n % 2 == 0:
                self.ACT(lambda: nc.scalar.copy(out=hT[:].rearrange("p e t -> p (e t)"), in_=ptr[:, 0:CH]), [ptr], [hT])
            else:
                self.DVE(lambda: nc.vector.tensor_copy(out=hT[:].rearrange("p e t -> p (e t)"), in_=ptr[:, 0:CH]), [ptr], [hT])
            st[("hT", n)] = hT

        def DN(n):
            g, j, s_, eg = units[n]
            wd = wds[g % 2][eg]
            hT = st.pop(("hT", n))
            rows = slice(j * CH + s_ * 128, j * CH + (s_ + 1) * 128)
            for hf in range(2):
                for e in range(4):
                    self.PE(lambda: nc.tensor.matmul(pos_[hf][:], lhsT=hT[:, e, :], rhs=wd[:, e, hf * 512:(hf + 1) * 512],
                                                     start=(eg == 0 and e == 0), stop=(eg == 1 and e == 3)),
                            [hT, wd], [pos_[hf]])
            if eg == 0:
                return
            xt = st.pop(("xt", n))
            for hf in range(2):
                hsl = slice(hf * 512, (hf + 1) * 512)
                ty = tys.next()
                self.DVE(lambda: nc.vector.tensor_tensor(out=ty[:], in0=pos_[hf][:], in1=self.gtb[:, 1, hsl], op=ALU.mult),
                         [pos_[hf], self.gtb], [ty])
                self.POOL(lambda: nc.gpsimd.tensor_tensor(out=xt[:, hsl], in0=xt[:, hsl], in1=ty[:], op=ALU.add), [xt, ty], [xt])
            if last and g == NG - 1:
                fs, fo = fss.next(), fos.next()
                self.ACT(lambda: nc.scalar.activation(out=fjunk[:], in_=xt[:], func=AF.Square, accum_out=fs[:]), [xt], [fjunk, fs])
                self.DVE(lambda: nc.vector.tensor_scalar(out=fs[:], in0=fs[:], scalar1=1.0 / D, scalar2=EPS, op0=ALU.mult, op1=ALU.add),
                         [fs], [fs])
                self.ACT(lambda: nc.scalar.activation(out=fs[:], in_=fs[:], func=AF.Sqrt), [fs], [fs])
                self.DVE(lambda: nc.vector.reciprocal(out=fs[:], in_=fs[:]), [fs], [fs])
                self.DVE(lambda: nc.vector.scalar_tensor_tensor(out=fo[:], in0=xt[:], scalar=fs[:, 0:1], in1=fgb[:],
                                                                 op0=ALU.mult, op1=ALU.mult), [xt, fs, fgb], [fo])
                self.store("pool", self.out[rows, :], fo[:], fo, dram=[self.R_out[j]])
            else:
                self.store("pool", self.xa[rows, :], xt[:], xt, dram=[self.R_xa[j]])

        N = len(units)
        per_stage = N // NG
        wq = load_w(1)
        GU(0)
        for n in range(N):
            if n + 1 < N:
                GU(n + 1)
            MID(n)
            if n >= 1:
                DN(n - 1)
                if n % per_stage == 0 and n // per_stage + 1 < NG:
                    wq = load_w(n // per_stage + 1)
            if wq and n % 2 == 1:
                wq.pop(0)()
        DN(N - 1)
        P.close()


def _const_tables(npar):
    consts = np.zeros((128, 4), np.float32)
    inv_freq = (10000.0 ** (-np.arange(0, ROPE, 2, dtype=np.float32) / ROPE)).astype(np.float32)
    for r in range(32):
        consts[64 + r, 0] = inv_freq[r % 16]
        consts[64 + r, 1] = -1.0 if r < 16 else 1.0
    ident = np.eye(128, dtype=np.float32)
    pk = np.arange(128)[:, None]
    f = np.arange(CH)[None, :]
    diag = [((pk + d * 128) <= f).astype(np.float32) for d in range(4)]
    zeros = np.zeros((128, CH), np.float32)
    ones = np.ones((128, CH), np.float32)
    lo = diag + [zeros] * 4
    hi = [ones] * 4 + diag
    return consts, ident, np.stack(lo), np.stack(hi)


def prep_inputs(inputs, npar, depth=DEPTH, used=None):
    f32 = np.float32
    L = DEPTH
    consts, ident, m_lo, m_hi = _const_tables(npar)
    g = lambda k: np.asarray(inputs[k])
    col = lambda v, n: np.ascontiguousarray(v.reshape(L, n, 128).transpose(0, 2, 1)).astype(f32)
    shared = {
        "consts": consts, "ident": ident,
        "ada_w": g("ada_w"), "ada_b": g("ada_b").reshape(L, 1, 6 * D),
        "n1g": col(g("norm1_g"), 8), "n2g": col(g("norm2_g"), 8),
        "w_in": g("w_in"), "qng": col(g("q_norm_g"), 3), "w_uq": g("w_uq"),
        "kvng": col(g("kv_norm_g"), 2), "w_ukv": g("w_ukv"), "w_oa": g("w_o_attn"),
        "conv_w": np.ascontiguousarray(g("conv_w").transpose(0, 2, 1).reshape(L, 4, 128, CK).transpose(0, 2, 1, 3)),
        "conv_v": np.ascontiguousarray(np.stack([g("conv_b"), g("conv_ln_g"), g("conv_ln_b")], 1)
                                       .reshape(L, 3, 4, 128).transpose(0, 3, 1, 2)),
        "w_co": g("w_conv_out"), "w_out": g("w_out"),
        "w_r": np.ascontiguousarray(np.concatenate([g("router_group_w"), g("router_expert_w")], -1)),
        "b_r": np.concatenate([g("router_group_b"), g("router_expert_b")], -1).reshape(L, 1, 36),
        "e_g": np.ascontiguousarray(g("expert_w_gate").reshape(L, NG, NE, 8, 128, FE).transpose(0, 1, 4, 3, 2, 5)
                                    ).reshape(L, NG, 128, 8 * NE * FE),
        "e_u": np.ascontiguousarray(g("expert_w_up").reshape(L, NG, NE, 8, 128, FE).transpose(0, 1, 4, 3, 2, 5)
                                    ).reshape(L, NG, 128, 8 * NE * FE),
        "e_d": np.ascontiguousarray(g("expert_w_down").transpose(0, 1, 3, 2, 4)).reshape(L, NG, 128, NE * D),
        "fng": g("final_norm_g").reshape(1, D),
    }
    x = g("x")
    c = g("c")
    pos = g("positions")
    nch = 16 // npar
    maps = []
    for core in range(8):
        b, p = core // 2, core % 2
        if npar == 1:
            chunks = list(range(16))
        else:
            chunks = own_chunks(2, p)
        rows = np.concatenate([np.arange(cj * CH, (cj + 1) * CH) for cj in chunks])
        masks = np.zeros((2, 8, 128, CH), ml_dtypes.bfloat16)
        hsel = np.zeros((1, 2 * nch), f32)
        for j in range(nch):
            if npar == 1:
                masks[0, :4] = m_lo[:4]
                masks[1, :4] = m_lo[:4]
                hsel[0, 2 * j] = 1.0
            else:
                is_hi = (j + p) % 2 == 1
                masks[j % 2] = m_hi if is_hi else m_lo
                hsel[0, 2 * j] = 0.0 if is_hi else 1.0
                hsel[0, 2 * j + 1] = 1.0 if is_hi else 0.0
        m = {k: (v[:depth] if (v.ndim >= 3 and v.shape[0] == L and k not in ("masks",)) else v) for k, v in shared.items()}
        m["x_own"] = np.ascontiguousarray(x[b][rows])
        m["pos_own"] = np.ascontiguousarray(pos[b][rows]).reshape(1, -1).astype(np.int32)
        m["c_col"] = np.ascontiguousarray(c[b].reshape(8, 128).T)
        m["masks"] = masks
        m["halo_sel"] = hsel
        if used is not None:
            m = {k: v for k, v in m.items() if k in used}
        maps.append((m, b, rows))
    return maps


_PROG_CACHE = {}


def get_prog(npar, **kw):
    key = (npar, tuple(sorted((k, str(v)) for k, v in kw.items())))
    if key not in _PROG_CACHE:
        _PROG_CACHE[key] = Prog(npar=npar, **kw)
    return _PROG_CACHE[key]


NPAR = 2


def kernel(**inputs):
    prog = get_prog(NPAR)
    maps = prep_inputs(inputs, NPAR, DEPTH, set(prog.used_inputs))
    res = run_bass_kernel_spmd(prog.nc, [m for m, _, _ in maps], core_ids=list(range(8)))
    out = np.zeros((B, S, D), np.float32)
    for core, (m, b, rows) in enumerate(maps):
        if NPAR == 1 and core % 2 == 1:
            continue
        out[b][rows] = res.results[core]["out"]
    return out
```
